# Optimizing a Trainium2 kernel written in Bass

```python
import jax, jax.numpy as jnp
from jax import lax
import numpy as np

D_MODEL = 1024
BATCH = 4
SEQ = 4096
DEPTH = 1

HEAD_DIM = 64
A_HEADS = 8
A_KV_HEADS = 2
A_GROUP = A_HEADS // A_KV_HEADS
A_HALF_WINDOW = 128
A_BLOCK = 128
B_HEADS = 8
DILATED_PATTERNS = ((128, 1), (512, 4), (2048, 16))
B_BLOCK = 64
A_Q = A_HEADS * HEAD_DIM
A_KV = A_KV_HEADS * HEAD_DIM
B_W = B_HEADS * HEAD_DIM
MIX_WIDTH = A_Q + B_W
IN_WIDTH = A_Q + 2 * A_KV + 3 * B_W
ROPE_THETA = 500000.0
ROT_DIM = HEAD_DIM // 4
N_EXPERTS = 16
CAPACITY_FACTOR = 2
EXPERT_FF = 2048
NORM_EPS = 1e-6
NEG_INF = -1e30

kernel_name = "hybrid_gqa_sink_dilated_ec_moe"


def rms_norm(x, g):
    xf = x.astype(jnp.float32)
    y = xf * lax.rsqrt(jnp.mean(xf * xf, axis=-1, keepdims=True) + NORM_EPS)
    return (y * g.astype(jnp.float32)).astype(x.dtype)


def rope_tables(seq_len):
    inv_freq = ROPE_THETA ** (-jnp.arange(0, ROT_DIM, 2, dtype=jnp.float32) / ROT_DIM)
    ang = jnp.arange(seq_len, dtype=jnp.float32)[:, None] * inv_freq[None, :]
    return jnp.cos(ang), jnp.sin(ang)


def partial_rope(x, cos, sin):
    half = ROT_DIM // 2
    shape = (1, x.shape[1]) + (1,) * (x.ndim - 3) + (half,)
    c = cos.reshape(shape)
    s = sin.reshape(shape)
    xf = x.astype(jnp.float32)
    x1, x2, rest = xf[..., :half], xf[..., half:ROT_DIM], xf[..., ROT_DIM:]
    out = jnp.concatenate([x1 * c - x2 * s, x2 * c + x1 * s, rest], axis=-1)
    return out.astype(x.dtype)


def banded_attention(q, k, v, half_window, block, sink=None):
    n, L, hk, g, dh = q.shape
    nb = -(-L // block)
    lp = nb * block
    pad = lp - L
    qp = jnp.pad(q, ((0, 0), (0, pad), (0, 0), (0, 0), (0, 0)))
    kp = jnp.pad(k, ((0, 0), (block, block + pad), (0, 0), (0, 0)))
    vp = jnp.pad(v, ((0, 0), (block, block + pad), (0, 0), (0, 0)))

    def windows(t):
        return jnp.concatenate(
            [t[:, i * block:i * block + lp].reshape(n, nb, block, hk, dh) for i in range(3)], axis=2)

    qb = qp.reshape(n, nb, block, hk, g, dh)
    kw, vw = windows(kp), windows(vp)
    qpos = jnp.arange(nb)[:, None] * block + jnp.arange(block)[None, :]
    kpos = (jnp.arange(nb)[:, None] - 1) * block + jnp.arange(3 * block)[None]
    rel = kpos[:, None, :] - qpos[:, :, None]
    valid = (jnp.abs(rel) <= half_window) & (kpos[:, None, :] >= 0) & (kpos[:, None, :] < L)

    s = jnp.einsum('nbqhgd,nbkhd->nbhgqk', qb.astype(jnp.float32), kw.astype(jnp.float32)) * (dh ** -0.5)
    s = jnp.where(valid[None, :, None, None], s, NEG_INF)
    m = jnp.max(s, axis=-1, keepdims=True)
    if sink is not None:
        sk = sink.astype(jnp.float32)[None, None, :, :, None, None]
        m = jnp.maximum(m, sk)
    p = jnp.exp(s - m)
    denom = jnp.sum(p, axis=-1, keepdims=True)
    if sink is not None:
        denom = denom + jnp.exp(sk - m)
    o = jnp.einsum('nbhgqk,nbkhd->nbhgqd', p, vw.astype(jnp.float32)) / denom
    lse = (m + jnp.log(denom))[..., 0]
    o = o.transpose(0, 1, 4, 2, 3, 5).reshape(n, lp, hk, g, dh)[:, :L]
    lse = lse.transpose(0, 1, 4, 2, 3).reshape(n, lp, hk, g)[:, :L]
    return o.astype(q.dtype), lse


def dilated_attention(q, k, v):
    b, s, h, dh = q.shape
    outs, lses = [], []
    for (w, d) in DILATED_PATTERNS:
        hw = w // (2 * d)

        def fold(t):
            return t.reshape(b, s // d, d, h, dh).transpose(0, 2, 1, 3, 4).reshape(b * d, s // d, h, dh)

        o, lse = banded_attention(fold(q)[:, :, :, None], fold(k), fold(v), hw, B_BLOCK)
        o = o[:, :, :, 0].reshape(b, d, s // d, h, dh).transpose(0, 2, 1, 3, 4).reshape(b, s, h, dh)
        lse = lse[..., 0].reshape(b, d, s // d, h).transpose(0, 2, 1, 3).reshape(b, s, h)
        outs.append(o.astype(jnp.float32))
        lses.append(lse)
    alpha = jax.nn.softmax(jnp.stack(lses, axis=0), axis=0)
    out = jnp.sum(alpha[..., None] * jnp.stack(outs, axis=0), axis=0)
    return out.astype(q.dtype)


def expert_choice_moe(h, w_router, w_gate, w_up, w_down):
    b, s, dm = h.shape
    cap = CAPACITY_FACTOR * s // N_EXPERTS
    logits = jnp.einsum('bsd,de->bse', h.astype(jnp.float32), w_router.astype(jnp.float32))
    affinity = jax.nn.softmax(logits, axis=-1)
    gates, idx = lax.top_k(jnp.swapaxes(affinity, 1, 2), cap)
    xe = jax.vmap(lambda hb, ib: hb[ib])(h, idx)
    a = jnp.einsum('becd,edf->becf', xe, w_gate)
    u = jnp.einsum('becd,edf->becf', xe, w_up)
    ye = jnp.einsum('becf,efd->becd', jax.nn.silu(a) * u, w_down)
    ye = ye * gates[..., None].astype(ye.dtype)
    out = jax.vmap(lambda yb, ib: jax.ops.segment_sum(
        yb.reshape(-1, dm), ib.reshape(-1), num_segments=s))(ye, idx)
    return out.astype(h.dtype)


def setup_inputs(seed: int = 0) -> dict:
    key = jax.random.key(seed)
    ks = jax.random.split(key, 14)
    f32 = jnp.float32
    nrm = lambda k, shape, fan: jax.random.normal(k, shape, f32) * (fan ** -0.5)
    gain = lambda k, shape: 1.0 + 0.02 * jax.random.normal(k, shape, f32)
    return {
        "x": jax.random.normal(ks[0], (BATCH, SEQ, D_MODEL), f32),
        "g_mix": gain(ks[1], (DEPTH, D_MODEL)),
        "w_in": nrm(ks[2], (DEPTH, D_MODEL, IN_WIDTH), D_MODEL),
        "a_sink": 0.5 * jax.random.normal(ks[3], (DEPTH, A_HEADS), f32),
        "g_out_a": gain(ks[4], (DEPTH, A_Q)),
        "g_out_b": gain(ks[5], (DEPTH, B_W)),
        "w_out": nrm(ks[6], (DEPTH, MIX_WIDTH, D_MODEL), MIX_WIDTH),
        "g_ffn": gain(ks[7], (DEPTH, D_MODEL)),
        "w_router": nrm(ks[8], (DEPTH, D_MODEL, N_EXPERTS), D_MODEL),
        "w_gate": nrm(ks[9], (DEPTH, N_EXPERTS, D_MODEL, EXPERT_FF), D_MODEL),
        "w_up": nrm(ks[10], (DEPTH, N_EXPERTS, D_MODEL, EXPERT_FF), D_MODEL),
        "w_down": nrm(ks[11], (DEPTH, N_EXPERTS, EXPERT_FF, D_MODEL), EXPERT_FF),
        "g_final": gain(ks[12], (D_MODEL,)),
    }


def reference(x, g_mix, w_in, a_sink, g_out_a, g_out_b, w_out, g_ffn,
              w_router, w_gate, w_up, w_down, g_final):
    b, s, _ = x.shape
    cos, sin = rope_tables(s)
    splits = np.cumsum([A_Q, A_KV, A_KV, B_W, B_W]).tolist()
    for l in range(DEPTH):
        h = rms_norm(x, g_mix[l])
        proj = jnp.einsum('bsd,de->bse', h, w_in[l])
        qa, ka, va, qb, kb, vb = jnp.split(proj, splits, axis=-1)
        qa = partial_rope(qa.reshape(b, s, A_KV_HEADS, A_GROUP, HEAD_DIM), cos, sin)
        ka = partial_rope(ka.reshape(b, s, A_KV_HEADS, HEAD_DIM), cos, sin)
        va = va.reshape(b, s, A_KV_HEADS, HEAD_DIM)
        oa, _ = banded_attention(qa, ka, va, A_HALF_WINDOW, A_BLOCK,
                                 sink=a_sink[l].reshape(A_KV_HEADS, A_GROUP))
        oa = oa.reshape(b, s, A_Q)
        qb = partial_rope(qb.reshape(b, s, B_HEADS, HEAD_DIM), cos, sin)
        kb = partial_rope(kb.reshape(b, s, B_HEADS, HEAD_DIM), cos, sin)
        vb = vb.reshape(b, s, B_HEADS, HEAD_DIM)
        ob = dilated_attention(qb, kb, vb).reshape(b, s, B_W)
        mixed = jnp.concatenate([rms_norm(oa, g_out_a[l]), rms_norm(ob, g_out_b[l])], axis=-1)
        x = x + jnp.einsum('bse,ed->bsd', mixed, w_out[l])
        h2 = rms_norm(x, g_ffn[l])
        x = x + expert_choice_moe(h2, w_router[l], w_gate[l], w_up[l], w_down[l])
    return rms_norm(x, g_final)
```

```python
import numpy as np
import ml_dtypes
from contextlib import ExitStack
import concourse.bass as bass
import concourse.mybir as mybir
from concourse.bass_utils import run_bass_kernel_spmd

F32 = mybir.dt.float32
I32 = mybir.dt.int32
BF16 = mybir.dt.bfloat16
ALU = mybir.AluOpType
AF = mybir.ActivationFunctionType
AX = mybir.AxisListType

D = 1024
SEQ = 4096
WIN = 3072
OWN = 2048
NT = 24
NO = 16
NE = 16
FF = 2048
CAP = 512
EPS = 1e-6
BIG = 1.0e6
ENGS = ["pe", "act", "dve", "pool", "sp"]


class Sched:
    def __init__(self, nc, n_dma=32, same_engine_wait=("act", "dve", "pool")):
        self.nc = nc
        self.ops = {e: [] for e in ENGS}
        self.cnt = {e: 0 for e in ENGS}
        self.waited = {e: {} for e in ENGS}
        self.last_w = {}
        self.readers = {}
        self.n_dma = n_dma
        self.dma_val = [0] * n_dma
        self.dma_rr = {"pool": 0, "sp": 0, "act": 0}
        self.same = set(same_engine_wait)
        self.all_tokens = {}

    def op(self, eng, fn, reads=(), writes=(), dma=False):
        writes = list(writes) + [r for r in reads if r.startswith("pb")]
        reads = [r for r in reads if not r.startswith("pb")]
        deps = set()
        for r in reads:
            if r in self.last_w:
                deps.add(self.last_w[r])
        for w in writes:
            if w in self.last_w:
                deps.add(self.last_w[w])
            for t in self.readers.get(w, ()):
                deps.add(t)
        if dma:
            half = self.n_dma // 2
            base = 0 if eng == "pool" else half
            k = base + self.dma_rr[eng]
            self.dma_rr[eng] = (self.dma_rr[eng] + 1) % half
            if self.dma_val[k] > 0:
                deps.add((("dma", k), self.dma_val[k]))
            self.dma_val[k] += 16
            token = (("dma", k), self.dma_val[k])
        else:
            self.cnt[eng] += 1
            token = (eng, self.cnt[eng])
        waits = []
        wd = self.waited[eng]
        mx = {}
        for key, val in deps:
            if mx.get(key, 0) < val:
                mx[key] = val
        for key, val in sorted(mx.items(), key=lambda t: str(t[0])):
            if key == eng and eng not in self.same:
                continue
            if wd.get(key, 0) < val:
                wd[key] = val
                waits.append((key, val))
        self.ops[eng].append((waits, fn, token))
        for r in reads:
            self.readers.setdefault(r, []).append(token)
        for w in writes:
            self.last_w[w] = token
            self.readers[w] = []
        self.all_tokens[token[0]] = max(self.all_tokens.get(token[0], 0), token[1])
        return token

    def barrier(self, engs=ENGS):
        toks = dict(self.all_tokens)
        for e in engs:
            waits = []
            wd = self.waited[e]
            for key, val in toks.items():
                if key == e:
                    continue
                if wd.get(key, 0) < val:
                    wd[key] = val
                    waits.append((key, val))
            if waits:
                self.ops[e].append((waits, None, None))

    def emit(self, semaphores):
        nc = self.nc

        def run(ename):
            def body(eng):
                for waits, fn, token in self.ops[ename]:
                    for key, val in waits:
                        eng.wait_ge(semaphores[key], val)
                    if fn is None:
                        continue
                    ins = fn(eng)
                    key = token[0]
                    if isinstance(key, tuple):
                        ins.then_inc(semaphores[key], 16)
                    else:
                        ins.then_inc(semaphores[key], 1)
            return body

        with nc.Block() as block:
            block.tensor(run("pe"))
            block.scalar(run("act"))
            block.vector(run("dve"))
            block.gpsimd(run("pool"))
            block.sync(run("sp"))


import os
SUB = int(os.environ.get('KSUB', '9'))
LOG = []
_DBG = {}
_BCREG = {}


def _bc(eng, val):
    key = (id(eng), val)
    if key not in _BCREG:
        _BCREG[key] = eng.to_reg(val)
    return _BCREG[key]


class Arena:
    def __init__(self, big, nbytes):
        self.big = big
        self.nbytes = nbytes
        self.off = 0

    def alloc(self, shape, dt):
        esz = 2 if dt == BF16 else 4
        n = int(np.prod(shape[1:]))
        nb = (n * esz + 63) // 64 * 64
        o = self.off
        self.off += nb
        LOG.append((o, tuple(shape), str(dt)))
        assert self.off <= self.nbytes, (self.off, self.nbytes)
        v = self.big[:, o // 4:(o + nb) // 4]
        if dt != F32:
            v = v.bitcast(dt)
        v = v[:, 0:n]
        if len(shape) == 3:
            v = v.rearrange("p (a b) -> p a b", a=shape[1])
        elif len(shape) == 4:
            v = v.rearrange("p (a b c) -> p a b c", a=shape[1], b=shape[2])
        return v


def build(n_exp=NE, debug=False, stop_after_phase1=False, level=9):
    nc = bass.Bass("TRN2", target_bir_lowering=False)
    _BCREG.clear()

    def din(name, shape, dt=F32):
        return nc.dram_tensor(name, list(shape), dt, kind="ExternalInput").ap()

    xw = din("xw", [WIN, D])
    cosw = din("cosw", [WIN, 8])
    sinw = din("sinw", [WIN, 8])
    gmix_d = din("g_mix", [1, D])
    gffn_d = din("g_ffn", [1, D])
    gfin_d = din("g_final", [1, D])
    gout_d = din("g_out", [1, D])
    win_d = din("w_in", [D, 2304])
    wout_d = din("w_out", [D, D])
    wr_d = din("w_router", [D, NE])
    wg_d = din("w_gate", [n_exp, D, FF])
    wu_d = din("w_up", [n_exp, D, FF])
    wd_d = din("w_down", [n_exp, FF, D])
    sink_d = din("sinkp", [1, 8])
    par_d = din("par", [1, 2])
    ident_d = din("ident", [128, 128], BF16)
    identf_d = din("identf", [128, 128])
    tri_d = din("tri", [128, 128])
    ones_d = din("ones", [128, 128])
    maskA_d = din("maskA", [128, 3 * 128], BF16)
    maskB_d = din("maskB", [128, 23 * 128], BF16)
    iota_d = din("iota", [1, CAP])
    tokc_d = din("tokc", [128, NO * 3])
    out_d = nc.dram_tensor("out", [OWN, D], F32, kind="ExternalOutput").ap()
    if debug:
        dbg_x1 = nc.dram_tensor("dbg_x1", [OWN, D], F32, kind="ExternalOutput").ap()
        dbg_aff = nc.dram_tensor("dbg_aff", [128, NO * NE], F32, kind="ExternalOutput").ap()
        dbg_thr = nc.dram_tensor("dbg_thr", [128, 2 * NE], F32, kind="ExternalOutput").ap()

    acc_d = nc.dram_tensor("acc", [OWN, D], F32).ap()
    h2_d = nc.dram_tensor("h2buf", [OWN, D], BF16).ap()
    cin_d = nc.dram_tensor("cin", [128, 2 * NO * NE], F32)
    cout_d = nc.dram_tensor("cout", [128, 2 * NO * NE], F32)

    S = Sched(nc)
    with ExitStack() as es:
        SB_BYTES = 196 * 1024
        big = es.enter_context(nc.sbuf_tensor("big", [128, SB_BYTES // 4], F32))
        AR = Arena(big, SB_BYTES)
        pbt = [es.enter_context(nc.psum_tensor(f"pb{i}", [128, 512], F32)) for i in range(8)]
        pb = [t[:, :] for t in pbt]
        pbh = [t[:, :].bitcast(BF16) for t in pbt]
        sems = {e: es.enter_context(nc.semaphore("sem_" + e)) for e in ENGS}
        for k in range(S.n_dma):
            sems[("dma", k)] = es.enter_context(nc.semaphore(f"dsem{k}"))

        ident = AR.alloc([128, 128], BF16)
        identf = AR.alloc([128, 128], F32)
        tri = AR.alloc([128, 128], F32)
        ones = AR.alloc([128, 128], F32)
        iota = AR.alloc([128, CAP], F32)
        tokc = AR.alloc([128, NO, 3], F32)
        par = AR.alloc([128, 2], F32)
        gfin = AR.alloc([128, D], F32)
        gffn = AR.alloc([128, D], F32)
        aff = AR.alloc([128, NO, NE], F32)
        small = AR.alloc([128, 64], F32)
        mark_persist = AR.off

        def ld(eng, dst, src, name, rd=()):
            S.op(eng, lambda e: e.dma_start(out=dst, in_=src), reads=rd, writes=[name], dma=True)

        ld("sp", ident, ident_d, "ident")
        ld("sp", identf, identf_d, "identf")
        ld("sp", tri, tri_d, "tri")
        ld("sp", ones, ones_d, "ones")
        ld("sp", iota, iota_d.partition_broadcast(128), "iota")
        ld("sp", tokc, tokc_d.rearrange("p (a b) -> p a b", a=NO), "tokc")
        ld("sp", par, par_d.partition_broadcast(128), "par")
        ld("sp", gfin, gfin_d.partition_broadcast(128), "gfin")
        ld("sp", gffn, gffn_d.partition_broadcast(128), "gffn")

        esink = AR.alloc([128, 8], F32)
        maskA = AR.alloc([128, 3 * 128], BF16)
        maskB = AR.alloc([128, 23 * 128], BF16)
        qaT = AR.alloc([128, 4, OWN], BF16)
        kaT = AR.alloc([128, 2, 17 * 128], BF16)
        qbT = AR.alloc([128, 4, OWN], BF16)
        kbT = AR.alloc([128, 4, WIN], BF16)
        vA = AR.alloc([128, 17, 2 * 65], BF16)
        vB = AR.alloc([128, NT, 8 * 65], BF16)
        xbuf = [AR.alloc([128, D], F32) for _ in range(2)]
        junk = AR.alloc([128, D], F32)
        hT = [AR.alloc([128, 8, 128], BF16) for _ in range(2)]
        mark_1a = AR.off
        gmix = AR.alloc([128, D], F32)
        cos_t = AR.alloc([128, NT, 8], F32)
        sin_t = AR.alloc([128, NT, 8], F32)
        win = AR.alloc([128, 8, 2304], BF16)
        xn = [AR.alloc([128, D], BF16) for _ in range(2)]
        qk_tm = [AR.alloc([128, 14 * 128], BF16) for _ in range(2)]
        tmpA = AR.alloc([128, 8 * 16], F32)
        tmpB = AR.alloc([128, 8 * 16], F32)
        print("phase1 sbuf bytes/partition:", AR.off)

        ld("sp", gmix, gmix_d.partition_broadcast(128), "gmix")
        ld("sp", cos_t, cosw.rearrange("(j p) d -> p j d", p=128), "cos")
        ld("sp", sin_t, sinw.rearrange("(j p) d -> p j d", p=128), "sin")
        ld("sp", esink, sink_d.partition_broadcast(128), "esink")
        ld("sp", maskA, maskA_d, "maskA")
        ld("sp", maskB, maskB_d, "maskB")
        win_src = win_d.rearrange("(k p) c -> p k c", p=128)
        for k0 in range(0, 8, 2):
            S.op("pool", lambda e, k0=k0: e.dma_start(out=win[:, k0:k0 + 2, :], in_=win_src[:, k0:k0 + 2, :]),
                 writes=[f"win{k0}"], dma=True)
        WIN_R = [f"win{k0}" for k0 in range(0, 8, 2)]
        S.op("act", lambda e: e.activation(out=esink, in_=esink, func=AF.Exp), reads=["esink"], writes=["esink"])
        vA4 = vA.rearrange("p j (h d) -> p j h d", h=2)
        vB4 = vB.rearrange("p j (h d) -> p j h d", h=8)
        S.op("pool", lambda e: e.memset(vA4[:, :, :, 64:65], 1.0), writes=["vA_ones"])
        S.op("pool", lambda e: e.memset(vB4[:, :, :, 64:65], 1.0), writes=["vB_ones"])

        psel = [0]

        def rope(j, pg, c0, H, dst, dups):
            if os.environ.get("KROPE", "1") == "0":
                return
            v = pg[:, c0:c0 + H * 64].rearrange("p (h d) -> p h d", h=H)
            x1 = v[:, :, 0:8]
            x2 = v[:, :, 8:16]
            cb = cos_t[:, j, :].unsqueeze(1).to_broadcast([128, H, 8])
            sb_ = sin_t[:, j, :].unsqueeze(1).to_broadcast([128, H, 8])
            t1c = tmpA[:, 0:H * 8].rearrange("p (h d) -> p h d", h=H)
            t2c = tmpA[:, 64:64 + H * 8].rearrange("p (h d) -> p h d", h=H)
            t1s = tmpB[:, 0:H * 8].rearrange("p (h d) -> p h d", h=H)
            t2s = tmpB[:, 64:64 + H * 8].rearrange("p (h d) -> p h d", h=H)
            pgn = dups["pg"]
            S.op("dve", lambda e: e.tensor_tensor(out=t1c, in0=x1, in1=cb, op=ALU.mult), reads=[pgn, "cos"], writes=["t1c"])
            S.op("dve", lambda e: e.tensor_tensor(out=t2c, in0=x2, in1=cb, op=ALU.mult), reads=[pgn, "cos"], writes=["t2c"])
            S.op("dve", lambda e: e.tensor_tensor(out=t1s, in0=x1, in1=sb_, op=ALU.mult), reads=[pgn, "sin"], writes=["t1s"])
            S.op("dve", lambda e: e.tensor_tensor(out=t2s, in0=x2, in1=sb_, op=ALU.mult), reads=[pgn, "sin"], writes=["t2s"])
            for dv, dn in dst:
                S.op("dve", lambda e, dv=dv: e.tensor_tensor(out=dv[:, :, 0:8], in0=t1c, in1=t2s, op=ALU.subtract),
                     reads=["t1c", "t2s"], writes=[dn + "_a"])
                S.op("dve", lambda e, dv=dv: e.tensor_tensor(out=dv[:, :, 8:16], in0=t2c, in1=t1s, op=ALU.add),
                     reads=["t2c", "t1s"], writes=[dn + "_b"])
                if os.environ.get("KROPE", "3") == "2":
                    continue
                S.op("act", lambda e, dv=dv: e.copy(out=dv[:, :, 16:64], in_=v[:, :, 16:64]), reads=[pgn], writes=[dn + "_c"])

        def proj_group(j, hTj, c0, ncols, bank):
            for kc in range(8):
                S.op("pe", lambda e, kc=kc: e.matmul(pb[bank][:, 0:ncols], lhsT=hTj[:, kc, :], rhs=win[:, kc, c0:c0 + ncols],
                                                     start=(kc == 0), stop=(kc == 7)),
                     reads=[f"hT{j % 2}"] + WIN_R, writes=[f"pb{bank}"])

        for j in range(NT if level >= 1 else 0):
            b = j % 2
            own = j < NO
            xt = xbuf[b]
            S.op("sp", lambda e, j=j, xt=xt: e.dma_start(out=xt, in_=xw[j * 128:(j + 1) * 128, :]), writes=[f"x{b}"], dma=True)
            S.op("act", lambda e, xt=xt: e.activation(out=junk, in_=xt, func=AF.Square, scale=1.0 / 32, accum_out=small[:, 0:1]),
                 reads=[f"x{b}"], writes=["junk", "ss"])
            S.op("act", lambda e: e.activation(out=small[:, 1:2], in_=small[:, 0:1], func=AF.Sqrt, bias=EPS), reads=["ss"], writes=["rs"])
            S.op("dve", lambda e: e.reciprocal(out=small[:, 2:3], in_=small[:, 1:2]), reads=["rs"], writes=["rstd"])
            S.op("dve", lambda e, xt=xt, b=b: e.scalar_tensor_tensor(out=xn[b], in0=xt, scalar=small[:, 2:3], in1=gmix, op0=ALU.mult, op1=ALU.mult),
                 reads=[f"x{b}", "rstd", "gmix"], writes=[f"xn{b}"])
            pT = pbh[5].rearrange("p (k c) -> p k c", k=8)
            for kc in range(8):
                S.op("pe", lambda e, kc=kc, b=b: e.transpose(out=pT[:, kc, :], in_=xn[b][:, kc * 128:(kc + 1) * 128], identity=ident),
                     reads=[f"xn{b}", "ident"], writes=["pb5"])
            S.op("act", lambda e, b=b: e.copy(out=hT[b], in_=pT), reads=["pb5"], writes=[f"hT{b}"])
            if SUB < 2:
                continue
            qk = qk_tm[b]
            qkn = f"qk{b}"
            if own:
                bank = psel[0] % 3; psel[0] += 1
                proj_group(j, hT[b], 0, 512, bank)
                rope(j, pb[bank], 0, 8, [(qk[:, 0:512].rearrange("p (h d) -> p h d", h=8), qkn + "_qa")], {"pg": f"pb{bank}"})
            if j <= NO:
                bank = psel[0] % 3; psel[0] += 1
                proj_group(j, hT[b], 512, 256, bank)
                kd = qk[:, 512:768].rearrange("p (h t d) -> p h t d", h=2, t=2)
                rope(j, pb[bank], 0, 2, [(kd[:, :, 0, :], qkn + "_ka0"), (kd[:, :, 1, :], qkn + "_ka1")], {"pg": f"pb{bank}"})
                S.op("act", lambda e, j=j, bank=bank: e.copy(out=vA4[:, j, :, 0:64], in_=pb[bank][:, 128:256].rearrange("p (h d) -> p h d", h=2)),
                     reads=[f"pb{bank}"], writes=[f"vA{j}"])
            if own:
                bank = psel[0] % 3; psel[0] += 1
                proj_group(j, hT[b], 768, 512, bank)
                rope(j, pb[bank], 0, 8, [(qk[:, 768:1280].rearrange("p (h d) -> p h d", h=8), qkn + "_qb")], {"pg": f"pb{bank}"})
            bank = psel[0] % 3; psel[0] += 1
            proj_group(j, hT[b], 1280, 512, bank)
            rope(j, pb[bank], 0, 8, [(qk[:, 1280:1792].rearrange("p (h d) -> p h d", h=8), qkn + "_kb")], {"pg": f"pb{bank}"})
            bank = psel[0] % 3; psel[0] += 1
            proj_group(j, hT[b], 1792, 512, bank)
            S.op("dve", lambda e, j=j, bank=bank: e.tensor_copy(out=vB4[:, j, :, 0:64], in_=pb[bank].rearrange("p (h d) -> p h d", h=8)),
                 reads=[f"pb{bank}"], writes=[f"vB{j}"])
            if SUB < 3:
                continue
            qT3 = pbh[3].rearrange("p (k c) -> p k c", k=8)
            qT4 = pbh[4].rearrange("p (k c) -> p k c", k=8)

            def tr(dst_bank_view, slot, ft, rd, bankname, qk=qk):
                S.op("pe", lambda e: e.transpose(out=dst_bank_view[:, slot, :], in_=qk[:, ft * 128:(ft + 1) * 128], identity=ident),
                     reads=rd + ["ident"], writes=[bankname])
            rd_qa = [qkn + "_qa_a", qkn + "_qa_b", qkn + "_qa_c"]
            rd_ka = [qkn + s for s in ("_ka0_a", "_ka0_b", "_ka0_c", "_ka1_a", "_ka1_b", "_ka1_c")]
            rd_qb = [qkn + "_qb_a", qkn + "_qb_b", qkn + "_qb_c"]
            rd_kb = [qkn + "_kb_a", qkn + "_kb_b", qkn + "_kb_c"]
            if own:
                for t in range(4):
                    tr(qT3, t, t, rd_qa, "pb3")
            if j <= NO:
                for t in range(2):
                    tr(qT3, 4 + t, 4 + t, rd_ka, "pb3")
            if own:
                S.op("act", lambda e, j=j: e.copy(out=qaT[:, :, j * 128:(j + 1) * 128], in_=qT3[:, 0:4, :]), reads=["pb3"], writes=[f"qaT{j}"])
            if j <= NO:
                S.op("dve", lambda e, j=j: e.tensor_copy(out=kaT[:, :, j * 128:(j + 1) * 128], in_=qT3[:, 4:6, :]), reads=["pb3"], writes=[f"kaT{j}"])
            if own:
                for t in range(4):
                    tr(qT4, t, 6 + t, rd_qb, "pb4")
            for t in range(4):
                tr(qT4, 4 + t, 10 + t, rd_kb, "pb4")
            if own:
                S.op("act", lambda e, j=j: e.copy(out=qbT[:, :, j * 128:(j + 1) * 128], in_=qT4[:, 0:4, :]), reads=["pb4"], writes=[f"qbT{j}"])
            S.op("dve", lambda e, j=j: e.tensor_copy(out=kbT[:, :, j * 128:(j + 1) * 128], in_=qT4[:, 4:8, :]), reads=["pb4"], writes=[f"kbT{j}"])

        S.barrier()
        AR.off = mark_1a
        gout = AR.alloc([128, D], F32)
        wout = AR.alloc([128, 8, D], BF16)
        wr = AR.alloc([128, 8, NE], F32)
        Pb = [AR.alloc([128, 512], BF16) for _ in range(3)]
        o_all = AR.alloc([128, 4, D], F32)
        mixed = [AR.alloc([128, D], BF16)] * 2
        x1t = [AR.alloc([128, D], F32)] * 2
        h2f = [AR.alloc([128, D], F32)] * 2
        h2b = [AR.alloc([128, D], BF16)] * 2
        h2T = AR.alloc([128, 8, 128], F32)
        print("phase1b sbuf bytes/partition:", AR.off)
        ld("sp", gout, gout_d.partition_broadcast(128), "gout")
        ld("sp", wr, wr_d.rearrange("(k p) c -> p k c", p=128), "wr")
        S.op("pool", lambda e: e.dma_start(out=wout, in_=wout_d.rearrange("(k p) c -> p k c", p=128)),
             writes=["wout"], dma=True)
        sbank = [0]
        obank = [0]
        pbuf = [0]

        def attention_group(g):
            for kvh in range(2):
                for qi in range(4):
                    qb_ = 4 * g + qi
                    kbs = [kb for kb in (qb_ - 1, qb_, qb_ + 1) if 0 <= kb <= 16]
                    OB = [3, 4, 6, 7]
                    OBN = [f"pb{q}" for q in OB]
                    for kb in kbs:
                        pi = pbuf[0] % 3; pbuf[0] += 1
                        P = Pb[pi]
                        for half in range(2):
                            sbk = sbank[0] % 3; sbank[0] += 1
                            r0 = half * 64
                            S.op("pe", lambda e, half=half, r0=r0, kb=kb, sbk=sbk, qb_=qb_, kvh=kvh: e.matmul(
                                pb[sbk][:, 0:256].rearrange("p (a c) -> p a c", a=2),
                                lhsT=kaT[r0:r0 + 64, kvh, kb * 128:(kb + 1) * 128],
                                rhs=qaT[r0:r0 + 64, 2 * kvh:2 * kvh + 2, qb_ * 128:(qb_ + 1) * 128],
                                start=True, stop=True),
                                reads=[f"kaT{kb}", f"qaT{qb_}"], writes=[f"pb{sbk}"])
                            S.op("act", lambda e, sbk=sbk, P=P, half=half: e.activation(out=P[:, half * 256:(half + 1) * 256], in_=pb[sbk][:, 0:256], func=AF.Exp, scale=0.125),
                                 reads=[f"pb{sbk}"], writes=[f"P{pi}"])
                        if kb != qb_:
                            blk = 0 if kb < qb_ else 2
                            S.op("dve", lambda e, P=P, blk=blk: e.tensor_tensor(
                                out=P.rearrange("p (h c) -> p h c", h=4), in0=P.rearrange("p (h c) -> p h c", h=4),
                                in1=maskA[:, blk * 128:(blk + 1) * 128].unsqueeze(1).to_broadcast([128, 4, 128]), op=ALU.mult),
                                reads=[f"P{pi}", "maskA"], writes=[f"P{pi}"])
                        for hh in range(4):
                            S.op("pe", lambda e, hh=hh, P=P, kb=kb, kvh=kvh, st=(kb == kbs[0]), sp_=(kb == kbs[-1]): e.matmul(
                                pb[OB[hh]][:, 0:65], lhsT=P[:, hh * 128:(hh + 1) * 128], rhs=vA4[:, kb, kvh, :],
                                start=st, stop=sp_),
                                reads=[f"P{pi}", f"vA{kb}", "vA_ones"], writes=[OBN[hh]])
                    denA = small[:, 8:12]
                    HD = [0, 2, 1, 3]
                    for hh in range(4):
                        S.op("dve", lambda e, hh=hh, kvh=kvh: e.tensor_tensor(out=denA[:, hh:hh + 1], in0=pb[OB[hh]][:, 64:65],
                                                                              in1=esink[:, kvh * 4 + hh:kvh * 4 + hh + 1], op=ALU.add),
                             reads=[OBN[hh], "esink"], writes=[f"den{hh}"])
                        S.op("dve", lambda e, hh=hh: e.reciprocal(out=denA[:, hh:hh + 1], in_=denA[:, hh:hh + 1]), reads=[f"den{hh}"], writes=[f"den{hh}"])
                        hd = kvh * 4 + HD[hh]
                        S.op("dve", lambda e, hh=hh, hd=hd, qi=qi: e.tensor_scalar(
                            out=o_all[:, qi, hd * 64:(hd + 1) * 64], in0=pb[OB[hh]][:, 0:64], scalar1=denA[:, hh:hh + 1], scalar2=None, op0=ALU.mult),
                            reads=[OBN[hh], f"den{hh}"], writes=[f"oall{qi}A{kvh}_{hh}"])
            for h in range(8):
                ft = h // 2
                r0 = (h % 2) * 64
                OB = [3, 4, 6, 7]
                OBN = [f"pb{q}" for q in OB]
                for kb in range(max(0, 4 * g - 8), min(NT, 4 * g + 12)):
                    qlo = max(4 * g, kb - 8)
                    qhi = min(4 * g + 3, kb + 8)
                    if qhi < qlo:
                        continue
                    ncol = (qhi - qlo + 1) * 128
                    sbk = sbank[0] % 3; sbank[0] += 1
                    pi = pbuf[0] % 3; pbuf[0] += 1
                    P = Pb[pi]
                    S.op("pe", lambda e, sbk=sbk, ncol=ncol, kb=kb, qlo=qlo, ft=ft, r0=r0: e.matmul(
                        pb[sbk][:, 0:ncol], lhsT=kbT[r0:r0 + 64, ft, kb * 128:(kb + 1) * 128],
                        rhs=qbT[r0:r0 + 64, ft, qlo * 128:qlo * 128 + ncol], start=True, stop=True),
                        reads=[f"kbT{kb}"] + [f"qbT{q}" for q in range(qlo, qhi + 1)], writes=[f"pb{sbk}"])
                    S.op("act", lambda e, sbk=sbk, P=P, ncol=ncol: e.activation(out=P[:, 0:ncol], in_=pb[sbk][:, 0:ncol], func=AF.Exp, scale=0.125),
                         reads=[f"pb{sbk}"], writes=[f"P{pi}"])
                    m0 = (qlo - kb + 11) * 128
                    S.op("dve", lambda e, P=P, ncol=ncol, m0=m0: e.tensor_tensor(out=P[:, 0:ncol], in0=P[:, 0:ncol], in1=maskB[:, m0:m0 + ncol], op=ALU.mult),
                         reads=[f"P{pi}", "maskB"], writes=[f"P{pi}"])
                    for qb_ in range(qlo, qhi + 1):
                        first = max(0, qb_ - 8)
                        last = min(NT - 1, qb_ + 8)
                        S.op("pe", lambda e, qb_=qb_, P=P, qlo=qlo, kb=kb, h=h, first=first, last=last: e.matmul(
                            pb[OB[qb_ - 4 * g]][:, 0:65], lhsT=P[:, (qb_ - qlo) * 128:(qb_ - qlo + 1) * 128], rhs=vB4[:, kb, h, :],
                            start=(kb == first), stop=(kb == last)),
                            reads=[f"P{pi}", f"vB{kb}", "vB_ones"], writes=[OBN[qb_ - 4 * g]])
                denB = small[:, 12:16]
                for qi in range(4):
                    S.op("dve", lambda e, qi=qi: e.reciprocal(out=denB[:, qi:qi + 1], in_=pb[OB[qi]][:, 64:65]), reads=[OBN[qi]], writes=[f"denB{qi}"])
                    S.op("dve", lambda e, qi=qi, h=h: e.tensor_scalar(
                        out=o_all[:, qi, 512 + h * 64:512 + (h + 1) * 64], in0=pb[OB[qi]][:, 0:64], scalar1=denB[:, qi:qi + 1], scalar2=None, op0=ALU.mult),
                        reads=[OBN[qi], f"denB{qi}"], writes=[f"oallB{h}_{qi}"])

        def finish_tile(g, qi):
            j = 4 * g + qi
            b = j % 2
            oa_r = [f"oall{qi}A{k_}_{h_}" for k_ in range(2) for h_ in range(4)]
            ob_r = [f"oallB{h}_{qi}" for h in range(8)]
            for gi, rd in ((0, oa_r), (1, ob_r)):
                S.op("act", lambda e, gi=gi: e.activation(out=junk[:, 0:512], in_=o_all[:, qi, gi * 512:(gi + 1) * 512], func=AF.Square,
                                                          scale=float(1.0 / np.sqrt(512.0)), accum_out=small[:, 16 + gi:17 + gi]),
                     reads=rd, writes=["junk", f"gss{gi}"])
                S.op("act", lambda e, gi=gi: e.activation(out=small[:, 18 + gi:19 + gi], in_=small[:, 16 + gi:17 + gi], func=AF.Sqrt, bias=EPS),
                     reads=[f"gss{gi}"], writes=[f"grs{gi}"])
                S.op("dve", lambda e, gi=gi: e.reciprocal(out=small[:, 20 + gi:21 + gi], in_=small[:, 18 + gi:19 + gi]), reads=[f"grs{gi}"], writes=[f"grstd{gi}"])
                S.op("dve", lambda e, gi=gi, b=b: e.scalar_tensor_tensor(
                    out=mixed[b][:, gi * 512:(gi + 1) * 512], in0=o_all[:, qi, gi * 512:(gi + 1) * 512], scalar=small[:, 20 + gi:21 + gi],
                    in1=gout[:, gi * 512:(gi + 1) * 512], op0=ALU.mult, op1=ALU.mult),
                    reads=rd + [f"grstd{gi}", "gout"], writes=[f"mixed{0}_{gi}"])
            pT = pbh[5].rearrange("p (k c) -> p k c", k=8)
            for kc in range(8):
                S.op("pe", lambda e, kc=kc, b=b: e.transpose(out=pT[:, kc, :], in_=mixed[b][:, kc * 128:(kc + 1) * 128], identity=ident),
                     reads=[f"mixed{0}_0", f"mixed{0}_1", "ident"], writes=["pb5"])
            S.op("act", lambda e, b=b: e.copy(out=hT[b], in_=pT), reads=["pb5"], writes=[f"hT{b}"])
            xt = xbuf[b]
            S.op("sp", lambda e, j=j, xt=xt: e.dma_start(out=xt, in_=xw[j * 128:(j + 1) * 128, :]), writes=[f"x{b}"], dma=True)
            for hf in range(2):
                bank = 6 + hf
                for kc in range(8):
                    S.op("pe", lambda e, kc=kc, b=b, hf=hf, bank=bank: e.matmul(
                        pb[bank], lhsT=hT[b][:, kc, :], rhs=wout[:, kc, hf * 512:(hf + 1) * 512], start=(kc == 0), stop=(kc == 7)),
                        reads=[f"hT{b}", "wout"], writes=[f"pb{bank}"])
                S.op("dve", lambda e, b=b, hf=hf, bank=bank, xt=xt: e.tensor_tensor(
                    out=x1t[b][:, hf * 512:(hf + 1) * 512], in0=pb[bank], in1=xt[:, hf * 512:(hf + 1) * 512], op=ALU.add),
                    reads=[f"pb{bank}", f"x{b}"], writes=[f"x1t{0}_{hf}"])
            x1r = [f"x1t{0}_0", f"x1t{0}_1"]
            S.op("sp", lambda e, j=j, b=b: e.dma_start(out=acc_d[j * 128:(j + 1) * 128, :], in_=x1t[b]), reads=x1r, writes=["acc"], dma=True)
            if debug:
                S.op("sp", lambda e, j=j, b=b: e.dma_start(out=dbg_x1[j * 128:(j + 1) * 128, :], in_=x1t[b]), reads=x1r, dma=True)
            S.op("act", lambda e, b=b: e.activation(out=junk, in_=x1t[b], func=AF.Square, scale=1.0 / 32, accum_out=small[:, 24:25]),
                 reads=x1r, writes=["junk", "ss2"])
            S.op("act", lambda e: e.activation(out=small[:, 25:26], in_=small[:, 24:25], func=AF.Sqrt, bias=EPS), reads=["ss2"], writes=["rs2"])
            S.op("dve", lambda e: e.reciprocal(out=small[:, 26:27], in_=small[:, 25:26]), reads=["rs2"], writes=["rstd2"])
            S.op("dve", lambda e, b=b: e.scalar_tensor_tensor(out=h2f[b], in0=x1t[b], scalar=small[:, 26:27], in1=gffn, op0=ALU.mult, op1=ALU.mult),
                 reads=x1r + ["rstd2", "gffn"], writes=[f"h2f{0}"])
            S.op("act", lambda e, b=b: e.copy(out=h2b[b], in_=h2f[b]), reads=[f"h2f{0}"], writes=[f"h2b{0}"])
            S.op("sp", lambda e, j=j, b=b: e.dma_start(out=h2_d[j * 128:(j + 1) * 128, :], in_=h2b[b]), reads=[f"h2b{0}"], writes=["h2d"], dma=True)
            for kc in range(8):
                bank = 3 + kc // 4
                S.op("pe", lambda e, kc=kc, b=b, bank=bank: e.transpose(
                    out=pb[bank][:, (kc % 4) * 128:(kc % 4 + 1) * 128], in_=h2f[b][:, kc * 128:(kc + 1) * 128], identity=identf),
                    reads=[f"h2f{0}", "identf"], writes=[f"pb{bank}"])
            S.op("act", lambda e: e.copy(out=h2T[:, 0:4, :], in_=pb[3].rearrange("p (k c) -> p k c", k=4)), reads=["pb3"], writes=["h2Ta"])
            S.op("dve", lambda e: e.tensor_copy(out=h2T[:, 4:8, :], in_=pb[4].rearrange("p (k c) -> p k c", k=4)), reads=["pb4"], writes=["h2Tb"])
            for kc in range(8):
                S.op("pe", lambda e, kc=kc: e.matmul(pb[5][:, 0:NE], lhsT=h2T[:, kc, :], rhs=wr[:, kc, :], start=(kc == 0), stop=(kc == 7)),
                     reads=["h2Ta", "h2Tb", "wr"], writes=["pb5"])
            S.op("dve", lambda e: e.reduce_max(out=small[:, 28:29], in_=pb[5][:, 0:NE], axis=AX.X), reads=["pb5"], writes=["lmax"])
            S.op("dve", lambda e: e.tensor_scalar(out=small[:, 29:30], in0=small[:, 28:29], scalar1=-1.0, scalar2=None, op0=ALU.mult),
                 reads=["lmax"], writes=["nlmax"])
            S.op("act", lambda e, j=j: e.activation(out=aff[:, j, :], in_=pb[5][:, 0:NE], func=AF.Exp, bias=small[:, 29:30], scale=1.0,
                                                    accum_out=small[:, 30:31]),
                 reads=["pb5", "nlmax"], writes=[f"aff{j}", "lsum"])
            S.op("dve", lambda e: e.reciprocal(out=small[:, 31:32], in_=small[:, 30:31]), reads=["lsum"], writes=["rlsum"])
            S.op("dve", lambda e, j=j: e.tensor_scalar(out=aff[:, j, :], in0=aff[:, j, :], scalar1=small[:, 31:32], scalar2=None, op0=ALU.mult),
                 reads=[f"aff{j}", "rlsum"], writes=[f"aff{j}"])

        for g in range(4 if level >= 2 else 0):
            attention_group(g)
            for qi in range(4):
                finish_tile(g, qi)

        AFF_R = [f"aff{j}" for j in range(NO)]
        if debug and level == 0:
            aff_in = din("aff_in", [128, NO * NE])
            S.op("sp", lambda e: e.dma_start(out=aff.rearrange("p a b -> p (a b)"), in_=aff_in), writes=AFF_R, dma=True)
        if debug and level >= 2:
            S.op("sp", lambda e: e.dma_start(out=dbg_aff, in_=aff.rearrange("p a b -> p (a b)")), reads=AFF_R, dma=True)

        if not stop_after_phase1:
            S.barrier()
            AR.off = mark_persist
            afull = AR.alloc([128, 2, NO * NE], F32)
            cmpb = AR.alloc([128, 2 * NO, NE], F32)
            lo = AR.alloc([128, NE], F32)
            hi = AR.alloc([128, NE], F32)
            mid = AR.alloc([128, NE], F32)
            cntp = AR.alloc([128, NE], F32)
            sel = AR.alloc([128, NE], F32)
            tmpn = AR.alloc([128, NE], F32)
            msk = AR.alloc([128, NO, NE], F32)
            dest = AR.alloc([128, NO, NE], F32)
            offs = AR.alloc([128, NO, NE], F32)
            vals = AR.alloc([128, NO, 4], F32)
            selb = [AR.alloc([128, CAP], F32) for _ in range(2)]
            idxf = AR.alloc([128, 4, 4], F32)
            idxi = [[AR.alloc([128, 1], I32) for _c in range(4)] for _ in range(2)]
            gate = [AR.alloc([128, 4], F32) for _ in range(2)]
            xg = AR.alloc([128, 4, D], BF16)
            xeT = [AR.alloc([128, 8, CAP], BF16) for _ in range(2)]
            hTm = AR.alloc([128, 16, CAP], BF16)
            sa = [AR.alloc([128, CAP], F32) for _ in range(2)]
            yo = [AR.alloc([128, D], F32) for _ in range(2)]
            wgb = [AR.alloc([128, 8, 1024], BF16) for _ in range(2)]
            wub = [AR.alloc([128, 8, 1024], BF16) for _ in range(2)]
            wdb = [AR.alloc([128, 8, D], BF16) for _ in range(3)]
            print("phase2 sbuf bytes/partition:", AR.off)

            aff2 = aff.rearrange("p a b -> p (a b)")
            S.op("dve", lambda e: e.tensor_scalar(out=afull[:, 0, :], in0=aff2, scalar1=par[:, 0:1], scalar2=None, op0=ALU.mult),
                 reads=AFF_R + ["par"], writes=["afull0"])
            S.op("dve", lambda e: e.tensor_scalar(out=afull[:, 1, :], in0=aff2, scalar1=par[:, 1:2], scalar2=None, op0=ALU.mult),
                 reads=AFF_R + ["par"], writes=["afull1"])
            S.op("pool", lambda e: e.dma_start(out=cin_d[:, :], in_=afull.rearrange("p a b -> p (a b)")), reads=["afull0", "afull1"], writes=["cin"], dma=True)

            def cc(e):
                return e.collective_compute("AllReduce", ALU.add, replica_groups=[[0, 1], [2, 3], [4, 5], [6, 7]],
                                            ins=[cin_d.ap().opt()], outs=[cout_d.ap().opt()])
            S.cnt["cc"] = 0
            S.ops["pool"].append(([(k, v) for k, v in [S.last_w["cin"]]], None, None))
            S.waited["pool"][S.last_w["cin"][0]] = max(S.waited["pool"].get(S.last_w["cin"][0], 0), S.last_w["cin"][1])
            S.ops["pool"].append(([], cc, ("cc", 1)))
            S.last_w["cout"] = ("cc", 1)
            S.readers["cout"] = []
            S.all_tokens["cc"] = 1
            S.op("pool", lambda e: e.dma_start(out=afull.rearrange("p a b -> p (a b)"), in_=cout_d[:, :]), reads=["cout"], writes=["afull0", "afull1"], dma=True)
            AF_R = ["afull0", "afull1"]
            S.op("dve", lambda e: e.memset(lo, 0.0), writes=["lo"])
            S.op("dve", lambda e: e.memset(hi, 1.0), writes=["hi"])
            af3 = afull.rearrange("p a (j e) -> p (a j) e", e=NE)
            for it in range(26):
                S.op("dve", lambda e: e.tensor_tensor(out=mid, in0=lo, in1=hi, op=ALU.add), reads=["lo", "hi"], writes=["mid"])
                S.op("dve", lambda e: e.tensor_scalar(out=mid, in0=mid, scalar1=0.5, scalar2=None, op0=ALU.mult), reads=["mid"], writes=["mid"])
                S.op("dve", lambda e: e.tensor_tensor(out=cmpb, in0=af3, in1=mid.unsqueeze(1).to_broadcast([128, 2 * NO, NE]), op=ALU.is_ge),
                     reads=AF_R + ["mid"], writes=["cmpb"])
                S.op("dve", lambda e: e.tensor_reduce(out=cntp, in_=cmpb.rearrange("p s e -> p e s"), axis=AX.X, op=ALU.add), reads=["cmpb"], writes=["cntp"])
                S.op("pe", lambda e: e.matmul(pb[5][:, 0:NE], lhsT=ones, rhs=cntp, start=True, stop=True), reads=["ones", "cntp"], writes=["pb5"])
                S.op("dve", lambda e: e.tensor_scalar(out=sel, in0=pb[5][:, 0:NE], scalar1=511.5, scalar2=None, op0=ALU.is_ge), reads=["pb5"], writes=["sel"])
                S.op("dve", lambda e: e.tensor_tensor(out=tmpn, in0=mid, in1=lo, op=ALU.subtract), reads=["mid", "lo"], writes=["tmpn"])
                S.op("dve", lambda e: e.tensor_tensor(out=tmpn, in0=tmpn, in1=sel, op=ALU.mult), reads=["tmpn", "sel"], writes=["tmpn"])
                S.op("dve", lambda e: e.tensor_tensor(out=lo, in0=lo, in1=tmpn, op=ALU.add), reads=["lo", "tmpn"], writes=["lo"])
                S.op("dve", lambda e: e.tensor_tensor(out=tmpn, in0=hi, in1=mid, op=ALU.subtract), reads=["hi", "mid"], writes=["tmpn"])
                S.op("dve", lambda e: e.tensor_tensor(out=tmpn, in0=tmpn, in1=sel, op=ALU.mult), reads=["tmpn", "sel"], writes=["tmpn"])
                S.op("dve", lambda e: e.tensor_tensor(out=hi, in0=mid, in1=tmpn, op=ALU.add), reads=["mid", "tmpn"], writes=["hi"])
            if debug:
                S.op("sp", lambda e: e.dma_start(out=dbg_thr[:, 0:NE], in_=lo), reads=["lo"], dma=True)
                S.op("sp", lambda e: e.dma_start(out=dbg_thr[:, NE:2 * NE], in_=hi), reads=["hi"], dma=True)
            S.op("dve", lambda e: e.tensor_tensor(out=msk, in0=aff, in1=lo.unsqueeze(1).to_broadcast([128, NO, NE]), op=ALU.is_ge),
                 reads=AFF_R + ["lo"], writes=["msk"])
            mk2 = msk.rearrange("p a b -> p (a b)")
            S.op("pe", lambda e: e.matmul(pb[3][:, 0:NO * NE], lhsT=tri, rhs=mk2, start=True, stop=True), reads=["tri", "msk"], writes=["pb3"])
            S.op("pe", lambda e: e.matmul(pb[4][:, 0:NO * NE], lhsT=ones, rhs=mk2, start=True, stop=True), reads=["ones", "msk"], writes=["pb4"])
            tot = pb[4][:, 0:NO * NE].rearrange("p (a b) -> p a b", a=NO)
            S.op("dve", lambda e: e.memset(offs[:, 0, :], 0.0), writes=["offs"])
            for j in range(1, NO):
                S.op("dve", lambda e, j=j: e.tensor_tensor(out=offs[:, j, :], in0=offs[:, j - 1, :], in1=tot[:, j - 1, :], op=ALU.add),
                     reads=["offs", "pb4"], writes=["offs"])
            S.op("dve", lambda e: e.tensor_tensor(out=dest, in0=offs, in1=pb[3][:, 0:NO * NE].rearrange("p (a b) -> p a b", a=NO), op=ALU.add),
                 reads=["offs", "pb3"], writes=["dest"])
            S.op("dve", lambda e: e.tensor_scalar(out=msk, in0=msk, scalar1=-BIG, scalar2=BIG, op0=ALU.mult, op1=ALU.add), reads=["msk"], writes=["msk"])
            S.op("dve", lambda e: e.tensor_tensor(out=dest, in0=dest, in1=msk, op=ALU.add), reads=["dest", "msk"], writes=["dest"])
            S.op("dve", lambda e: e.tensor_copy(out=vals[:, :, 0:3], in_=tokc), reads=["tokc"], writes=["vals_c"])
            S.op("pool", lambda e: e.memset(xg, 0.0), writes=["xg0", "xg1", "xg2", "xg3"])

            wg_src = [wg_d[x].rearrange("(k p) f -> p k f", p=128) for x in range(n_exp)]
            wu_src = [wu_d[x].rearrange("(k p) f -> p k f", p=128) for x in range(n_exp)]
            wd_src = [wd_d[x].rearrange("(k p) c -> p k c", p=128) for x in range(n_exp)]

            def load_weights(x):
                for hf in range(2):
                    S.op("pool", lambda e, x=x, hf=hf: e.dma_start(out=wgb[hf], in_=wg_src[x][:, :, hf * 1024:(hf + 1) * 1024]),
                         writes=[f"wg{hf}"], dma=True)
                    S.op("pool", lambda e, x=x, hf=hf: e.dma_start(out=wub[hf], in_=wu_src[x][:, :, hf * 1024:(hf + 1) * 1024]),
                         writes=[f"wu{hf}"], dma=True)
                for hf in range(2):
                    bi = (2 * x + hf) % 3
                    S.op("pool", lambda e, x=x, hf=hf, bi=bi: e.dma_start(out=wdb[bi], in_=wd_src[x][:, hf * 8:(hf + 1) * 8, :]),
                         writes=[f"wd{bi}"], dma=True)

            CH = [(0, 128), (128, 128), (256, 128), (384, 128)]
            _DBG["g"] = (xg, h2_d, idxi)
            load_weights(0)
            for x in range(n_exp):
                pp = x % 2
                S.op("dve", lambda e, x=x: e.tensor_copy(out=vals[:, :, 3], in_=aff[:, :, x]), reads=AFF_R, writes=["vals_g"])
                for j in range(NO):
                    sb_ = selb[j % 2]
                    S.op("dve", lambda e, j=j, x=x, sb_=sb_: e.tensor_scalar(out=sb_, in0=iota, scalar1=dest[:, j, x:x + 1], scalar2=None, op0=ALU.is_equal),
                         reads=["iota", "dest"], writes=[f"selb{j % 2}"])
                    for c, (s0, sn) in enumerate(CH):
                        IB = [5, 4, 6, 7]
                        S.op("pe", lambda e, j=j, c=c, s0=s0, sn=sn, sb_=sb_: e.matmul(
                            pb[IB[c]][0:sn, 0:4], lhsT=sb_[:, s0:s0 + sn], rhs=vals[:, j, :], start=(j == 0), stop=(j == NO - 1)),
                            reads=[f"selb{j % 2}", "vals_c", "vals_g"], writes=[f"pb{IB[c]}"])
                for c in range(4):
                    S.op("act", lambda e, c=c: e.copy(out=idxf[:, c, :], in_=pb[[5, 4, 6, 7][c]][:, 0:4]), reads=[f"pb{[5, 4, 6, 7][c]}"], writes=["idxf"])
                S.op("dve", lambda e: e.tensor_scalar(out=idxf[:, :, 0], in0=idxf[:, :, 0], scalar1=64.0, scalar2=None, op0=ALU.mult), reads=["idxf"], writes=["idxf"])
                S.op("dve", lambda e: e.tensor_tensor(out=idxf[:, :, 0], in0=idxf[:, :, 0], in1=idxf[:, :, 1], op=ALU.add), reads=["idxf"], writes=["idxf"])
                S.op("dve", lambda e: e.tensor_scalar(out=idxf[:, :, 2], in0=idxf[:, :, 2], scalar1=-BIG, scalar2=BIG, op0=ALU.mult, op1=ALU.add), reads=["idxf"], writes=["idxf"])
                S.op("dve", lambda e: e.tensor_tensor(out=idxf[:, :, 0], in0=idxf[:, :, 0], in1=idxf[:, :, 2], op=ALU.add), reads=["idxf"], writes=["idxf"])
                for c in range(4):
                    S.op("dve", lambda e, pp=pp, c=c: e.tensor_copy(out=idxi[pp][c], in_=idxf[:, c, 0:1]), reads=["idxf"], writes=[f"idxi{pp}"])
                S.op("dve", lambda e, pp=pp: e.tensor_copy(out=gate[pp], in_=idxf[:, :, 3]), reads=["idxf"], writes=[f"gate{pp}"])
                for c, (s0, sn) in enumerate(CH):
                    S.op("pool", lambda e, c=c, sn=sn, pp=pp: e.indirect_dma_start(
                        out=xg[0:sn, c, :], out_offset=None, in_=h2_d[:, :],
                        in_offset=bass.IndirectOffsetOnAxis(ap=idxi[pp][c][0:sn, :], axis=0),
                        bounds_check=_bc(e, OWN - 1), oob_is_err=False),
                        reads=[f"idxi{pp}", "h2d"], writes=[f"xg{c}"], dma=True)
                pT = pbh[5].rearrange("p (k c) -> p k c", k=8)
                for c, (s0, sn) in enumerate(CH):
                    for kc in range(8):
                        S.op("pe", lambda e, c=c, kc=kc, sn=sn: e.transpose(out=pT[:, kc, 0:sn], in_=xg[0:sn, c, kc * 128:(kc + 1) * 128], identity=ident[0:sn, 0:sn]),
                             reads=[f"xg{c}", "ident"], writes=["pb5"])
                    eng = "act" if c % 2 == 0 else "dve"
                    if eng == "act":
                        S.op("act", lambda e, s0=s0, sn=sn, pp=pp: e.copy(out=xeT[pp][:, :, s0:s0 + sn], in_=pT[:, :, 0:sn]), reads=["pb5"], writes=[f"xeT{pp}_{c}"])
                    else:
                        S.op("dve", lambda e, s0=s0, sn=sn, pp=pp: e.tensor_copy(out=xeT[pp][:, :, s0:s0 + sn], in_=pT[:, :, 0:sn]), reads=["pb5"], writes=[f"xeT{pp}_{c}"])
                XE_R = [f"xeT{pp}_{c}" for c in range(4)]
                for fc in range(16):
                    hf = fc // 8
                    ba = (2 * fc) % 4
                    bu = (2 * fc + 1) % 4
                    for kc in range(8):
                        S.op("pe", lambda e, kc=kc, fc=fc, hf=hf, ba=ba, pp=pp: e.matmul(
                            pb[ba][:, 0:CAP], lhsT=wgb[hf][:, kc, (fc % 8) * 128:(fc % 8 + 1) * 128], rhs=xeT[pp][:, kc, :], start=(kc == 0), stop=(kc == 7)),
                            reads=[f"wg{hf}"] + XE_R, writes=[f"pb{ba}"])
                    for kc in range(8):
                        S.op("pe", lambda e, kc=kc, fc=fc, hf=hf, bu=bu, pp=pp: e.matmul(
                            pb[bu][:, 0:CAP], lhsT=wub[hf][:, kc, (fc % 8) * 128:(fc % 8 + 1) * 128], rhs=xeT[pp][:, kc, :], start=(kc == 0), stop=(kc == 7)),
                            reads=[f"wu{hf}"] + XE_R, writes=[f"pb{bu}"])
                    S.op("act", lambda e, fc=fc, ba=ba: e.activation(out=sa[fc % 2], in_=pb[ba][:, 0:CAP], func=AF.Silu), reads=[f"pb{ba}"], writes=[f"sa{fc % 2}"])
                    S.op("dve", lambda e, fc=fc, bu=bu: e.tensor_tensor(out=hTm[:, fc, :], in0=pb[bu][:, 0:CAP], in1=sa[fc % 2], op=ALU.mult),
                         reads=[f"pb{bu}", f"sa{fc % 2}"], writes=[f"hTm{fc}"])
                    if fc == 7 and x + 1 < n_exp:
                        S.op("pool", lambda e, x=x: e.dma_start(out=wgb[0], in_=wg_src[x + 1][:, :, 0:1024]), writes=["wg0"], dma=True)
                        S.op("pool", lambda e, x=x: e.dma_start(out=wub[0], in_=wu_src[x + 1][:, :, 0:1024]), writes=["wu0"], dma=True)
                if x + 1 < n_exp:
                    S.op("pool", lambda e, x=x: e.dma_start(out=wgb[1], in_=wg_src[x + 1][:, :, 1024:2048]), writes=["wg1"], dma=True)
                    S.op("pool", lambda e, x=x: e.dma_start(out=wub[1], in_=wu_src[x + 1][:, :, 1024:2048]), writes=["wu1"], dma=True)
                H_R = [f"hTm{fc}" for fc in range(16)]
                for c, (s0, sn) in enumerate(CH):
                    yb = (4 * x + c) % 2
                    for hf2 in range(2):
                        bank = 6 + hf2
                        for fc in range(16):
                            bi = (2 * x + fc // 8) % 3
                            S.op("pe", lambda e, fc=fc, bi=bi, s0=s0, sn=sn, hf2=hf2, bank=bank: e.matmul(
                                pb[bank][0:sn, :], lhsT=hTm[:, fc, s0:s0 + sn], rhs=wdb[bi][:, fc % 8, hf2 * 512:(hf2 + 1) * 512],
                                start=(fc == 0), stop=(fc == 15)),
                                reads=[f"hTm{fc}", f"wd{bi}"], writes=[f"pb{bank}"])
                        if hf2 == 0:
                            S.op("act", lambda e, sn=sn, yb=yb, c=c, pp=pp, bank=bank: e.activation(
                                out=yo[yb][0:sn, 0:512], in_=pb[bank][0:sn, :], func=AF.Copy, scale=gate[pp][0:sn, c:c + 1]),
                                reads=[f"pb{bank}", f"gate{pp}"], writes=[f"yo{yb}_0"])
                        else:
                            S.op("dve", lambda e, sn=sn, yb=yb, c=c, pp=pp, bank=bank: e.tensor_scalar(
                                out=yo[yb][0:sn, 512:1024], in0=pb[bank][0:sn, :], scalar1=gate[pp][0:sn, c:c + 1], scalar2=None, op0=ALU.mult),
                                reads=[f"pb{bank}", f"gate{pp}"], writes=[f"yo{yb}_1"])
                    prev = ["acc"] if x == 0 else [f"accw{x - 1}_{k}" for k in range(4)]
                    S.op("pool", lambda e, yb=yb, c=c, pp=pp: e.indirect_dma_start(
                        out=acc_d[:, :], out_offset=bass.IndirectOffsetOnAxis(ap=idxi[pp][c][:, :], axis=0),
                        in_=yo[yb][:, :], in_offset=None, bounds_check=_bc(e, OWN - 1), oob_is_err=False, compute_op=ALU.add),
                        reads=[f"yo{yb}_0", f"yo{yb}_1", f"idxi{pp}"] + prev, writes=[f"accw{x}_{c}"], dma=True)
                if x + 1 < n_exp:
                    for hf in range(2):
                        bi = (2 * (x + 1) + hf) % 3
                        S.op("pool", lambda e, x=x, hf=hf, bi=bi: e.dma_start(out=wdb[bi], in_=wd_src[x + 1][:, hf * 8:(hf + 1) * 8, :]),
                             writes=[f"wd{bi}"], dma=True)

        S.barrier()
        AR.off = mark_persist
        fin = [AR.alloc([128, D], F32) for _ in range(2)]
        fjunk = AR.alloc([128, D], F32)
        ACC_FINAL = ["acc"] if stop_after_phase1 else [f"accw{n_exp - 1}_{k}" for k in range(4)]
        for j in range(NO):
            b = j % 2
            S.op("sp", lambda e, j=j, b=b: e.dma_start(out=fin[b], in_=acc_d[j * 128:(j + 1) * 128, :]), reads=ACC_FINAL, writes=[f"fin{b}"], dma=True)
            S.op("act", lambda e, b=b: e.activation(out=fjunk, in_=fin[b], func=AF.Square, scale=1.0 / 32, accum_out=small[:, 40:41]),
                 reads=[f"fin{b}"], writes=["junkf", "fss"])
            S.op("act", lambda e: e.activation(out=small[:, 41:42], in_=small[:, 40:41], func=AF.Sqrt, bias=EPS), reads=["fss"], writes=["frs"])
            S.op("dve", lambda e: e.reciprocal(out=small[:, 42:43], in_=small[:, 41:42]), reads=["frs"], writes=["frstd"])
            S.op("dve", lambda e, b=b: e.scalar_tensor_tensor(out=fin[b], in0=fin[b], scalar=small[:, 42:43], in1=gfin, op0=ALU.mult, op1=ALU.mult),
                 reads=[f"fin{b}", "frstd", "gfin"], writes=[f"fin{b}"])
            S.op("sp", lambda e, j=j, b=b: e.dma_start(out=out_d[j * 128:(j + 1) * 128, :], in_=fin[b]), reads=[f"fin{b}"], dma=True)
        S.barrier()
        sems["cc"] = es.enter_context(nc.semaphore("sem_cc"))
        S.emit(sems)
    return nc


def _weight_mask(delta):
    a = np.abs(delta)
    w = (a <= 64).astype(np.float32)
    w += ((a <= 256) & (delta % 4 == 0)).astype(np.float32)
    w += ((a <= 1024) & (delta % 16 == 0)).astype(np.float32)
    return w


def _consts():
    bf = ml_dtypes.bfloat16
    kk = np.arange(128)[:, None]
    qq = np.arange(128)[None, :]
    mA = np.concatenate([(kk >= qq), np.ones((128, 128), bool), (kk <= qq)], axis=1).astype(np.float32)
    blocks = []
    for o in range(23):
        d = 128 * (11 - o) + kk - qq
        blocks.append(_weight_mask(d))
    mB = np.concatenate(blocks, axis=1)
    tri = (kk < qq).astype(np.float32)
    tok = (np.arange(NO)[None, :] * 128 + np.arange(128)[:, None])
    tokc = np.stack([tok // 64, tok % 64, np.ones_like(tok)], axis=-1).astype(np.float32).reshape(128, NO * 3)
    return {
        "ident": np.eye(128).astype(bf),
        "identf": np.eye(128, dtype=np.float32),
        "tri": tri,
        "ones": np.ones((128, 128), np.float32),
        "maskA": mA.astype(bf),
        "maskB": mB.astype(bf),
        "iota": np.arange(CAP, dtype=np.float32).reshape(1, CAP),
        "tokc": np.ascontiguousarray(tokc),
    }


def _positions(c):
    half = c % 2
    i = np.arange(WIN)
    return i if half == 0 else (SEQ - 1 - i)


def _in_maps(x, g_mix, w_in, a_sink, g_out_a, g_out_b, w_out, g_ffn, w_router, w_gate, w_up, w_down, g_final, n_exp=NE):
    consts = _consts()
    inv_freq = (np.float32(500000.0) ** (-np.arange(0, 16, 2, dtype=np.float32) / np.float32(16))).astype(np.float32)
    sink = np.asarray(a_sink[0], np.float32)
    perm = [0, 2, 1, 3, 4, 6, 5, 7]
    shared = {
        "g_mix": np.ascontiguousarray(g_mix[0:1]), "g_ffn": np.ascontiguousarray(g_ffn[0:1]),
        "g_final": np.ascontiguousarray(np.asarray(g_final).reshape(1, D)),
        "g_out": np.ascontiguousarray(np.concatenate([g_out_a[0], g_out_b[0]])[None, :]),
        "w_in": np.ascontiguousarray(w_in[0]), "w_out": np.ascontiguousarray(w_out[0]),
        "w_router": np.ascontiguousarray(w_router[0]),
        "w_gate": np.ascontiguousarray(w_gate[0][:n_exp]), "w_up": np.ascontiguousarray(w_up[0][:n_exp]),
        "w_down": np.ascontiguousarray(w_down[0][:n_exp]),
        "sinkp": np.ascontiguousarray(sink[perm][None, :]),
    }
    shared.update(consts)
    maps = []
    for c in range(8):
        pos = _positions(c)
        ang = pos.astype(np.float32)[:, None] * inv_freq[None, :]
        m = dict(shared)
        m["xw"] = np.ascontiguousarray(x[c // 2][pos])
        m["cosw"] = np.cos(ang).astype(np.float32)
        m["sinw"] = np.sin(ang).astype(np.float32)
        m["par"] = np.array([[1.0, 0.0]] if c % 2 == 0 else [[0.0, 1.0]], np.float32)
        maps.append(m)
    return maps


_NC_CACHE = {}


def kernel(x, g_mix, w_in, a_sink, g_out_a, g_out_b, w_out, g_ffn, w_router, w_gate, w_up, w_down, g_final):
    args = [np.asarray(a) for a in (x, g_mix, w_in, a_sink, g_out_a, g_out_b, w_out, g_ffn, w_router, w_gate, w_up, w_down, g_final)]
    if "nc" not in _NC_CACHE:
        _NC_CACHE["nc"] = build()
    nc = _NC_CACHE["nc"]
    maps = _in_maps(*args)
    res = run_bass_kernel_spmd(nc, maps, core_ids=list(range(8)))
    out = np.empty((4, SEQ, D), np.float32)
    for c in range(8):
        pos = _positions(c)[:OWN]
        out[c // 2][pos] = res.results[c]["out"]
    return out
```

```python
import numpy as np
import ml_dtypes
from contextlib import ExitStack
import concourse.bass as bass
import concourse.mybir as mybir
from concourse.bass_utils import run_bass_kernel_spmd

F32 = mybir.dt.float32
I32 = mybir.dt.int32
BF16 = mybir.dt.bfloat16
ALU = mybir.AluOpType
AF = mybir.ActivationFunctionType
AX = mybir.AxisListType

D = 1024
SEQ = 4096
WIN = 3072
OWN = 2048
NT = 24
NO = 16
NE = 16
FF = 2048
CAP = 512
EPS = 1e-6
BIG = 1.0e6
ENGS = ["pe", "act", "dve", "pool", "sp"]


class Sched:
    def __init__(self, nc, n_dma=32, same_engine_wait=("act", "dve", "pool")):
        self.nc = nc
        self.ops = {e: [] for e in ENGS}
        self.cnt = {e: 0 for e in ENGS}
        self.waited = {e: {} for e in ENGS}
        self.last_w = {}
        self.readers = {}
        self.n_dma = n_dma
        self.dma_val = [0] * n_dma
        self.dma_rr = {"pool": 0, "sp": 0, "act": 0}
        self.same = set(same_engine_wait)
        self.all_tokens = {}

    def op(self, eng, fn, reads=(), writes=(), dma=False):
        writes = list(writes) + [r for r in reads if r.startswith("pb")]
        reads = [r for r in reads if not r.startswith("pb")]
        deps = set()
        for r in reads:
            if r in self.last_w:
                deps.add(self.last_w[r])
        for w in writes:
            if w in self.last_w:
                deps.add(self.last_w[w])
            for t in self.readers.get(w, ()):
                deps.add(t)
        if dma:
            half = self.n_dma // 2
            base = 0 if eng == "pool" else half
            k = base + self.dma_rr[eng]
            self.dma_rr[eng] = (self.dma_rr[eng] + 1) % half
            if self.dma_val[k] > 0:
                deps.add((("dma", k), self.dma_val[k]))
            self.dma_val[k] += 16
            token = (("dma", k), self.dma_val[k])
        else:
            self.cnt[eng] += 1
            token = (eng, self.cnt[eng])
        waits = []
        wd = self.waited[eng]
        mx = {}
        for key, val in deps:
            if mx.get(key, 0) < val:
                mx[key] = val
        for key, val in sorted(mx.items(), key=lambda t: str(t[0])):
            if key == eng and eng not in self.same:
                continue
            if wd.get(key, 0) < val:
                wd[key] = val
                waits.append((key, val))
        self.ops[eng].append((waits, fn, token))
        for r in reads:
            self.readers.setdefault(r, []).append(token)
        for w in writes:
            self.last_w[w] = token
            self.readers[w] = []
        self.all_tokens[token[0]] = max(self.all_tokens.get(token[0], 0), token[1])
        return token

    def barrier(self, engs=ENGS):
        toks = dict(self.all_tokens)
        for e in engs:
            waits = []
            wd = self.waited[e]
            for key, val in toks.items():
                if key == e:
                    continue
                if wd.get(key, 0) < val:
                    wd[key] = val
                    waits.append((key, val))
            if waits:
                self.ops[e].append((waits, None, None))

    def emit(self, semaphores):
        nc = self.nc

        def run(ename):
            def body(eng):
                for waits, fn, token in self.ops[ename]:
                    for key, val in waits:
                        eng.wait_ge(semaphores[key], val)
                    if fn is None:
                        continue
                    ins = fn(eng)
                    key = token[0]
                    if isinstance(key, tuple):
                        ins.then_inc(semaphores[key], 16)
                    else:
                        ins.then_inc(semaphores[key], 1)
            return body

        with nc.Block() as block:
            block.tensor(run("pe"))
            block.scalar(run("act"))
            block.vector(run("dve"))
            block.gpsimd(run("pool"))
            block.sync(run("sp"))


import os
SUB = int(os.environ.get('KSUB', '9'))
LOG = []
_DBG = {}
_BCREG = {}


def _bc(eng, val):
    key = (id(eng), val)
    if key not in _BCREG:
        _BCREG[key] = eng.to_reg(val)
    return _BCREG[key]


class Arena:
    def __init__(self, big, nbytes):
        self.big = big
        self.nbytes = nbytes
        self.off = 0

    def alloc(self, shape, dt):
        esz = 2 if dt == BF16 else 4
        n = int(np.prod(shape[1:]))
        nb = (n * esz + 63) // 64 * 64
        o = self.off
        self.off += nb
        LOG.append((o, tuple(shape), str(dt)))
        assert self.off <= self.nbytes, (self.off, self.nbytes)
        v = self.big[:, o // 4:(o + nb) // 4]
        if dt != F32:
            v = v.bitcast(dt)
        v = v[:, 0:n]
        if len(shape) == 3:
            v = v.rearrange("p (a b) -> p a b", a=shape[1])
        elif len(shape) == 4:
            v = v.rearrange("p (a b c) -> p a b c", a=shape[1], b=shape[2])
        return v


def build(n_exp=NE, debug=False, stop_after_phase1=False, level=9):
    nc = bass.Bass("TRN2", target_bir_lowering=False)
    _BCREG.clear()

    def din(name, shape, dt=F32):
        return nc.dram_tensor(name, list(shape), dt, kind="ExternalInput").ap()

    xw = din("xw", [WIN, D])
    cosw = din("cosw", [WIN, 8])
    sinw = din("sinw", [WIN, 8])
    gmix_d = din("g_mix", [1, D])
    gffn_d = din("g_ffn", [1, D])
    gfin_d = din("g_final", [1, D])
    gout_d = din("g_out", [1, D])
    win_d = din("w_in", [D, 2304])
    wout_d = din("w_out", [D, D])
    wr_d = din("w_router", [D, NE])
    wg_d = din("w_gate", [n_exp, D, FF])
    wu_d = din("w_up", [n_exp, D, FF])
    wd_d = din("w_down", [n_exp, FF, D])
    sink_d = din("sinkp", [1, 8])
    par_d = din("par", [1, 2])
    ident_d = din("ident", [128, 128], BF16)
    identf_d = din("identf", [128, 128])
    tri_d = din("tri", [128, 128])
    ones_d = din("ones", [128, 128])
    maskA_d = din("maskA", [128, 3 * 128], BF16)
    maskB_d = din("maskB", [128, 23 * 128], BF16)
    iota_d = din("iota", [1, CAP])
    tokc_d = din("tokc", [128, NO * 3])
    out_d = nc.dram_tensor("out", [OWN, D], F32, kind="ExternalOutput").ap()
    if debug:
        dbg_x1 = nc.dram_tensor("dbg_x1", [OWN, D], F32, kind="ExternalOutput").ap()
        dbg_aff = nc.dram_tensor("dbg_aff", [128, NO * NE], F32, kind="ExternalOutput").ap()
        dbg_thr = nc.dram_tensor("dbg_thr", [128, 2 * NE], F32, kind="ExternalOutput").ap()

    acc_d = nc.dram_tensor("acc", [OWN, D], F32).ap()
    h2_d = nc.dram_tensor("h2buf", [OWN, D], BF16).ap()
    cin_d = nc.dram_tensor("cin", [128, 2 * NO * NE], F32)
    cout_d = nc.dram_tensor("cout", [128, 2 * NO * NE], F32)

    S = Sched(nc)
    with ExitStack() as es:
        SB_BYTES = 196 * 1024
        big = es.enter_context(nc.sbuf_tensor("big", [128, SB_BYTES // 4], F32))
        AR = Arena(big, SB_BYTES)
        pbt = [es.enter_context(nc.psum_tensor(f"pb{i}", [128, 512], F32)) for i in range(8)]
        pb = [t[:, :] for t in pbt]
        pbh = [t[:, :].bitcast(BF16) for t in pbt]
        sems = {e: es.enter_context(nc.semaphore("sem_" + e)) for e in ENGS}
        for k in range(S.n_dma):
            sems[("dma", k)] = es.enter_context(nc.semaphore(f"dsem{k}"))

        ident = AR.alloc([128, 128], BF16)
        identf = AR.alloc([128, 128], F32)
        tri = AR.alloc([128, 128], F32)
        ones = AR.alloc([128, 128], F32)
        iota = AR.alloc([128, CAP], F32)
        tokc = AR.alloc([128, NO, 3], F32)
        par = AR.alloc([128, 2], F32)
        gfin = AR.alloc([128, D], F32)
        gffn = AR.alloc([128, D], F32)
        aff = AR.alloc([128, NO, NE], F32)
        small = AR.alloc([128, 64], F32)
        mark_persist = AR.off

        def ld(eng, dst, src, name, rd=()):
            S.op(eng, lambda e: e.dma_start(out=dst, in_=src), reads=rd, writes=[name], dma=True)

        ld("sp", ident, ident_d, "ident")
        ld("sp", identf, identf_d, "identf")
        ld("sp", tri, tri_d, "tri")
        ld("sp", ones, ones_d, "ones")
        ld("sp", iota, iota_d.partition_broadcast(128), "iota")
        ld("sp", tokc, tokc_d.rearrange("p (a b) -> p a b", a=NO), "tokc")
        ld("sp", par, par_d.partition_broadcast(128), "par")
        ld("sp", gfin, gfin_d.partition_broadcast(128), "gfin")
        ld("sp", gffn, gffn_d.partition_broadcast(128), "gffn")

        esink = AR.alloc([128, 8], F32)
        maskA = AR.alloc([128, 3 * 128], BF16)
        maskB = AR.alloc([128, 23 * 128], BF16)
        qaT = AR.alloc([128, 4, OWN], BF16)
        kaT = AR.alloc([128, 2, 17 * 128], BF16)
        qbT = AR.alloc([128, 4, OWN], BF16)
        kbT = AR.alloc([128, 4, WIN], BF16)
        vA = AR.alloc([128, 17, 2 * 65], BF16)
        vB = AR.alloc([128, NT, 8 * 65], BF16)
        xbuf = [AR.alloc([128, D], F32) for _ in range(2)]
        junk = AR.alloc([128, D], F32)
        hT = [AR.alloc([128, 8, 128], BF16) for _ in range(2)]
        mark_1a = AR.off
        gmix = AR.alloc([128, D], F32)
        cos_t = AR.alloc([128, NT, 8], F32)
        sin_t = AR.alloc([128, NT, 8], F32)
        win = AR.alloc([128, 8, 2304], BF16)
        xn = [AR.alloc([128, D], BF16) for _ in range(2)]
        qk_tm = [AR.alloc([128, 14 * 128], BF16) for _ in range(2)]
        tmpA = AR.alloc([128, 8 * 16], F32)
        tmpB = AR.alloc([128, 8 * 16], F32)
        print("phase1 sbuf bytes/partition:", AR.off)

        ld("sp", gmix, gmix_d.partition_broadcast(128), "gmix")
        ld("sp", cos_t, cosw.rearrange("(j p) d -> p j d", p=128), "cos")
        ld("sp", sin_t, sinw.rearrange("(j p) d -> p j d", p=128), "sin")
        ld("sp", esink, sink_d.partition_broadcast(128), "esink")
        ld("sp", maskA, maskA_d, "maskA")
        ld("sp", maskB, maskB_d, "maskB")
        win_src = win_d.rearrange("(k p) c -> p k c", p=128)
        for k0 in range(0, 8, 2):
            S.op("pool", lambda e, k0=k0: e.dma_start(out=win[:, k0:k0 + 2, :], in_=win_src[:, k0:k0 + 2, :]),
                 writes=[f"win{k0}"], dma=True)
        WIN_R = [f"win{k0}" for k0 in range(0, 8, 2)]
        S.op("act", lambda e: e.activation(out=esink, in_=esink, func=AF.Exp), reads=["esink"], writes=["esink"])
        vA4 = vA.rearrange("p j (h d) -> p j h d", h=2)
        vB4 = vB.rearrange("p j (h d) -> p j h d", h=8)
        S.op("pool", lambda e: e.memset(vA4[:, :, :, 64:65], 1.0), writes=["vA_ones"])
        S.op("pool", lambda e: e.memset(vB4[:, :, :, 64:65], 1.0), writes=["vB_ones"])

        psel = [0]

        def rope(j, pg, c0, H, dst, dups):
            if os.environ.get("KROPE", "1") == "0":
                return
            v = pg[:, c0:c0 + H * 64].rearrange("p (h d) -> p h d", h=H)
            x1 = v[:, :, 0:8]
            x2 = v[:, :, 8:16]
            cb = cos_t[:, j, :].unsqueeze(1).to_broadcast([128, H, 8])
            sb_ = sin_t[:, j, :].unsqueeze(1).to_broadcast([128, H, 8])
            t1c = tmpA[:, 0:H * 8].rearrange("p (h d) -> p h d", h=H)
            t2c = tmpA[:, 64:64 + H * 8].rearrange("p (h d) -> p h d", h=H)
            t1s = tmpB[:, 0:H * 8].rearrange("p (h d) -> p h d", h=H)
            t2s = tmpB[:, 64:64 + H * 8].rearrange("p (h d) -> p h d", h=H)
            pgn = dups["pg"]
            S.op("dve", lambda e: e.tensor_tensor(out=t1c, in0=x1, in1=cb, op=ALU.mult), reads=[pgn, "cos"], writes=["t1c"])
            S.op("dve", lambda e: e.tensor_tensor(out=t2c, in0=x2, in1=cb, op=ALU.mult), reads=[pgn, "cos"], writes=["t2c"])
            S.op("dve", lambda e: e.tensor_tensor(out=t1s, in0=x1, in1=sb_, op=ALU.mult), reads=[pgn, "sin"], writes=["t1s"])
            S.op("dve", lambda e: e.tensor_tensor(out=t2s, in0=x2, in1=sb_, op=ALU.mult), reads=[pgn, "sin"], writes=["t2s"])
            for dv, dn in dst:
                S.op("dve", lambda e, dv=dv: e.tensor_tensor(out=dv[:, :, 0:8], in0=t1c, in1=t2s, op=ALU.subtract),
                     reads=["t1c", "t2s"], writes=[dn + "_a"])
                S.op("dve", lambda e, dv=dv: e.tensor_tensor(out=dv[:, :, 8:16], in0=t2c, in1=t1s, op=ALU.add),
                     reads=["t2c", "t1s"], writes=[dn + "_b"])
                if os.environ.get("KROPE", "3") == "2":
                    continue
                S.op("act", lambda e, dv=dv: e.copy(out=dv[:, :, 16:64], in_=v[:, :, 16:64]), reads=[pgn], writes=[dn + "_c"])

        def proj_group(j, hTj, c0, ncols, bank):
            for kc in range(8):
                S.op("pe", lambda e, kc=kc: e.matmul(pb[bank][:, 0:ncols], lhsT=hTj[:, kc, :], rhs=win[:, kc, c0:c0 + ncols],
                                                     start=(kc == 0), stop=(kc == 7)),
                     reads=[f"hT{j % 2}"] + WIN_R, writes=[f"pb{bank}"])

        for j in range(NT if level >= 1 else 0):
            b = j % 2
            own = j < NO
            xt = xbuf[b]
            S.op("sp", lambda e, j=j, xt=xt: e.dma_start(out=xt, in_=xw[j * 128:(j + 1) * 128, :]), writes=[f"x{b}"], dma=True)
            S.op("act", lambda e, xt=xt: e.activation(out=junk, in_=xt, func=AF.Square, scale=1.0 / 32, accum_out=small[:, 0:1]),
                 reads=[f"x{b}"], writes=["junk", "ss"])
            S.op("act", lambda e: e.activation(out=small[:, 1:2], in_=small[:, 0:1], func=AF.Sqrt, bias=EPS), reads=["ss"], writes=["rs"])
            S.op("dve", lambda e: e.reciprocal(out=small[:, 2:3], in_=small[:, 1:2]), reads=["rs"], writes=["rstd"])
            S.op("dve", lambda e, xt=xt, b=b: e.scalar_tensor_tensor(out=xn[b], in0=xt, scalar=small[:, 2:3], in1=gmix, op0=ALU.mult, op1=ALU.mult),
                 reads=[f"x{b}", "rstd", "gmix"], writes=[f"xn{b}"])
            pT = pbh[5].rearrange("p (k c) -> p k c", k=8)
            for kc in range(8):
                S.op("pe", lambda e, kc=kc, b=b: e.transpose(out=pT[:, kc, :], in_=xn[b][:, kc * 128:(kc + 1) * 128], identity=ident),
                     reads=[f"xn{b}", "ident"], writes=["pb5"])
            S.op("act", lambda e, b=b: e.copy(out=hT[b], in_=pT), reads=["pb5"], writes=[f"hT{b}"])
            if SUB < 2:
                continue
            qk = qk_tm[b]
            qkn = f"qk{b}"
            if own:
                bank = psel[0] % 3; psel[0] += 1
                proj_group(j, hT[b], 0, 512, bank)
                rope(j, pb[bank], 0, 8, [(qk[:, 0:512].rearrange("p (h d) -> p h d", h=8), qkn + "_qa")], {"pg": f"pb{bank}"})
            if j <= NO:
                bank = psel[0] % 3; psel[0] += 1
                proj_group(j, hT[b], 512, 256, bank)
                kd = qk[:, 512:768].rearrange("p (h t d) -> p h t d", h=2, t=2)
                rope(j, pb[bank], 0, 2, [(kd[:, :, 0, :], qkn + "_ka0"), (kd[:, :, 1, :], qkn + "_ka1")], {"pg": f"pb{bank}"})
                S.op("act", lambda e, j=j, bank=bank: e.copy(out=vA4[:, j, :, 0:64], in_=pb[bank][:, 128:256].rearrange("p (h d) -> p h d", h=2)),
                     reads=[f"pb{bank}"], writes=[f"vA{j}"])
            if own:
                bank = psel[0] % 3; psel[0] += 1
                proj_group(j, hT[b], 768, 512, bank)
                rope(j, pb[bank], 0, 8, [(qk[:, 768:1280].rearrange("p (h d) -> p h d", h=8), qkn + "_qb")], {"pg": f"pb{bank}"})
            bank = psel[0] % 3; psel[0] += 1
            proj_group(j, hT[b], 1280, 512, bank)
            rope(j, pb[bank], 0, 8, [(qk[:, 1280:1792].rearrange("p (h d) -> p h d", h=8), qkn + "_kb")], {"pg": f"pb{bank}"})
            bank = psel[0] % 3; psel[0] += 1
            proj_group(j, hT[b], 1792, 512, bank)
            S.op("dve", lambda e, j=j, bank=bank: e.tensor_copy(out=vB4[:, j, :, 0:64], in_=pb[bank].rearrange("p (h d) -> p h d", h=8)),
                 reads=[f"pb{bank}"], writes=[f"vB{j}"])
            if SUB < 3:
                continue
            qT3 = pbh[3].rearrange("p (k c) -> p k c", k=8)
            qT4 = pbh[4].rearrange("p (k c) -> p k c", k=8)

            def tr(dst_bank_view, slot, ft, rd, bankname, qk=qk):
                S.op("pe", lambda e: e.transpose(out=dst_bank_view[:, slot, :], in_=qk[:, ft * 128:(ft + 1) * 128], identity=ident),
                     reads=rd + ["ident"], writes=[bankname])
            rd_qa = [qkn + "_qa_a", qkn + "_qa_b", qkn + "_qa_c"]
            rd_ka = [qkn + s for s in ("_ka0_a", "_ka0_b", "_ka0_c", "_ka1_a", "_ka1_b", "_ka1_c")]
            rd_qb = [qkn + "_qb_a", qkn + "_qb_b", qkn + "_qb_c"]
            rd_kb = [qkn + "_kb_a", qkn + "_kb_b", qkn + "_kb_c"]
            if own:
                for t in range(4):
                    tr(qT3, t, t, rd_qa, "pb3")
            if j <= NO:
                for t in range(2):
                    tr(qT3, 4 + t, 4 + t, rd_ka, "pb3")
            if own:
                S.op("act", lambda e, j=j: e.copy(out=qaT[:, :, j * 128:(j + 1) * 128], in_=qT3[:, 0:4, :]), reads=["pb3"], writes=[f"qaT{j}"])
            if j <= NO:
                S.op("dve", lambda e, j=j: e.tensor_copy(out=kaT[:, :, j * 128:(j + 1) * 128], in_=qT3[:, 4:6, :]), reads=["pb3"], writes=[f"kaT{j}"])
            if own:
                for t in range(4):
                    tr(qT4, t, 6 + t, rd_qb, "pb4")
            for t in range(4):
                tr(qT4, 4 + t, 10 + t, rd_kb, "pb4")
            if own:
                S.op("act", lambda e, j=j: e.copy(out=qbT[:, :, j * 128:(j + 1) * 128], in_=qT4[:, 0:4, :]), reads=["pb4"], writes=[f"qbT{j}"])
            S.op("dve", lambda e, j=j: e.tensor_copy(out=kbT[:, :, j * 128:(j + 1) * 128], in_=qT4[:, 4:8, :]), reads=["pb4"], writes=[f"kbT{j}"])

        S.barrier()
        AR.off = mark_1a
        gout = AR.alloc([128, D], F32)
        wout = AR.alloc([128, 8, D], BF16)
        wr = AR.alloc([128, 8, NE], F32)
        NPB = 4
        Pb = [AR.alloc([128, 512], BF16) for _ in range(NPB)]
        o_all2 = AR.alloc([128, 2, 4 * D], BF16)
        mixed = [AR.alloc([128, D], BF16)] * 2
        x1t = [AR.alloc([128, D], F32)] * 2
        h2f = [AR.alloc([128, D], F32)] * 2
        h2b = [AR.alloc([128, D], BF16)] * 2
        h2T = AR.alloc([128, 4, 128], F32)
        print("phase1b sbuf bytes/partition:", AR.off)
        ld("sp", gout, gout_d.partition_broadcast(128), "gout")
        ld("sp", wr, wr_d.rearrange("(k p) c -> p k c", p=128), "wr")
        S.op("pool", lambda e: e.dma_start(out=wout, in_=wout_d.rearrange("(k p) c -> p k c", p=128)),
             writes=["wout"], dma=True)
        sbank = [0]
        pbuf = [0]
        OB = [3, 4, 6, 7]
        OBN = [f"pb{q}" for q in OB]
        HD = [0, 2, 1, 3]

        def o_view(g):
            return o_all2[:, g % 2, :].rearrange("p (q d) -> p q d", q=4)

        def attn_steps(g):
            steps = []
            o_all = o_view(g)
            gp = g % 2
            for kvh in range(2):
                for qi in range(4):
                    qb_ = 4 * g + qi
                    kbs = [kb for kb in (qb_ - 1, qb_, qb_ + 1) if 0 <= kb <= 16]
                    for kb in kbs:
                        st = {}

                        def s1(st=st, kb=kb, qb_=qb_, kvh=kvh):
                            pi = pbuf[0] % NPB; pbuf[0] += 1
                            st["pi"] = pi
                            P = Pb[pi]
                            for half in range(2):
                                sbk = sbank[0] % 3; sbank[0] += 1
                                r0 = half * 64
                                S.op("pe", lambda e, half=half, r0=r0, sbk=sbk: e.matmul(
                                    pb[sbk][:, 0:256].rearrange("p (a c) -> p a c", a=2),
                                    lhsT=kaT[r0:r0 + 64, kvh, kb * 128:(kb + 1) * 128],
                                    rhs=qaT[r0:r0 + 64, 2 * kvh:2 * kvh + 2, qb_ * 128:(qb_ + 1) * 128],
                                    start=True, stop=True),
                                    reads=[f"kaT{kb}", f"qaT{qb_}"], writes=[f"pb{sbk}"])
                                S.op("act", lambda e, sbk=sbk, half=half: e.activation(out=P[:, half * 256:(half + 1) * 256], in_=pb[sbk][:, 0:256], func=AF.Exp, scale=0.125),
                                     reads=[f"pb{sbk}"], writes=[f"P{pi}"])
                            if kb != qb_:
                                blk = 0 if kb < qb_ else 2
                                S.op("dve", lambda e: e.tensor_tensor(
                                    out=P.rearrange("p (h c) -> p h c", h=4), in0=P.rearrange("p (h c) -> p h c", h=4),
                                    in1=maskA[:, blk * 128:(blk + 1) * 128].unsqueeze(1).to_broadcast([128, 4, 128]), op=ALU.mult),
                                    reads=[f"P{pi}", "maskA"], writes=[f"P{pi}"])

                        def s2(st=st, kb=kb, kbs=kbs, kvh=kvh, qi=qi):
                            pi = st["pi"]
                            P = Pb[pi]
                            for hh in range(4):
                                S.op("pe", lambda e, hh=hh: e.matmul(
                                    pb[OB[hh]][:, 0:65], lhsT=P[:, hh * 128:(hh + 1) * 128], rhs=vA4[:, kb, kvh, :],
                                    start=(kb == kbs[0]), stop=(kb == kbs[-1])),
                                    reads=[f"P{pi}", f"vA{kb}", "vA_ones"], writes=[OBN[hh]])
                            if kb == kbs[-1]:
                                denA = small[:, 8:12]
                                for hh in range(4):
                                    S.op("dve", lambda e, hh=hh: e.tensor_tensor(out=denA[:, hh:hh + 1], in0=pb[OB[hh]][:, 64:65],
                                                                                 in1=esink[:, kvh * 4 + hh:kvh * 4 + hh + 1], op=ALU.add),
                                         reads=[OBN[hh], "esink"], writes=[f"den{hh}"])
                                    S.op("dve", lambda e, hh=hh: e.reciprocal(out=denA[:, hh:hh + 1], in_=denA[:, hh:hh + 1]), reads=[f"den{hh}"], writes=[f"den{hh}"])
                                    hd = kvh * 4 + HD[hh]
                                    S.op("dve", lambda e, hh=hh, hd=hd: e.tensor_scalar(
                                        out=o_all[:, qi, hd * 64:(hd + 1) * 64], in0=pb[OB[hh]][:, 0:64], scalar1=denA[:, hh:hh + 1], scalar2=None, op0=ALU.mult),
                                        reads=[OBN[hh], f"den{hh}"], writes=[f"oall{gp}_{qi}A{kvh}_{hh}"])
                        steps.append((s1, s2))
            for h in range(8):
                ft = h // 2
                r0 = (h % 2) * 64
                kb_list = []
                for kb in range(max(0, 4 * g - 8), min(NT, 4 * g + 12)):
                    qlo = max(4 * g, kb - 8)
                    qhi = min(4 * g + 3, kb + 8)
                    if qhi >= qlo:
                        kb_list.append((kb, qlo, qhi))
                for (kb, qlo, qhi) in kb_list:
                    st = {}
                    ncol = (qhi - qlo + 1) * 128

                    def s1(st=st, kb=kb, qlo=qlo, qhi=qhi, ncol=ncol, ft=ft, r0=r0):
                        sbk = sbank[0] % 3; sbank[0] += 1
                        pi = pbuf[0] % NPB; pbuf[0] += 1
                        st["pi"] = pi
                        P = Pb[pi]
                        S.op("pe", lambda e: e.matmul(
                            pb[sbk][:, 0:ncol], lhsT=kbT[r0:r0 + 64, ft, kb * 128:(kb + 1) * 128],
                            rhs=qbT[r0:r0 + 64, ft, qlo * 128:qlo * 128 + ncol], start=True, stop=True),
                            reads=[f"kbT{kb}"] + [f"qbT{q}" for q in range(qlo, qhi + 1)], writes=[f"pb{sbk}"])
                        S.op("act", lambda e: e.activation(out=P[:, 0:ncol], in_=pb[sbk][:, 0:ncol], func=AF.Exp, scale=0.125),
                             reads=[f"pb{sbk}"], writes=[f"P{pi}"])
                        m0 = (qlo - kb + 11) * 128
                        S.op("dve", lambda e: e.tensor_tensor(out=P[:, 0:ncol], in0=P[:, 0:ncol], in1=maskB[:, m0:m0 + ncol], op=ALU.mult),
                             reads=[f"P{pi}", "maskB"], writes=[f"P{pi}"])

                    def s2(st=st, kb=kb, qlo=qlo, qhi=qhi, h=h, last_kb=kb_list[-1][0]):
                        pi = st["pi"]
                        P = Pb[pi]
                        for qb_ in range(qlo, qhi + 1):
                            first = max(0, qb_ - 8)
                            last = min(NT - 1, qb_ + 8)
                            S.op("pe", lambda e, qb_=qb_, first=first, last=last: e.matmul(
                                pb[OB[qb_ - 4 * g]][:, 0:65], lhsT=P[:, (qb_ - qlo) * 128:(qb_ - qlo + 1) * 128], rhs=vB4[:, kb, h, :],
                                start=(kb == first), stop=(kb == last)),
                                reads=[f"P{pi}", f"vB{kb}", "vB_ones"], writes=[OBN[qb_ - 4 * g]])
                        if kb == last_kb:
                            denB = small[:, 12:16]
                            for qi in range(4):
                                S.op("dve", lambda e, qi=qi: e.reciprocal(out=denB[:, qi:qi + 1], in_=pb[OB[qi]][:, 64:65]), reads=[OBN[qi]], writes=[f"denB{qi}"])
                                S.op("dve", lambda e, qi=qi: e.tensor_scalar(
                                    out=o_all[:, qi, 512 + h * 64:512 + (h + 1) * 64], in0=pb[OB[qi]][:, 0:64], scalar1=denB[:, qi:qi + 1], scalar2=None, op0=ALU.mult),
                                    reads=[OBN[qi], f"denB{qi}"], writes=[f"oallB{gp}_{h}_{qi}"])
                    steps.append((s1, s2))
            return steps

        def finish_tile(g, qi):
            j = 4 * g + qi
            b = j % 2
            gp = g % 2
            o_all = o_view(g)
            oa_r = [f"oall{gp}_{qi}A{k_}_{h_}" for k_ in range(2) for h_ in range(4)]
            ob_r = [f"oallB{gp}_{h}_{qi}" for h in range(8)]
            for gi, rd in ((0, oa_r), (1, ob_r)):
                S.op("act", lambda e, gi=gi: e.activation(out=junk[:, 0:512], in_=o_all[:, qi, gi * 512:(gi + 1) * 512], func=AF.Square,
                                                          scale=float(1.0 / np.sqrt(512.0)), accum_out=small[:, 16 + gi:17 + gi]),
                     reads=rd, writes=["junk", f"gss{gi}"])
                S.op("act", lambda e, gi=gi: e.activation(out=small[:, 18 + gi:19 + gi], in_=small[:, 16 + gi:17 + gi], func=AF.Sqrt, bias=EPS),
                     reads=[f"gss{gi}"], writes=[f"grs{gi}"])
                S.op("dve", lambda e, gi=gi: e.reciprocal(out=small[:, 20 + gi:21 + gi], in_=small[:, 18 + gi:19 + gi]), reads=[f"grs{gi}"], writes=[f"grstd{gi}"])
                S.op("dve", lambda e, gi=gi: e.scalar_tensor_tensor(
                    out=mixed[0][:, gi * 512:(gi + 1) * 512], in0=o_all[:, qi, gi * 512:(gi + 1) * 512], scalar=small[:, 20 + gi:21 + gi],
                    in1=gout[:, gi * 512:(gi + 1) * 512], op0=ALU.mult, op1=ALU.mult),
                    reads=rd + [f"grstd{gi}", "gout"], writes=[f"mixed0_{gi}"])
            yield
            pT = pbh[5].rearrange("p (k c) -> p k c", k=8)
            for kc in range(8):
                S.op("pe", lambda e, kc=kc: e.transpose(out=pT[:, kc, :], in_=mixed[0][:, kc * 128:(kc + 1) * 128], identity=ident),
                     reads=["mixed0_0", "mixed0_1", "ident"], writes=["pb5"])
            S.op("act", lambda e: e.copy(out=hT[b], in_=pT), reads=["pb5"], writes=[f"hT{b}"])
            xt = xbuf[b]
            S.op("sp", lambda e: e.dma_start(out=xt, in_=xw[j * 128:(j + 1) * 128, :]), writes=[f"x{b}"], dma=True)
            yield
            for hf in range(2):
                for kc in range(8):
                    S.op("pe", lambda e, kc=kc, hf=hf: e.matmul(
                        pb[5], lhsT=hT[b][:, kc, :], rhs=wout[:, kc, hf * 512:(hf + 1) * 512], start=(kc == 0), stop=(kc == 7)),
                        reads=[f"hT{b}", "wout"], writes=["pb5"])
                S.op("dve", lambda e, hf=hf: e.tensor_tensor(
                    out=x1t[0][:, hf * 512:(hf + 1) * 512], in0=pb[5], in1=xt[:, hf * 512:(hf + 1) * 512], op=ALU.add),
                    reads=["pb5", f"x{b}"], writes=[f"x1t0_{hf}"])
                yield
            x1r = ["x1t0_0", "x1t0_1"]
            S.op("sp", lambda e: e.dma_start(out=acc_d[j * 128:(j + 1) * 128, :], in_=x1t[0]), reads=x1r, writes=["acc"], dma=True)
            if debug:
                S.op("sp", lambda e: e.dma_start(out=dbg_x1[j * 128:(j + 1) * 128, :], in_=x1t[0]), reads=x1r, dma=True)
            S.op("act", lambda e: e.activation(out=junk, in_=x1t[0], func=AF.Square, scale=1.0 / 32, accum_out=small[:, 24:25]),
                 reads=x1r, writes=["junk", "ss2"])
            S.op("act", lambda e: e.activation(out=small[:, 25:26], in_=small[:, 24:25], func=AF.Sqrt, bias=EPS), reads=["ss2"], writes=["rs2"])
            S.op("dve", lambda e: e.reciprocal(out=small[:, 26:27], in_=small[:, 25:26]), reads=["rs2"], writes=["rstd2"])
            S.op("dve", lambda e: e.scalar_tensor_tensor(out=h2f[0], in0=x1t[0], scalar=small[:, 26:27], in1=gffn, op0=ALU.mult, op1=ALU.mult),
                 reads=x1r + ["rstd2", "gffn"], writes=["h2f0"])
            S.op("act", lambda e: e.copy(out=h2b[0], in_=h2f[0]), reads=["h2f0"], writes=["h2b0"])
            S.op("sp", lambda e: e.dma_start(out=h2_d[j * 128:(j + 1) * 128, :], in_=h2b[0]), reads=["h2b0"], writes=["h2d"], dma=True)
            yield
            lg = small[:, 44:60]
            for part in range(2):
                for k4 in range(4):
                    kc = part * 4 + k4
                    S.op("pe", lambda e, kc=kc, k4=k4: e.transpose(
                        out=pb[5][:, k4 * 128:(k4 + 1) * 128], in_=h2f[0][:, kc * 128:(kc + 1) * 128], identity=identf),
                        reads=["h2f0", "identf"], writes=["pb5"])
                eng = "act" if part == 0 else "dve"
                if eng == "act":
                    S.op("act", lambda e: e.copy(out=h2T, in_=pb[5].rearrange("p (k c) -> p k c", k=4)), reads=["pb5"], writes=["h2T"])
                else:
                    S.op("dve", lambda e: e.tensor_copy(out=h2T, in_=pb[5].rearrange("p (k c) -> p k c", k=4)), reads=["pb5"], writes=["h2T"])
                for k4 in range(4):
                    kc = part * 4 + k4
                    S.op("pe", lambda e, kc=kc, k4=k4: e.matmul(pb[5][:, 0:NE], lhsT=h2T[:, k4, :], rhs=wr[:, kc, :], start=(k4 == 0), stop=(k4 == 3)),
                         reads=["h2T", "wr"], writes=["pb5"])
                if part == 0:
                    S.op("dve", lambda e: e.tensor_copy(out=lg, in_=pb[5][:, 0:NE]), reads=["pb5"], writes=["lg"])
                else:
                    S.op("dve", lambda e: e.tensor_tensor(out=lg, in0=pb[5][:, 0:NE], in1=lg, op=ALU.add), reads=["pb5", "lg"], writes=["lg"])
                yield
            S.op("dve", lambda e: e.reduce_max(out=small[:, 28:29], in_=lg, axis=AX.X), reads=["lg"], writes=["lmax"])
            S.op("dve", lambda e: e.tensor_scalar(out=small[:, 29:30], in0=small[:, 28:29], scalar1=-1.0, scalar2=None, op0=ALU.mult),
                 reads=["lmax"], writes=["nlmax"])
            S.op("act", lambda e: e.activation(out=aff[:, j, :], in_=lg, func=AF.Exp, bias=small[:, 29:30], scale=1.0,
                                               accum_out=small[:, 30:31]),
                 reads=["lg", "nlmax"], writes=[f"aff{j}", "lsum"])
            S.op("dve", lambda e: e.reciprocal(out=small[:, 31:32], in_=small[:, 30:31]), reads=["lsum"], writes=["rlsum"])
            S.op("dve", lambda e: e.tensor_scalar(out=aff[:, j, :], in0=aff[:, j, :], scalar1=small[:, 31:32], scalar2=None, op0=ALU.mult),
                 reads=[f"aff{j}", "rlsum"], writes=[f"aff{j}"])
            yield

        def chain(gens):
            for gn in gens:
                yield from gn

        LOOK = 2
        pending = None
        for g in range(4 if level >= 2 else 0):
            steps = attn_steps(g)
            n = len(steps)
            every = 5
            for i in range(n + LOOK):
                if i < n:
                    steps[i][0]()
                if i >= LOOK:
                    steps[i - LOOK][1]()
                if pending is not None and i % every == every - 1:
                    if next(pending, "done") == "done":
                        pending = None
            if pending is not None:
                for _ in pending:
                    pass
            pending = chain([finish_tile(g, qi) for qi in range(4)])
        if pending is not None:
            for _ in pending:
                pass

        AFF_R = [f"aff{j}" for j in range(NO)]
        if debug and level == 0:
            aff_in = din("aff_in", [128, NO * NE])
            S.op("sp", lambda e: e.dma_start(out=aff.rearrange("p a b -> p (a b)"), in_=aff_in), writes=AFF_R, dma=True)
        if debug and level >= 2:
            S.op("sp", lambda e: e.dma_start(out=dbg_aff, in_=aff.rearrange("p a b -> p (a b)")), reads=AFF_R, dma=True)

        if not stop_after_phase1:
            S.barrier()
            AR.off = mark_persist
            afull = AR.alloc([128, 2, NO * NE], F32)
            cmpb = AR.alloc([128, 2 * NO, NE], F32)
            lo = AR.alloc([128, NE], F32)
            hi = AR.alloc([128, NE], F32)
            mid = AR.alloc([128, NE], F32)
            cntp = AR.alloc([128, NE], F32)
            sel = AR.alloc([128, NE], F32)
            tmpn = AR.alloc([128, NE], F32)
            msk = AR.alloc([128, NO, NE], F32)
            dest = AR.alloc([128, NO, NE], F32)
            offs = AR.alloc([128, NO, NE], F32)
            vals = AR.alloc([128, NO, 4], F32)
            selb = [AR.alloc([128, CAP], F32) for _ in range(2)]
            idxf = AR.alloc([128, 4, 4], F32)
            idxi = [[AR.alloc([128, 1], I32) for _c in range(4)] for _ in range(2)]
            gate = [AR.alloc([128, 4], F32) for _ in range(2)]
            xg = AR.alloc([128, 4, D], BF16)
            xeT = [AR.alloc([128, 8, CAP], BF16) for _ in range(2)]
            hTm = AR.alloc([128, 16, CAP], BF16)
            sa = [AR.alloc([128, CAP], F32) for _ in range(2)]
            yo = [AR.alloc([128, D], F32) for _ in range(2)]
            wgb = [AR.alloc([128, 8, 1024], BF16) for _ in range(2)]
            wub = [AR.alloc([128, 8, 1024], BF16) for _ in range(2)]
            wdb = [AR.alloc([128, 8, D], BF16) for _ in range(3)]
            print("phase2 sbuf bytes/partition:", AR.off)

            aff2 = aff.rearrange("p a b -> p (a b)")
            S.op("dve", lambda e: e.tensor_scalar(out=afull[:, 0, :], in0=aff2, scalar1=par[:, 0:1], scalar2=None, op0=ALU.mult),
                 reads=AFF_R + ["par"], writes=["afull0"])
            S.op("dve", lambda e: e.tensor_scalar(out=afull[:, 1, :], in0=aff2, scalar1=par[:, 1:2], scalar2=None, op0=ALU.mult),
                 reads=AFF_R + ["par"], writes=["afull1"])
            S.op("pool", lambda e: e.dma_start(out=cin_d[:, :], in_=afull.rearrange("p a b -> p (a b)")), reads=["afull0", "afull1"], writes=["cin"], dma=True)

            def cc(e):
                return e.collective_compute("AllReduce", ALU.add, replica_groups=[[0, 1], [2, 3], [4, 5], [6, 7]],
                                            ins=[cin_d.ap().opt()], outs=[cout_d.ap().opt()])
            S.cnt["cc"] = 0
            S.ops["pool"].append(([(k, v) for k, v in [S.last_w["cin"]]], None, None))
            S.waited["pool"][S.last_w["cin"][0]] = max(S.waited["pool"].get(S.last_w["cin"][0], 0), S.last_w["cin"][1])
            S.ops["pool"].append(([], cc, ("cc", 1)))
            S.last_w["cout"] = ("cc", 1)
            S.readers["cout"] = []
            S.all_tokens["cc"] = 1
            S.op("pool", lambda e: e.dma_start(out=afull.rearrange("p a b -> p (a b)"), in_=cout_d[:, :]), reads=["cout"], writes=["afull0", "afull1"], dma=True)
            AF_R = ["afull0", "afull1"]
            S.op("dve", lambda e: e.memset(lo, 0.0), writes=["lo"])
            S.op("dve", lambda e: e.memset(hi, 1.0), writes=["hi"])
            af3 = afull.rearrange("p a (j e) -> p (a j) e", e=NE)
            for it in range(26):
                S.op("dve", lambda e: e.tensor_tensor(out=mid, in0=lo, in1=hi, op=ALU.add), reads=["lo", "hi"], writes=["mid"])
                S.op("dve", lambda e: e.tensor_scalar(out=mid, in0=mid, scalar1=0.5, scalar2=None, op0=ALU.mult), reads=["mid"], writes=["mid"])
                S.op("dve", lambda e: e.tensor_tensor(out=cmpb, in0=af3, in1=mid.unsqueeze(1).to_broadcast([128, 2 * NO, NE]), op=ALU.is_ge),
                     reads=AF_R + ["mid"], writes=["cmpb"])
                S.op("dve", lambda e: e.tensor_reduce(out=cntp, in_=cmpb.rearrange("p s e -> p e s"), axis=AX.X, op=ALU.add), reads=["cmpb"], writes=["cntp"])
                S.op("pe", lambda e: e.matmul(pb[5][:, 0:NE], lhsT=ones, rhs=cntp, start=True, stop=True), reads=["ones", "cntp"], writes=["pb5"])
                S.op("dve", lambda e: e.tensor_scalar(out=sel, in0=pb[5][:, 0:NE], scalar1=511.5, scalar2=None, op0=ALU.is_ge), reads=["pb5"], writes=["sel"])
                S.op("dve", lambda e: e.tensor_tensor(out=tmpn, in0=mid, in1=lo, op=ALU.subtract), reads=["mid", "lo"], writes=["tmpn"])
                S.op("dve", lambda e: e.tensor_tensor(out=tmpn, in0=tmpn, in1=sel, op=ALU.mult), reads=["tmpn", "sel"], writes=["tmpn"])
                S.op("dve", lambda e: e.tensor_tensor(out=lo, in0=lo, in1=tmpn, op=ALU.add), reads=["lo", "tmpn"], writes=["lo"])
                S.op("dve", lambda e: e.tensor_tensor(out=tmpn, in0=hi, in1=mid, op=ALU.subtract), reads=["hi", "mid"], writes=["tmpn"])
                S.op("dve", lambda e: e.tensor_tensor(out=tmpn, in0=tmpn, in1=sel, op=ALU.mult), reads=["tmpn", "sel"], writes=["tmpn"])
                S.op("dve", lambda e: e.tensor_tensor(out=hi, in0=mid, in1=tmpn, op=ALU.add), reads=["mid", "tmpn"], writes=["hi"])
            if debug:
                S.op("sp", lambda e: e.dma_start(out=dbg_thr[:, 0:NE], in_=lo), reads=["lo"], dma=True)
                S.op("sp", lambda e: e.dma_start(out=dbg_thr[:, NE:2 * NE], in_=hi), reads=["hi"], dma=True)
            S.op("dve", lambda e: e.tensor_tensor(out=msk, in0=aff, in1=lo.unsqueeze(1).to_broadcast([128, NO, NE]), op=ALU.is_ge),
                 reads=AFF_R + ["lo"], writes=["msk"])
            mk2 = msk.rearrange("p a b -> p (a b)")
            S.op("pe", lambda e: e.matmul(pb[3][:, 0:NO * NE], lhsT=tri, rhs=mk2, start=True, stop=True), reads=["tri", "msk"], writes=["pb3"])
            S.op("pe", lambda e: e.matmul(pb[4][:, 0:NO * NE], lhsT=ones, rhs=mk2, start=True, stop=True), reads=["ones", "msk"], writes=["pb4"])
            tot = pb[4][:, 0:NO * NE].rearrange("p (a b) -> p a b", a=NO)
            S.op("dve", lambda e: e.memset(offs[:, 0, :], 0.0), writes=["offs"])
            for j in range(1, NO):
                S.op("dve", lambda e, j=j: e.tensor_tensor(out=offs[:, j, :], in0=offs[:, j - 1, :], in1=tot[:, j - 1, :], op=ALU.add),
                     reads=["offs", "pb4"], writes=["offs"])
            S.op("dve", lambda e: e.tensor_tensor(out=dest, in0=offs, in1=pb[3][:, 0:NO * NE].rearrange("p (a b) -> p a b", a=NO), op=ALU.add),
                 reads=["offs", "pb3"], writes=["dest"])
            S.op("dve", lambda e: e.tensor_scalar(out=msk, in0=msk, scalar1=-BIG, scalar2=BIG, op0=ALU.mult, op1=ALU.add), reads=["msk"], writes=["msk"])
            S.op("dve", lambda e: e.tensor_tensor(out=dest, in0=dest, in1=msk, op=ALU.add), reads=["dest", "msk"], writes=["dest"])
            S.op("dve", lambda e: e.tensor_copy(out=vals[:, :, 0:3], in_=tokc), reads=["tokc"], writes=["vals_c"])
            S.op("pool", lambda e: e.memset(xg, 0.0), writes=["xg0", "xg1", "xg2", "xg3"])

            wg_src = [wg_d[x].rearrange("(k p) f -> p k f", p=128) for x in range(n_exp)]
            wu_src = [wu_d[x].rearrange("(k p) f -> p k f", p=128) for x in range(n_exp)]
            wd_src = [wd_d[x].rearrange("(k p) c -> p k c", p=128) for x in range(n_exp)]

            def load_weights(x):
                for hf in range(2):
                    S.op("pool", lambda e, x=x, hf=hf: e.dma_start(out=wgb[hf], in_=wg_src[x][:, :, hf * 1024:(hf + 1) * 1024]),
                         writes=[f"wg{hf}"], dma=True)
                    S.op("pool", lambda e, x=x, hf=hf: e.dma_start(out=wub[hf], in_=wu_src[x][:, :, hf * 1024:(hf + 1) * 1024]),
                         writes=[f"wu{hf}"], dma=True)
                for hf in range(2):
                    bi = (2 * x + hf) % 3
                    S.op("pool", lambda e, x=x, hf=hf, bi=bi: e.dma_start(out=wdb[bi], in_=wd_src[x][:, hf * 8:(hf + 1) * 8, :]),
                         writes=[f"wd{bi}"], dma=True)

            CH = [(0, 128), (128, 128), (256, 128), (384, 128)]
            _DBG["g"] = (xg, h2_d, idxi)
            load_weights(0)
            for x in range(n_exp):
                pp = x % 2
                S.op("dve", lambda e, x=x: e.tensor_copy(out=vals[:, :, 3], in_=aff[:, :, x]), reads=AFF_R, writes=["vals_g"])
                for j in range(NO):
                    sb_ = selb[j % 2]
                    S.op("dve", lambda e, j=j, x=x, sb_=sb_: e.tensor_scalar(out=sb_, in0=iota, scalar1=dest[:, j, x:x + 1], scalar2=None, op0=ALU.is_equal),
                         reads=["iota", "dest"], writes=[f"selb{j % 2}"])
                    for c, (s0, sn) in enumerate(CH):
                        IB = [5, 4, 6, 7]
                        S.op("pe", lambda e, j=j, c=c, s0=s0, sn=sn, sb_=sb_: e.matmul(
                            pb[IB[c]][0:sn, 0:4], lhsT=sb_[:, s0:s0 + sn], rhs=vals[:, j, :], start=(j == 0), stop=(j == NO - 1)),
                            reads=[f"selb{j % 2}", "vals_c", "vals_g"], writes=[f"pb{IB[c]}"])
                for c in range(4):
                    S.op("act", lambda e, c=c: e.copy(out=idxf[:, c, :], in_=pb[[5, 4, 6, 7][c]][:, 0:4]), reads=[f"pb{[5, 4, 6, 7][c]}"], writes=["idxf"])
                S.op("dve", lambda e: e.tensor_scalar(out=idxf[:, :, 0], in0=idxf[:, :, 0], scalar1=64.0, scalar2=None, op0=ALU.mult), reads=["idxf"], writes=["idxf"])
                S.op("dve", lambda e: e.tensor_tensor(out=idxf[:, :, 0], in0=idxf[:, :, 0], in1=idxf[:, :, 1], op=ALU.add), reads=["idxf"], writes=["idxf"])
                S.op("dve", lambda e: e.tensor_scalar(out=idxf[:, :, 2], in0=idxf[:, :, 2], scalar1=-BIG, scalar2=BIG, op0=ALU.mult, op1=ALU.add), reads=["idxf"], writes=["idxf"])
                S.op("dve", lambda e: e.tensor_tensor(out=idxf[:, :, 0], in0=idxf[:, :, 0], in1=idxf[:, :, 2], op=ALU.add), reads=["idxf"], writes=["idxf"])
                for c in range(4):
                    S.op("dve", lambda e, pp=pp, c=c: e.tensor_copy(out=idxi[pp][c], in_=idxf[:, c, 0:1]), reads=["idxf"], writes=[f"idxi{pp}"])
                S.op("dve", lambda e, pp=pp: e.tensor_copy(out=gate[pp], in_=idxf[:, :, 3]), reads=["idxf"], writes=[f"gate{pp}"])
                for c, (s0, sn) in enumerate(CH):
                    S.op("pool", lambda e, c=c, sn=sn, pp=pp: e.indirect_dma_start(
                        out=xg[0:sn, c, :], out_offset=None, in_=h2_d[:, :],
                        in_offset=bass.IndirectOffsetOnAxis(ap=idxi[pp][c][0:sn, :], axis=0),
                        bounds_check=_bc(e, OWN - 1), oob_is_err=False),
                        reads=[f"idxi{pp}", "h2d"], writes=[f"xg{c}"], dma=True)
                pT = pbh[5].rearrange("p (k c) -> p k c", k=8)
                for c, (s0, sn) in enumerate(CH):
                    for kc in range(8):
                        S.op("pe", lambda e, c=c, kc=kc, sn=sn: e.transpose(out=pT[:, kc, 0:sn], in_=xg[0:sn, c, kc * 128:(kc + 1) * 128], identity=ident[0:sn, 0:sn]),
                             reads=[f"xg{c}", "ident"], writes=["pb5"])
                    eng = "act" if c % 2 == 0 else "dve"
                    if eng == "act":
                        S.op("act", lambda e, s0=s0, sn=sn, pp=pp: e.copy(out=xeT[pp][:, :, s0:s0 + sn], in_=pT[:, :, 0:sn]), reads=["pb5"], writes=[f"xeT{pp}_{c}"])
                    else:
                        S.op("dve", lambda e, s0=s0, sn=sn, pp=pp: e.tensor_copy(out=xeT[pp][:, :, s0:s0 + sn], in_=pT[:, :, 0:sn]), reads=["pb5"], writes=[f"xeT{pp}_{c}"])
                XE_R = [f"xeT{pp}_{c}" for c in range(4)]
                for fc in range(16):
                    hf = fc // 8
                    ba = (2 * fc) % 4
                    bu = (2 * fc + 1) % 4
                    for kc in range(8):
                        S.op("pe", lambda e, kc=kc, fc=fc, hf=hf, ba=ba, pp=pp: e.matmul(
                            pb[ba][:, 0:CAP], lhsT=wgb[hf][:, kc, (fc % 8) * 128:(fc % 8 + 1) * 128], rhs=xeT[pp][:, kc, :], start=(kc == 0), stop=(kc == 7)),
                            reads=[f"wg{hf}"] + XE_R, writes=[f"pb{ba}"])
                    for kc in range(8):
                        S.op("pe", lambda e, kc=kc, fc=fc, hf=hf, bu=bu, pp=pp: e.matmul(
                            pb[bu][:, 0:CAP], lhsT=wub[hf][:, kc, (fc % 8) * 128:(fc % 8 + 1) * 128], rhs=xeT[pp][:, kc, :], start=(kc == 0), stop=(kc == 7)),
                            reads=[f"wu{hf}"] + XE_R, writes=[f"pb{bu}"])
                    S.op("act", lambda e, fc=fc, ba=ba: e.activation(out=sa[fc % 2], in_=pb[ba][:, 0:CAP], func=AF.Silu), reads=[f"pb{ba}"], writes=[f"sa{fc % 2}"])
                    S.op("dve", lambda e, fc=fc, bu=bu: e.tensor_tensor(out=hTm[:, fc, :], in0=pb[bu][:, 0:CAP], in1=sa[fc % 2], op=ALU.mult),
                         reads=[f"pb{bu}", f"sa{fc % 2}"], writes=[f"hTm{fc}"])
                    if fc == 7 and x + 1 < n_exp:
                        S.op("pool", lambda e, x=x: e.dma_start(out=wgb[0], in_=wg_src[x + 1][:, :, 0:1024]), writes=["wg0"], dma=True)
                        S.op("pool", lambda e, x=x: e.dma_start(out=wub[0], in_=wu_src[x + 1][:, :, 0:1024]), writes=["wu0"], dma=True)
                if x + 1 < n_exp:
                    S.op("pool", lambda e, x=x: e.dma_start(out=wgb[1], in_=wg_src[x + 1][:, :, 1024:2048]), writes=["wg1"], dma=True)
                    S.op("pool", lambda e, x=x: e.dma_start(out=wub[1], in_=wu_src[x + 1][:, :, 1024:2048]), writes=["wu1"], dma=True)
                H_R = [f"hTm{fc}" for fc in range(16)]
                for c, (s0, sn) in enumerate(CH):
                    yb = (4 * x + c) % 2
                    for hf2 in range(2):
                        bank = 6 + hf2
                        for fc in range(16):
                            bi = (2 * x + fc // 8) % 3
                            S.op("pe", lambda e, fc=fc, bi=bi, s0=s0, sn=sn, hf2=hf2, bank=bank: e.matmul(
                                pb[bank][0:sn, :], lhsT=hTm[:, fc, s0:s0 + sn], rhs=wdb[bi][:, fc % 8, hf2 * 512:(hf2 + 1) * 512],
                                start=(fc == 0), stop=(fc == 15)),
                                reads=[f"hTm{fc}", f"wd{bi}"], writes=[f"pb{bank}"])
                        if hf2 == 0:
                            S.op("act", lambda e, sn=sn, yb=yb, c=c, pp=pp, bank=bank: e.activation(
                                out=yo[yb][0:sn, 0:512], in_=pb[bank][0:sn, :], func=AF.Copy, scale=gate[pp][0:sn, c:c + 1]),
                                reads=[f"pb{bank}", f"gate{pp}"], writes=[f"yo{yb}_0"])
                        else:
                            S.op("dve", lambda e, sn=sn, yb=yb, c=c, pp=pp, bank=bank: e.tensor_scalar(
                                out=yo[yb][0:sn, 512:1024], in0=pb[bank][0:sn, :], scalar1=gate[pp][0:sn, c:c + 1], scalar2=None, op0=ALU.mult),
                                reads=[f"pb{bank}", f"gate{pp}"], writes=[f"yo{yb}_1"])
                    prev = ["acc"] if x == 0 else [f"accw{x - 1}_{k}" for k in range(4)]
                    S.op("pool", lambda e, yb=yb, c=c, pp=pp: e.indirect_dma_start(
                        out=acc_d[:, :], out_offset=bass.IndirectOffsetOnAxis(ap=idxi[pp][c][:, :], axis=0),
                        in_=yo[yb][:, :], in_offset=None, bounds_check=_bc(e, OWN - 1), oob_is_err=False, compute_op=ALU.add),
                        reads=[f"yo{yb}_0", f"yo{yb}_1", f"idxi{pp}"] + prev, writes=[f"accw{x}_{c}"], dma=True)
                if x + 1 < n_exp:
                    for hf in range(2):
                        bi = (2 * (x + 1) + hf) % 3
                        S.op("pool", lambda e, x=x, hf=hf, bi=bi: e.dma_start(out=wdb[bi], in_=wd_src[x + 1][:, hf * 8:(hf + 1) * 8, :]),
                             writes=[f"wd{bi}"], dma=True)

        S.barrier()
        AR.off = mark_persist
        fin = [AR.alloc([128, D], F32) for _ in range(2)]
        fjunk = AR.alloc([128, D], F32)
        ACC_FINAL = ["acc"] if stop_after_phase1 else [f"accw{n_exp - 1}_{k}" for k in range(4)]
        for j in range(NO):
            b = j % 2
            S.op("sp", lambda e, j=j, b=b: e.dma_start(out=fin[b], in_=acc_d[j * 128:(j + 1) * 128, :]), reads=ACC_FINAL, writes=[f"fin{b}"], dma=True)
            S.op("act", lambda e, b=b: e.activation(out=fjunk, in_=fin[b], func=AF.Square, scale=1.0 / 32, accum_out=small[:, 40:41]),
                 reads=[f"fin{b}"], writes=["junkf", "fss"])
            S.op("act", lambda e: e.activation(out=small[:, 41:42], in_=small[:, 40:41], func=AF.Sqrt, bias=EPS), reads=["fss"], writes=["frs"])
            S.op("dve", lambda e: e.reciprocal(out=small[:, 42:43], in_=small[:, 41:42]), reads=["frs"], writes=["frstd"])
            S.op("dve", lambda e, b=b: e.scalar_tensor_tensor(out=fin[b], in0=fin[b], scalar=small[:, 42:43], in1=gfin, op0=ALU.mult, op1=ALU.mult),
                 reads=[f"fin{b}", "frstd", "gfin"], writes=[f"fin{b}"])
            S.op("sp", lambda e, j=j, b=b: e.dma_start(out=out_d[j * 128:(j + 1) * 128, :], in_=fin[b]), reads=[f"fin{b}"], dma=True)
        S.barrier()
        sems["cc"] = es.enter_context(nc.semaphore("sem_cc"))
        S.emit(sems)
    return nc


def _weight_mask(delta):
    a = np.abs(delta)
    w = (a <= 64).astype(np.float32)
    w += ((a <= 256) & (delta % 4 == 0)).astype(np.float32)
    w += ((a <= 1024) & (delta % 16 == 0)).astype(np.float32)
    return w


def _consts():
    bf = ml_dtypes.bfloat16
    kk = np.arange(128)[:, None]
    qq = np.arange(128)[None, :]
    mA = np.concatenate([(kk >= qq), np.ones((128, 128), bool), (kk <= qq)], axis=1).astype(np.float32)
    blocks = []
    for o in range(23):
        d = 128 * (11 - o) + kk - qq
        blocks.append(_weight_mask(d))
    mB = np.concatenate(blocks, axis=1)
    tri = (kk < qq).astype(np.float32)
    tok = (np.arange(NO)[None, :] * 128 + np.arange(128)[:, None])
    tokc = np.stack([tok // 64, tok % 64, np.ones_like(tok)], axis=-1).astype(np.float32).reshape(128, NO * 3)
    return {
        "ident": np.eye(128).astype(bf),
        "identf": np.eye(128, dtype=np.float32),
        "tri": tri,
        "ones": np.ones((128, 128), np.float32),
        "maskA": mA.astype(bf),
        "maskB": mB.astype(bf),
        "iota": np.arange(CAP, dtype=np.float32).reshape(1, CAP),
        "tokc": np.ascontiguousarray(tokc),
    }


def _positions(c):
    half = c % 2
    i = np.arange(WIN)
    return i if half == 0 else (SEQ - 1 - i)


def _in_maps(x, g_mix, w_in, a_sink, g_out_a, g_out_b, w_out, g_ffn, w_router, w_gate, w_up, w_down, g_final, n_exp=NE):
    consts = _consts()
    inv_freq = (np.float32(500000.0) ** (-np.arange(0, 16, 2, dtype=np.float32) / np.float32(16))).astype(np.float32)
    sink = np.asarray(a_sink[0], np.float32)
    perm = [0, 2, 1, 3, 4, 6, 5, 7]
    shared = {
        "g_mix": np.ascontiguousarray(g_mix[0:1]), "g_ffn": np.ascontiguousarray(g_ffn[0:1]),
        "g_final": np.ascontiguousarray(np.asarray(g_final).reshape(1, D)),
        "g_out": np.ascontiguousarray(np.concatenate([g_out_a[0], g_out_b[0]])[None, :]),
        "w_in": np.ascontiguousarray(w_in[0]), "w_out": np.ascontiguousarray(w_out[0]),
        "w_router": np.ascontiguousarray(w_router[0]),
        "w_gate": np.ascontiguousarray(w_gate[0][:n_exp]), "w_up": np.ascontiguousarray(w_up[0][:n_exp]),
        "w_down": np.ascontiguousarray(w_down[0][:n_exp]),
        "sinkp": np.ascontiguousarray(sink[perm][None, :]),
    }
    shared.update(consts)
    maps = []
    for c in range(8):
        pos = _positions(c)
        ang = pos.astype(np.float32)[:, None] * inv_freq[None, :]
        m = dict(shared)
        m["xw"] = np.ascontiguousarray(x[c // 2][pos])
        m["cosw"] = np.cos(ang).astype(np.float32)
        m["sinw"] = np.sin(ang).astype(np.float32)
        m["par"] = np.array([[1.0, 0.0]] if c % 2 == 0 else [[0.0, 1.0]], np.float32)
        maps.append(m)
    return maps


_NC_CACHE = {}


def kernel(x, g_mix, w_in, a_sink, g_out_a, g_out_b, w_out, g_ffn, w_router, w_gate, w_up, w_down, g_final):
    args = [np.asarray(a) for a in (x, g_mix, w_in, a_sink, g_out_a, g_out_b, w_out, g_ffn, w_router, w_gate, w_up, w_down, g_final)]
    if "nc" not in _NC_CACHE:
        _NC_CACHE["nc"] = build()
    nc = _NC_CACHE["nc"]
    maps = _in_maps(*args)
    res = run_bass_kernel_spmd(nc, maps, core_ids=list(range(8)))
    out = np.empty((4, SEQ, D), np.float32)
    for c in range(8):
        pos = _positions(c)[:OWN]
        out[c // 2][pos] = res.results[c]["out"]
    return out
```

```python
import numpy as np
import ml_dtypes
from contextlib import ExitStack
import concourse.bass as bass
import concourse.mybir as mybir
from concourse.bass_utils import run_bass_kernel_spmd

F32 = mybir.dt.float32
I32 = mybir.dt.int32
BF16 = mybir.dt.bfloat16
ALU = mybir.AluOpType
AF = mybir.ActivationFunctionType
AX = mybir.AxisListType

D = 1024
SEQ = 4096
WIN = 3072
OWN = 2048
NT = 24
NO = 16
NE = 16
FF = 2048
CAP = 512
EPS = 1e-6
BIG = 1.0e6
ENGS = ["pe", "act", "dve", "pool", "sp"]


class Sched:
    def __init__(self, nc, n_dma=32, same_engine_wait=("act", "dve", "pool")):
        self.nc = nc
        self.ops = {e: [] for e in ENGS}
        self.cnt = {e: 0 for e in ENGS}
        self.waited = {e: {} for e in ENGS}
        self.last_w = {}
        self.readers = {}
        self.n_dma = n_dma
        self.dma_val = [0] * n_dma
        self.dma_rr = {"pool": 0, "sp": 0, "act": 0}
        self.same = set(same_engine_wait)
        self.all_tokens = {}

    def op(self, eng, fn, reads=(), writes=(), dma=False):
        writes = list(writes) + [r for r in reads if r.startswith("pb")]
        reads = [r for r in reads if not r.startswith("pb")]
        deps = set()
        for r in reads:
            if r in self.last_w:
                deps.add(self.last_w[r])
        for w in writes:
            if w in self.last_w:
                deps.add(self.last_w[w])
            for t in self.readers.get(w, ()):
                deps.add(t)
        if dma:
            half = self.n_dma // 2
            base = 0 if eng == "pool" else half
            k = base + self.dma_rr[eng]
            self.dma_rr[eng] = (self.dma_rr[eng] + 1) % half
            if self.dma_val[k] > 0:
                deps.add((("dma", k), self.dma_val[k]))
            self.dma_val[k] += 16
            token = (("dma", k), self.dma_val[k])
        else:
            self.cnt[eng] += 1
            token = (eng, self.cnt[eng])
        waits = []
        wd = self.waited[eng]
        mx = {}
        for key, val in deps:
            if mx.get(key, 0) < val:
                mx[key] = val
        for key, val in sorted(mx.items(), key=lambda t: str(t[0])):
            if key == eng and eng not in self.same:
                continue
            if wd.get(key, 0) < val:
                wd[key] = val
                waits.append((key, val))
        self.ops[eng].append((waits, fn, token))
        for r in reads:
            self.readers.setdefault(r, []).append(token)
        for w in writes:
            self.last_w[w] = token
            self.readers[w] = []
        self.all_tokens[token[0]] = max(self.all_tokens.get(token[0], 0), token[1])
        return token

    def barrier(self, engs=ENGS):
        toks = dict(self.all_tokens)
        for e in engs:
            waits = []
            wd = self.waited[e]
            for key, val in toks.items():
                if key == e:
                    continue
                if wd.get(key, 0) < val:
                    wd[key] = val
                    waits.append((key, val))
            if waits:
                self.ops[e].append((waits, None, None))

    def emit(self, semaphores):
        nc = self.nc

        def run(ename):
            def body(eng):
                for waits, fn, token in self.ops[ename]:
                    for key, val in waits:
                        eng.wait_ge(semaphores[key], val)
                    if fn is None:
                        continue
                    ins = fn(eng)
                    key = token[0]
                    if isinstance(key, tuple):
                        ins.then_inc(semaphores[key], 16)
                    else:
                        ins.then_inc(semaphores[key], 1)
            return body

        with nc.Block() as block:
            block.tensor(run("pe"))
            block.scalar(run("act"))
            block.vector(run("dve"))
            block.gpsimd(run("pool"))
            block.sync(run("sp"))


import os
SUB = int(os.environ.get('KSUB', '9'))
LOG = []
_DBG = {}
_BCREG = {}


def _bc(eng, val):
    key = (id(eng), val)
    if key not in _BCREG:
        _BCREG[key] = eng.to_reg(val)
    return _BCREG[key]


class Arena:
    def __init__(self, big, nbytes):
        self.big = big
        self.nbytes = nbytes
        self.off = 0

    def alloc(self, shape, dt):
        esz = 2 if dt == BF16 else 4
        n = int(np.prod(shape[1:]))
        nb = (n * esz + 63) // 64 * 64
        o = self.off
        self.off += nb
        LOG.append((o, tuple(shape), str(dt)))
        assert self.off <= self.nbytes, (self.off, self.nbytes)
        v = self.big[:, o // 4:(o + nb) // 4]
        if dt != F32:
            v = v.bitcast(dt)
        v = v[:, 0:n]
        if len(shape) == 3:
            v = v.rearrange("p (a b) -> p a b", a=shape[1])
        elif len(shape) == 4:
            v = v.rearrange("p (a b c) -> p a b c", a=shape[1], b=shape[2])
        return v


def build(n_exp=NE, debug=False, stop_after_phase1=False, level=9):
    nc = bass.Bass("TRN2", target_bir_lowering=False)
    _BCREG.clear()

    def din(name, shape, dt=F32):
        return nc.dram_tensor(name, list(shape), dt, kind="ExternalInput").ap()

    xw = din("xw", [WIN, D])
    cosw = din("cosw", [WIN, 8])
    sinw = din("sinw", [WIN, 8])
    gmix_d = din("g_mix", [1, D])
    gffn_d = din("g_ffn", [1, D])
    gfin_d = din("g_final", [1, D])
    gout_d = din("g_out", [1, D])
    win_d = din("w_in", [D, 2304])
    wout_d = din("w_out", [D, D])
    wr_d = din("w_router", [D, NE])
    wg_d = din("w_gate", [n_exp, D, FF])
    wu_d = din("w_up", [n_exp, D, FF])
    wd_d = din("w_down", [n_exp, FF, D])
    sink_d = din("sinkp", [1, 8])
    par_d = din("par", [1, 2])
    ident_d = din("ident", [128, 128], BF16)
    identf_d = din("identf", [128, 128])
    tri_d = din("tri", [128, 128])
    ones_d = din("ones", [128, 128])
    maskA_d = din("maskA", [128, 3 * 128], BF16)
    maskB_d = din("maskB", [128, 23 * 128], BF16)
    iota_d = din("iota", [1, CAP])
    tokc_d = din("tokc", [128, NO * 3])
    out_d = nc.dram_tensor("out", [OWN, D], F32, kind="ExternalOutput").ap()
    if debug:
        dbg_x1 = nc.dram_tensor("dbg_x1", [OWN, D], F32, kind="ExternalOutput").ap()
        dbg_aff = nc.dram_tensor("dbg_aff", [128, NO * NE], F32, kind="ExternalOutput").ap()
        dbg_thr = nc.dram_tensor("dbg_thr", [128, 2 * NE], F32, kind="ExternalOutput").ap()

    acc_d = nc.dram_tensor("acc", [OWN, D], F32).ap()
    h2_d = nc.dram_tensor("h2buf", [OWN, D], BF16).ap()
    cin_d = nc.dram_tensor("cin", [128, 2 * NO * NE], F32)
    cout_d = nc.dram_tensor("cout", [128, 2 * NO * NE], F32)

    S = Sched(nc)
    with ExitStack() as es:
        SB_BYTES = 196 * 1024
        big = es.enter_context(nc.sbuf_tensor("big", [128, SB_BYTES // 4], F32))
        AR = Arena(big, SB_BYTES)
        pbt = [es.enter_context(nc.psum_tensor(f"pb{i}", [128, 512], F32)) for i in range(8)]
        pb = [t[:, :] for t in pbt]
        pbh = [t[:, :].bitcast(BF16) for t in pbt]
        sems = {e: es.enter_context(nc.semaphore("sem_" + e)) for e in ENGS}
        for k in range(S.n_dma):
            sems[("dma", k)] = es.enter_context(nc.semaphore(f"dsem{k}"))

        ident = AR.alloc([128, 128], BF16)
        identf = AR.alloc([128, 128], F32)
        tri = AR.alloc([128, 128], F32)
        ones = AR.alloc([128, 128], F32)
        iota = AR.alloc([128, CAP], F32)
        tokc = AR.alloc([128, NO, 3], F32)
        par = AR.alloc([128, 2], F32)
        gfin = AR.alloc([128, D], F32)
        gffn = AR.alloc([128, D], F32)
        aff = AR.alloc([128, NO, NE], F32)
        small = AR.alloc([128, 64], F32)
        mark_persist = AR.off

        def ld(eng, dst, src, name, rd=()):
            S.op(eng, lambda e: e.dma_start(out=dst, in_=src), reads=rd, writes=[name], dma=True)

        ld("sp", ident, ident_d, "ident")
        ld("sp", identf, identf_d, "identf")
        ld("sp", tri, tri_d, "tri")
        ld("sp", ones, ones_d, "ones")
        ld("sp", iota, iota_d.partition_broadcast(128), "iota")
        ld("sp", tokc, tokc_d.rearrange("p (a b) -> p a b", a=NO), "tokc")
        ld("sp", par, par_d.partition_broadcast(128), "par")
        ld("sp", gfin, gfin_d.partition_broadcast(128), "gfin")
        ld("sp", gffn, gffn_d.partition_broadcast(128), "gffn")

        esink = AR.alloc([128, 8], F32)
        maskA = AR.alloc([128, 3 * 128], BF16)
        maskB = AR.alloc([128, 23 * 128], BF16)
        qaT = AR.alloc([128, 4, OWN], BF16)
        kaT = AR.alloc([128, 2, 17 * 128], BF16)
        qbT = AR.alloc([128, 4, OWN], BF16)
        kbT = AR.alloc([128, 4, WIN], BF16)
        vA = AR.alloc([128, 17, 2 * 65], BF16)
        vB = AR.alloc([128, NT, 8 * 65], BF16)
        xbuf = [AR.alloc([128, D], F32) for _ in range(2)]
        junk = AR.alloc([128, D], F32)
        hT = [AR.alloc([128, 8, 128], BF16) for _ in range(2)]
        mark_1a = AR.off
        gmix = AR.alloc([128, D], F32)
        cos_t = AR.alloc([128, NT, 8], F32)
        sin_t = AR.alloc([128, NT, 8], F32)
        win = AR.alloc([128, 8, 2304], BF16)
        xn = [AR.alloc([128, D], BF16) for _ in range(2)]
        qk_tm = [AR.alloc([128, 14 * 128], BF16) for _ in range(2)]
        tmpA = AR.alloc([128, 8 * 16], F32)
        tmpB = AR.alloc([128, 8 * 16], F32)
        print("phase1 sbuf bytes/partition:", AR.off)

        ld("sp", gmix, gmix_d.partition_broadcast(128), "gmix")
        ld("sp", cos_t, cosw.rearrange("(j p) d -> p j d", p=128), "cos")
        ld("sp", sin_t, sinw.rearrange("(j p) d -> p j d", p=128), "sin")
        ld("sp", esink, sink_d.partition_broadcast(128), "esink")
        ld("sp", maskA, maskA_d, "maskA")
        ld("sp", maskB, maskB_d, "maskB")
        win_src = win_d.rearrange("(k p) c -> p k c", p=128)
        for k0 in range(0, 8, 2):
            S.op("pool", lambda e, k0=k0: e.dma_start(out=win[:, k0:k0 + 2, :], in_=win_src[:, k0:k0 + 2, :]),
                 writes=[f"win{k0}"], dma=True)
        WIN_R = [f"win{k0}" for k0 in range(0, 8, 2)]
        S.op("act", lambda e: e.activation(out=esink, in_=esink, func=AF.Exp), reads=["esink"], writes=["esink"])
        vA4 = vA.rearrange("p j (h d) -> p j h d", h=2)
        vB4 = vB.rearrange("p j (h d) -> p j h d", h=8)
        S.op("pool", lambda e: e.memset(vA4[:, :, :, 64:65], 1.0), writes=["vA_ones"])
        S.op("pool", lambda e: e.memset(vB4[:, :, :, 64:65], 1.0), writes=["vB_ones"])

        psel = [0]

        def rope(j, pg, c0, H, dst, dups):
            if os.environ.get("KROPE", "1") == "0":
                return
            v = pg[:, c0:c0 + H * 64].rearrange("p (h d) -> p h d", h=H)
            x1 = v[:, :, 0:8]
            x2 = v[:, :, 8:16]
            cb = cos_t[:, j, :].unsqueeze(1).to_broadcast([128, H, 8])
            sb_ = sin_t[:, j, :].unsqueeze(1).to_broadcast([128, H, 8])
            t1c = tmpA[:, 0:H * 8].rearrange("p (h d) -> p h d", h=H)
            t2c = tmpA[:, 64:64 + H * 8].rearrange("p (h d) -> p h d", h=H)
            t1s = tmpB[:, 0:H * 8].rearrange("p (h d) -> p h d", h=H)
            t2s = tmpB[:, 64:64 + H * 8].rearrange("p (h d) -> p h d", h=H)
            pgn = dups["pg"]
            S.op("dve", lambda e: e.tensor_tensor(out=t1c, in0=x1, in1=cb, op=ALU.mult), reads=[pgn, "cos"], writes=["t1c"])
            S.op("dve", lambda e: e.tensor_tensor(out=t2c, in0=x2, in1=cb, op=ALU.mult), reads=[pgn, "cos"], writes=["t2c"])
            S.op("dve", lambda e: e.tensor_tensor(out=t1s, in0=x1, in1=sb_, op=ALU.mult), reads=[pgn, "sin"], writes=["t1s"])
            S.op("dve", lambda e: e.tensor_tensor(out=t2s, in0=x2, in1=sb_, op=ALU.mult), reads=[pgn, "sin"], writes=["t2s"])
            for dv, dn in dst:
                S.op("dve", lambda e, dv=dv: e.tensor_tensor(out=dv[:, :, 0:8], in0=t1c, in1=t2s, op=ALU.subtract),
                     reads=["t1c", "t2s"], writes=[dn + "_a"])
                S.op("dve", lambda e, dv=dv: e.tensor_tensor(out=dv[:, :, 8:16], in0=t2c, in1=t1s, op=ALU.add),
                     reads=["t2c", "t1s"], writes=[dn + "_b"])
                if os.environ.get("KROPE", "3") == "2":
                    continue
                S.op("act", lambda e, dv=dv: e.copy(out=dv[:, :, 16:64], in_=v[:, :, 16:64]), reads=[pgn], writes=[dn + "_c"])

        def proj_group(j, hTj, c0, ncols, bank):
            for kc in range(8):
                S.op("pe", lambda e, kc=kc: e.matmul(pb[bank][:, 0:ncols], lhsT=hTj[:, kc, :], rhs=win[:, kc, c0:c0 + ncols],
                                                     start=(kc == 0), stop=(kc == 7)),
                     reads=[f"hT{j % 2}"] + WIN_R, writes=[f"pb{bank}"])

        def tile_gen(j):
            b = j % 2
            own = j < NO
            xt = xbuf[b]
            S.op("sp", lambda e, j=j, xt=xt: e.dma_start(out=xt, in_=xw[j * 128:(j + 1) * 128, :]), writes=[f"x{b}"], dma=True)
            S.op("act", lambda e, xt=xt: e.activation(out=junk, in_=xt, func=AF.Square, scale=1.0 / 32, accum_out=small[:, 0:1]),
                 reads=[f"x{b}"], writes=["junk", "ss"])
            S.op("act", lambda e: e.activation(out=small[:, 1:2], in_=small[:, 0:1], func=AF.Sqrt, bias=EPS), reads=["ss"], writes=["rs"])
            S.op("dve", lambda e: e.reciprocal(out=small[:, 2:3], in_=small[:, 1:2]), reads=["rs"], writes=["rstd"])
            S.op("dve", lambda e, xt=xt, b=b: e.scalar_tensor_tensor(out=xn[b], in0=xt, scalar=small[:, 2:3], in1=gmix, op0=ALU.mult, op1=ALU.mult),
                 reads=[f"x{b}", "rstd", "gmix"], writes=[f"xn{b}"])
            pT = pbh[5].rearrange("p (k c) -> p k c", k=8)
            for kc in range(8):
                S.op("pe", lambda e, kc=kc, b=b: e.transpose(out=pT[:, kc, :], in_=xn[b][:, kc * 128:(kc + 1) * 128], identity=ident),
                     reads=[f"xn{b}", "ident"], writes=["pb5"])
            S.op("act", lambda e, b=b: e.copy(out=hT[b], in_=pT), reads=["pb5"], writes=[f"hT{b}"])
            yield
            qk = qk_tm[b]
            qkn = f"qk{b}"
            if own:
                bank = psel[0] % 3; psel[0] += 1
                proj_group(j, hT[b], 0, 512, bank)
                rope(j, pb[bank], 0, 8, [(qk[:, 0:512].rearrange("p (h d) -> p h d", h=8), qkn + "_qa")], {"pg": f"pb{bank}"})
            if j <= NO:
                bank = psel[0] % 3; psel[0] += 1
                proj_group(j, hT[b], 512, 256, bank)
                kd = qk[:, 512:768].rearrange("p (h t d) -> p h t d", h=2, t=2)
                rope(j, pb[bank], 0, 2, [(kd[:, :, 0, :], qkn + "_ka0"), (kd[:, :, 1, :], qkn + "_ka1")], {"pg": f"pb{bank}"})
                S.op("act", lambda e, j=j, bank=bank: e.copy(out=vA4[:, j, :, 0:64], in_=pb[bank][:, 128:256].rearrange("p (h d) -> p h d", h=2)),
                     reads=[f"pb{bank}"], writes=[f"vA{j}"])
            if own:
                bank = psel[0] % 3; psel[0] += 1
                proj_group(j, hT[b], 768, 512, bank)
                rope(j, pb[bank], 0, 8, [(qk[:, 768:1280].rearrange("p (h d) -> p h d", h=8), qkn + "_qb")], {"pg": f"pb{bank}"})
            bank = psel[0] % 3; psel[0] += 1
            proj_group(j, hT[b], 1280, 512, bank)
            rope(j, pb[bank], 0, 8, [(qk[:, 1280:1792].rearrange("p (h d) -> p h d", h=8), qkn + "_kb")], {"pg": f"pb{bank}"})
            bank = psel[0] % 3; psel[0] += 1
            proj_group(j, hT[b], 1792, 512, bank)
            S.op("dve", lambda e, j=j, bank=bank: e.tensor_copy(out=vB4[:, j, :, 0:64], in_=pb[bank].rearrange("p (h d) -> p h d", h=8)),
                 reads=[f"pb{bank}"], writes=[f"vB{j}"])
            yield
            qT3 = pbh[3].rearrange("p (k c) -> p k c", k=8)
            qT4 = pbh[4].rearrange("p (k c) -> p k c", k=8)

            def tr(dst_bank_view, slot, ft, rd, bankname, qk=qk):
                S.op("pe", lambda e: e.transpose(out=dst_bank_view[:, slot, :], in_=qk[:, ft * 128:(ft + 1) * 128], identity=ident),
                     reads=rd + ["ident"], writes=[bankname])
            rd_qa = [qkn + "_qa_a", qkn + "_qa_b", qkn + "_qa_c"]
            rd_ka = [qkn + s for s in ("_ka0_a", "_ka0_b", "_ka0_c", "_ka1_a", "_ka1_b", "_ka1_c")]
            rd_qb = [qkn + "_qb_a", qkn + "_qb_b", qkn + "_qb_c"]
            rd_kb = [qkn + "_kb_a", qkn + "_kb_b", qkn + "_kb_c"]
            if own:
                for t in range(4):
                    tr(qT3, t, t, rd_qa, "pb3")
            if j <= NO:
                for t in range(2):
                    tr(qT3, 4 + t, 4 + t, rd_ka, "pb3")
            if own:
                S.op("act", lambda e, j=j: e.copy(out=qaT[:, :, j * 128:(j + 1) * 128], in_=qT3[:, 0:4, :]), reads=["pb3"], writes=[f"qaT{j}"])
            if j <= NO:
                S.op("dve", lambda e, j=j: e.tensor_copy(out=kaT[:, :, j * 128:(j + 1) * 128], in_=qT3[:, 4:6, :]), reads=["pb3"], writes=[f"kaT{j}"])
            if own:
                for t in range(4):
                    tr(qT4, t, 6 + t, rd_qb, "pb4")
            for t in range(4):
                tr(qT4, 4 + t, 10 + t, rd_kb, "pb4")
            if own:
                S.op("act", lambda e, j=j: e.copy(out=qbT[:, :, j * 128:(j + 1) * 128], in_=qT4[:, 0:4, :]), reads=["pb4"], writes=[f"qbT{j}"])
            S.op("dve", lambda e, j=j: e.tensor_copy(out=kbT[:, :, j * 128:(j + 1) * 128], in_=qT4[:, 4:8, :]), reads=["pb4"], writes=[f"kbT{j}"])

        if level >= 1:
            gens = [tile_gen(j) for j in range(NT)]
            next(gens[0])
            for j in range(NT):
                if j + 1 < NT:
                    next(gens[j + 1])
                next(gens[j])
                if j >= 1:
                    next(gens[j - 1], None)
            next(gens[NT - 1], None)

        S.barrier()
        AR.off = mark_1a
        gout = AR.alloc([128, D], F32)
        wout = AR.alloc([128, 8, D], BF16)
        wr = AR.alloc([128, 8, NE], F32)
        NPB = 4
        Pb = [AR.alloc([128, 512], BF16) for _ in range(NPB)]
        o_all2 = AR.alloc([128, 2, 4 * D], BF16)
        mixed = [AR.alloc([128, D], BF16)] * 2
        x1t = [AR.alloc([128, D], F32)] * 2
        h2f = [AR.alloc([128, D], F32)] * 2
        h2b = [AR.alloc([128, D], BF16)] * 2
        h2T = AR.alloc([128, 4, 128], F32)
        print("phase1b sbuf bytes/partition:", AR.off)
        ld("sp", gout, gout_d.partition_broadcast(128), "gout")
        ld("sp", wr, wr_d.rearrange("(k p) c -> p k c", p=128), "wr")
        S.op("pool", lambda e: e.dma_start(out=wout, in_=wout_d.rearrange("(k p) c -> p k c", p=128)),
             writes=["wout"], dma=True)
        sbank = [0]
        pbuf = [0]
        OB = [3, 4, 6, 7]
        OBN = [f"pb{q}" for q in OB]
        HD = [0, 2, 1, 3]

        def o_view(g):
            return o_all2[:, g % 2, :].rearrange("p (q d) -> p q d", q=4)

        def attn_steps(g):
            steps = []
            o_all = o_view(g)
            gp = g % 2
            for kvh in range(2):
                for qi in range(4):
                    qb_ = 4 * g + qi
                    kbs = [kb for kb in (qb_ - 1, qb_, qb_ + 1) if 0 <= kb <= 16]
                    for kb in kbs:
                        st = {}

                        def s1(st=st, kb=kb, qb_=qb_, kvh=kvh):
                            pi = pbuf[0] % NPB; pbuf[0] += 1
                            st["pi"] = pi
                            P = Pb[pi]
                            for half in range(2):
                                sbk = sbank[0] % 3; sbank[0] += 1
                                r0 = half * 64
                                S.op("pe", lambda e, half=half, r0=r0, sbk=sbk: e.matmul(
                                    pb[sbk][:, 0:256].rearrange("p (a c) -> p a c", a=2),
                                    lhsT=kaT[r0:r0 + 64, kvh, kb * 128:(kb + 1) * 128],
                                    rhs=qaT[r0:r0 + 64, 2 * kvh:2 * kvh + 2, qb_ * 128:(qb_ + 1) * 128],
                                    start=True, stop=True),
                                    reads=[f"kaT{kb}", f"qaT{qb_}"], writes=[f"pb{sbk}"])
                                S.op("act", lambda e, sbk=sbk, half=half: e.activation(out=P[:, half * 256:(half + 1) * 256], in_=pb[sbk][:, 0:256], func=AF.Exp, scale=0.125),
                                     reads=[f"pb{sbk}"], writes=[f"P{pi}"])
                            if kb != qb_:
                                blk = 0 if kb < qb_ else 2
                                S.op("dve", lambda e: e.tensor_tensor(
                                    out=P.rearrange("p (h c) -> p h c", h=4), in0=P.rearrange("p (h c) -> p h c", h=4),
                                    in1=maskA[:, blk * 128:(blk + 1) * 128].unsqueeze(1).to_broadcast([128, 4, 128]), op=ALU.mult),
                                    reads=[f"P{pi}", "maskA"], writes=[f"P{pi}"])

                        def s2(st=st, kb=kb, kbs=kbs, kvh=kvh, qi=qi):
                            pi = st["pi"]
                            P = Pb[pi]
                            for hh in range(4):
                                S.op("pe", lambda e, hh=hh: e.matmul(
                                    pb[OB[hh]][:, 0:65], lhsT=P[:, hh * 128:(hh + 1) * 128], rhs=vA4[:, kb, kvh, :],
                                    start=(kb == kbs[0]), stop=(kb == kbs[-1])),
                                    reads=[f"P{pi}", f"vA{kb}", "vA_ones"], writes=[OBN[hh]])
                            if kb == kbs[-1]:
                                denA = small[:, 8:12]
                                for hh in range(4):
                                    S.op("dve", lambda e, hh=hh: e.tensor_tensor(out=denA[:, hh:hh + 1], in0=pb[OB[hh]][:, 64:65],
                                                                                 in1=esink[:, kvh * 4 + hh:kvh * 4 + hh + 1], op=ALU.add),
                                         reads=[OBN[hh], "esink"], writes=[f"den{hh}"])
                                    S.op("dve", lambda e, hh=hh: e.reciprocal(out=denA[:, hh:hh + 1], in_=denA[:, hh:hh + 1]), reads=[f"den{hh}"], writes=[f"den{hh}"])
                                    hd = kvh * 4 + HD[hh]
                                    S.op("dve", lambda e, hh=hh, hd=hd: e.tensor_scalar(
                                        out=o_all[:, qi, hd * 64:(hd + 1) * 64], in0=pb[OB[hh]][:, 0:64], scalar1=denA[:, hh:hh + 1], scalar2=None, op0=ALU.mult),
                                        reads=[OBN[hh], f"den{hh}"], writes=[f"oall{gp}_{qi}A{kvh}_{hh}"])
                        steps.append((s1, s2))
            for h in range(8):
                ft = h // 2
                r0 = (h % 2) * 64
                kb_list = []
                for kb in range(max(0, 4 * g - 8), min(NT, 4 * g + 12)):
                    qlo = max(4 * g, kb - 8)
                    qhi = min(4 * g + 3, kb + 8)
                    if qhi >= qlo:
                        kb_list.append((kb, qlo, qhi))
                for (kb, qlo, qhi) in kb_list:
                    st = {}
                    ncol = (qhi - qlo + 1) * 128

                    def s1(st=st, kb=kb, qlo=qlo, qhi=qhi, ncol=ncol, ft=ft, r0=r0):
                        sbk = sbank[0] % 3; sbank[0] += 1
                        pi = pbuf[0] % NPB; pbuf[0] += 1
                        st["pi"] = pi
                        P = Pb[pi]
                        S.op("pe", lambda e: e.matmul(
                            pb[sbk][:, 0:ncol], lhsT=kbT[r0:r0 + 64, ft, kb * 128:(kb + 1) * 128],
                            rhs=qbT[r0:r0 + 64, ft, qlo * 128:qlo * 128 + ncol], start=True, stop=True),
                            reads=[f"kbT{kb}"] + [f"qbT{q}" for q in range(qlo, qhi + 1)], writes=[f"pb{sbk}"])
                        S.op("act", lambda e: e.activation(out=P[:, 0:ncol], in_=pb[sbk][:, 0:ncol], func=AF.Exp, scale=0.125),
                             reads=[f"pb{sbk}"], writes=[f"P{pi}"])
                        m0 = (qlo - kb + 11) * 128
                        S.op("dve", lambda e: e.tensor_tensor(out=P[:, 0:ncol], in0=P[:, 0:ncol], in1=maskB[:, m0:m0 + ncol], op=ALU.mult),
                             reads=[f"P{pi}", "maskB"], writes=[f"P{pi}"])

                    def s2(st=st, kb=kb, qlo=qlo, qhi=qhi, h=h, last_kb=kb_list[-1][0]):
                        pi = st["pi"]
                        P = Pb[pi]
                        for qb_ in range(qlo, qhi + 1):
                            first = max(0, qb_ - 8)
                            last = min(NT - 1, qb_ + 8)
                            S.op("pe", lambda e, qb_=qb_, first=first, last=last: e.matmul(
                                pb[OB[qb_ - 4 * g]][:, 0:65], lhsT=P[:, (qb_ - qlo) * 128:(qb_ - qlo + 1) * 128], rhs=vB4[:, kb, h, :],
                                start=(kb == first), stop=(kb == last)),
                                reads=[f"P{pi}", f"vB{kb}", "vB_ones"], writes=[OBN[qb_ - 4 * g]])
                        if kb == last_kb:
                            denB = small[:, 12:16]
                            for qi in range(4):
                                S.op("dve", lambda e, qi=qi: e.reciprocal(out=denB[:, qi:qi + 1], in_=pb[OB[qi]][:, 64:65]), reads=[OBN[qi]], writes=[f"denB{qi}"])
                                S.op("dve", lambda e, qi=qi: e.tensor_scalar(
                                    out=o_all[:, qi, 512 + h * 64:512 + (h + 1) * 64], in0=pb[OB[qi]][:, 0:64], scalar1=denB[:, qi:qi + 1], scalar2=None, op0=ALU.mult),
                                    reads=[OBN[qi], f"denB{qi}"], writes=[f"oallB{gp}_{h}_{qi}"])
                    steps.append((s1, s2))
            return steps

        def finish_tile(g, qi):
            j = 4 * g + qi
            b = j % 2
            gp = g % 2
            o_all = o_view(g)
            oa_r = [f"oall{gp}_{qi}A{k_}_{h_}" for k_ in range(2) for h_ in range(4)]
            ob_r = [f"oallB{gp}_{h}_{qi}" for h in range(8)]
            for gi, rd in ((0, oa_r), (1, ob_r)):
                S.op("act", lambda e, gi=gi: e.activation(out=junk[:, 0:512], in_=o_all[:, qi, gi * 512:(gi + 1) * 512], func=AF.Square,
                                                          scale=float(1.0 / np.sqrt(512.0)), accum_out=small[:, 16 + gi:17 + gi]),
                     reads=rd, writes=["junk", f"gss{gi}"])
                S.op("act", lambda e, gi=gi: e.activation(out=small[:, 18 + gi:19 + gi], in_=small[:, 16 + gi:17 + gi], func=AF.Sqrt, bias=EPS),
                     reads=[f"gss{gi}"], writes=[f"grs{gi}"])
                S.op("dve", lambda e, gi=gi: e.reciprocal(out=small[:, 20 + gi:21 + gi], in_=small[:, 18 + gi:19 + gi]), reads=[f"grs{gi}"], writes=[f"grstd{gi}"])
                S.op("dve", lambda e, gi=gi: e.scalar_tensor_tensor(
                    out=mixed[0][:, gi * 512:(gi + 1) * 512], in0=o_all[:, qi, gi * 512:(gi + 1) * 512], scalar=small[:, 20 + gi:21 + gi],
                    in1=gout[:, gi * 512:(gi + 1) * 512], op0=ALU.mult, op1=ALU.mult),
                    reads=rd + [f"grstd{gi}", "gout"], writes=[f"mixed0_{gi}"])
            yield
            pT = pbh[5].rearrange("p (k c) -> p k c", k=8)
            for kc in range(8):
                S.op("pe", lambda e, kc=kc: e.transpose(out=pT[:, kc, :], in_=mixed[0][:, kc * 128:(kc + 1) * 128], identity=ident),
                     reads=["mixed0_0", "mixed0_1", "ident"], writes=["pb5"])
            S.op("act", lambda e: e.copy(out=hT[b], in_=pT), reads=["pb5"], writes=[f"hT{b}"])
            xt = xbuf[b]
            S.op("sp", lambda e: e.dma_start(out=xt, in_=xw[j * 128:(j + 1) * 128, :]), writes=[f"x{b}"], dma=True)
            yield
            for hf in range(2):
                for kc in range(8):
                    S.op("pe", lambda e, kc=kc, hf=hf: e.matmul(
                        pb[5], lhsT=hT[b][:, kc, :], rhs=wout[:, kc, hf * 512:(hf + 1) * 512], start=(kc == 0), stop=(kc == 7)),
                        reads=[f"hT{b}", "wout"], writes=["pb5"])
                S.op("dve", lambda e, hf=hf: e.tensor_tensor(
                    out=x1t[0][:, hf * 512:(hf + 1) * 512], in0=pb[5], in1=xt[:, hf * 512:(hf + 1) * 512], op=ALU.add),
                    reads=["pb5", f"x{b}"], writes=[f"x1t0_{hf}"])
                yield
            x1r = ["x1t0_0", "x1t0_1"]
            S.op("sp", lambda e: e.dma_start(out=acc_d[j * 128:(j + 1) * 128, :], in_=x1t[0]), reads=x1r, writes=["acc"], dma=True)
            if debug:
                S.op("sp", lambda e: e.dma_start(out=dbg_x1[j * 128:(j + 1) * 128, :], in_=x1t[0]), reads=x1r, dma=True)
            S.op("act", lambda e: e.activation(out=junk, in_=x1t[0], func=AF.Square, scale=1.0 / 32, accum_out=small[:, 24:25]),
                 reads=x1r, writes=["junk", "ss2"])
            S.op("act", lambda e: e.activation(out=small[:, 25:26], in_=small[:, 24:25], func=AF.Sqrt, bias=EPS), reads=["ss2"], writes=["rs2"])
            S.op("dve", lambda e: e.reciprocal(out=small[:, 26:27], in_=small[:, 25:26]), reads=["rs2"], writes=["rstd2"])
            S.op("dve", lambda e: e.scalar_tensor_tensor(out=h2f[0], in0=x1t[0], scalar=small[:, 26:27], in1=gffn, op0=ALU.mult, op1=ALU.mult),
                 reads=x1r + ["rstd2", "gffn"], writes=["h2f0"])
            S.op("act", lambda e: e.copy(out=h2b[0], in_=h2f[0]), reads=["h2f0"], writes=["h2b0"])
            S.op("sp", lambda e: e.dma_start(out=h2_d[j * 128:(j + 1) * 128, :], in_=h2b[0]), reads=["h2b0"], writes=["h2d"], dma=True)
            yield
            lg = small[:, 44:60]
            for part in range(2):
                for k4 in range(4):
                    kc = part * 4 + k4
                    S.op("pe", lambda e, kc=kc, k4=k4: e.transpose(
                        out=pb[5][:, k4 * 128:(k4 + 1) * 128], in_=h2f[0][:, kc * 128:(kc + 1) * 128], identity=identf),
                        reads=["h2f0", "identf"], writes=["pb5"])
                eng = "act" if part == 0 else "dve"
                if eng == "act":
                    S.op("act", lambda e: e.copy(out=h2T, in_=pb[5].rearrange("p (k c) -> p k c", k=4)), reads=["pb5"], writes=["h2T"])
                else:
                    S.op("dve", lambda e: e.tensor_copy(out=h2T, in_=pb[5].rearrange("p (k c) -> p k c", k=4)), reads=["pb5"], writes=["h2T"])
                yield
                for k4 in range(4):
                    kc = part * 4 + k4
                    S.op("pe", lambda e, kc=kc, k4=k4: e.matmul(pb[5][:, 0:NE], lhsT=h2T[:, k4, :], rhs=wr[:, kc, :], start=(k4 == 0), stop=(k4 == 3)),
                         reads=["h2T", "wr"], writes=["pb5"])
                if part == 0:
                    S.op("dve", lambda e: e.tensor_copy(out=lg, in_=pb[5][:, 0:NE]), reads=["pb5"], writes=["lg"])
                else:
                    S.op("dve", lambda e: e.tensor_tensor(out=lg, in0=pb[5][:, 0:NE], in1=lg, op=ALU.add), reads=["pb5", "lg"], writes=["lg"])
                yield
            S.op("dve", lambda e: e.reduce_max(out=small[:, 28:29], in_=lg, axis=AX.X), reads=["lg"], writes=["lmax"])
            S.op("dve", lambda e: e.tensor_scalar(out=small[:, 29:30], in0=small[:, 28:29], scalar1=-1.0, scalar2=None, op0=ALU.mult),
                 reads=["lmax"], writes=["nlmax"])
            S.op("act", lambda e: e.activation(out=aff[:, j, :], in_=lg, func=AF.Exp, bias=small[:, 29:30], scale=1.0,
                                               accum_out=small[:, 30:31]),
                 reads=["lg", "nlmax"], writes=[f"aff{j}", "lsum"])
            S.op("dve", lambda e: e.reciprocal(out=small[:, 31:32], in_=small[:, 30:31]), reads=["lsum"], writes=["rlsum"])
            S.op("dve", lambda e: e.tensor_scalar(out=aff[:, j, :], in0=aff[:, j, :], scalar1=small[:, 31:32], scalar2=None, op0=ALU.mult),
                 reads=[f"aff{j}", "rlsum"], writes=[f"aff{j}"])
            yield

        def chain(gens):
            for gn in gens:
                yield from gn

        LOOK = 2
        pending = None
        for g in range(4 if level >= 2 else 0):
            steps = attn_steps(g)
            n = len(steps)
            every = 3
            for i in range(n + LOOK):
                if i < n:
                    steps[i][0]()
                if i >= LOOK:
                    steps[i - LOOK][1]()
                if pending is not None and i % every == every - 1:
                    if next(pending, "done") == "done":
                        pending = None
            if pending is not None:
                for _ in pending:
                    pass
            pending = chain([finish_tile(g, qi) for qi in range(4)])
        if pending is not None:
            for _ in pending:
                pass

        AFF_R = [f"aff{j}" for j in range(NO)]
        if debug and level == 0:
            aff_in = din("aff_in", [128, NO * NE])
            S.op("sp", lambda e: e.dma_start(out=aff.rearrange("p a b -> p (a b)"), in_=aff_in), writes=AFF_R, dma=True)
        if debug and level >= 2:
            S.op("sp", lambda e: e.dma_start(out=dbg_aff, in_=aff.rearrange("p a b -> p (a b)")), reads=AFF_R, dma=True)

        if not stop_after_phase1:
            S.barrier()
            AR.off = mark_persist
            afull = AR.alloc([128, 2, NO * NE], F32)
            cmpb = AR.alloc([128, 2 * NO, NE], F32)
            lo = AR.alloc([128, NE], F32)
            hi = AR.alloc([128, NE], F32)
            mid = AR.alloc([128, NE], F32)
            cntp = AR.alloc([128, NE], F32)
            sel = AR.alloc([128, NE], F32)
            tmpn = AR.alloc([128, NE], F32)
            msk = AR.alloc([128, NO, NE], F32)
            dest = AR.alloc([128, NO, NE], F32)
            offs = AR.alloc([128, NO, NE], F32)
            vals = AR.alloc([128, NO, 6], BF16)
            gres = AR.alloc([128, NO], F32)
            selb = [AR.alloc([128, CAP], BF16) for _ in range(2)]
            idxf = AR.alloc([128, 4, 6], F32)
            idxi = [[AR.alloc([128, 1], I32) for _c in range(4)] for _ in range(2)]
            gate = [AR.alloc([128, 4], F32) for _ in range(2)]
            xg = AR.alloc([128, 4, D], BF16)
            xeT = [AR.alloc([128, 8, CAP], BF16) for _ in range(2)]
            hTm = AR.alloc([128, 16, CAP], BF16)
            sa = [AR.alloc([128, CAP], F32) for _ in range(2)]
            yo = [AR.alloc([128, D], F32) for _ in range(2)]
            wgb = [AR.alloc([128, 8, 1024], BF16) for _ in range(2)]
            wub = [AR.alloc([128, 8, 1024], BF16) for _ in range(2)]
            wdb = [AR.alloc([128, 8, D], BF16) for _ in range(3)]
            print("phase2 sbuf bytes/partition:", AR.off)

            aff2 = aff.rearrange("p a b -> p (a b)")
            S.op("dve", lambda e: e.tensor_scalar(out=afull[:, 0, :], in0=aff2, scalar1=par[:, 0:1], scalar2=None, op0=ALU.mult),
                 reads=AFF_R + ["par"], writes=["afull0"])
            S.op("dve", lambda e: e.tensor_scalar(out=afull[:, 1, :], in0=aff2, scalar1=par[:, 1:2], scalar2=None, op0=ALU.mult),
                 reads=AFF_R + ["par"], writes=["afull1"])
            S.op("pool", lambda e: e.dma_start(out=cin_d[:, :], in_=afull.rearrange("p a b -> p (a b)")), reads=["afull0", "afull1"], writes=["cin"], dma=True)

            def cc(e):
                return e.collective_compute("AllReduce", ALU.add, replica_groups=[[0, 1], [2, 3], [4, 5], [6, 7]],
                                            ins=[cin_d.ap().opt()], outs=[cout_d.ap().opt()])
            S.cnt["cc"] = 0
            S.ops["pool"].append(([(k, v) for k, v in [S.last_w["cin"]]], None, None))
            S.waited["pool"][S.last_w["cin"][0]] = max(S.waited["pool"].get(S.last_w["cin"][0], 0), S.last_w["cin"][1])
            S.ops["pool"].append(([], cc, ("cc", 1)))
            S.last_w["cout"] = ("cc", 1)
            S.readers["cout"] = []
            S.all_tokens["cc"] = 1
            S.op("pool", lambda e: e.dma_start(out=afull.rearrange("p a b -> p (a b)"), in_=cout_d[:, :]), reads=["cout"], writes=["afull0", "afull1"], dma=True)
            AF_R = ["afull0", "afull1"]
            S.op("dve", lambda e: e.memset(lo, 0.0), writes=["lo"])
            S.op("dve", lambda e: e.memset(hi, 1.0), writes=["hi"])
            af3 = afull.rearrange("p a (j e) -> p (a j) e", e=NE)
            for it in range(26):
                S.op("dve", lambda e: e.tensor_tensor(out=mid, in0=lo, in1=hi, op=ALU.add), reads=["lo", "hi"], writes=["mid"])
                S.op("dve", lambda e: e.tensor_scalar(out=mid, in0=mid, scalar1=0.5, scalar2=None, op0=ALU.mult), reads=["mid"], writes=["mid"])
                S.op("dve", lambda e: e.tensor_tensor(out=cmpb, in0=af3, in1=mid.unsqueeze(1).to_broadcast([128, 2 * NO, NE]), op=ALU.is_ge),
                     reads=AF_R + ["mid"], writes=["cmpb"])
                S.op("dve", lambda e: e.tensor_reduce(out=cntp, in_=cmpb.rearrange("p s e -> p e s"), axis=AX.X, op=ALU.add), reads=["cmpb"], writes=["cntp"])
                S.op("pe", lambda e: e.matmul(pb[5][:, 0:NE], lhsT=ones, rhs=cntp, start=True, stop=True), reads=["ones", "cntp"], writes=["pb5"])
                S.op("dve", lambda e: e.tensor_scalar(out=sel, in0=pb[5][:, 0:NE], scalar1=511.5, scalar2=None, op0=ALU.is_ge), reads=["pb5"], writes=["sel"])
                S.op("dve", lambda e: e.tensor_tensor(out=tmpn, in0=mid, in1=lo, op=ALU.subtract), reads=["mid", "lo"], writes=["tmpn"])
                S.op("dve", lambda e: e.tensor_tensor(out=tmpn, in0=tmpn, in1=sel, op=ALU.mult), reads=["tmpn", "sel"], writes=["tmpn"])
                S.op("dve", lambda e: e.tensor_tensor(out=lo, in0=lo, in1=tmpn, op=ALU.add), reads=["lo", "tmpn"], writes=["lo"])
                S.op("dve", lambda e: e.tensor_tensor(out=tmpn, in0=hi, in1=mid, op=ALU.subtract), reads=["hi", "mid"], writes=["tmpn"])
                S.op("dve", lambda e: e.tensor_tensor(out=tmpn, in0=tmpn, in1=sel, op=ALU.mult), reads=["tmpn", "sel"], writes=["tmpn"])
                S.op("dve", lambda e: e.tensor_tensor(out=hi, in0=mid, in1=tmpn, op=ALU.add), reads=["mid", "tmpn"], writes=["hi"])
            if debug:
                S.op("sp", lambda e: e.dma_start(out=dbg_thr[:, 0:NE], in_=lo), reads=["lo"], dma=True)
                S.op("sp", lambda e: e.dma_start(out=dbg_thr[:, NE:2 * NE], in_=hi), reads=["hi"], dma=True)
            S.op("dve", lambda e: e.tensor_tensor(out=msk, in0=aff, in1=lo.unsqueeze(1).to_broadcast([128, NO, NE]), op=ALU.is_ge),
                 reads=AFF_R + ["lo"], writes=["msk"])
            mk2 = msk.rearrange("p a b -> p (a b)")
            S.op("pe", lambda e: e.matmul(pb[3][:, 0:NO * NE], lhsT=tri, rhs=mk2, start=True, stop=True), reads=["tri", "msk"], writes=["pb3"])
            S.op("pe", lambda e: e.matmul(pb[4][:, 0:NO * NE], lhsT=ones, rhs=mk2, start=True, stop=True), reads=["ones", "msk"], writes=["pb4"])
            tot = pb[4][:, 0:NO * NE].rearrange("p (a b) -> p a b", a=NO)
            S.op("dve", lambda e: e.memset(offs[:, 0, :], 0.0), writes=["offs"])
            for j in range(1, NO):
                S.op("dve", lambda e, j=j: e.tensor_tensor(out=offs[:, j, :], in0=offs[:, j - 1, :], in1=tot[:, j - 1, :], op=ALU.add),
                     reads=["offs", "pb4"], writes=["offs"])
            S.op("dve", lambda e: e.tensor_tensor(out=dest, in0=offs, in1=pb[3][:, 0:NO * NE].rearrange("p (a b) -> p a b", a=NO), op=ALU.add),
                 reads=["offs", "pb3"], writes=["dest"])
            S.op("dve", lambda e: e.tensor_scalar(out=msk, in0=msk, scalar1=-BIG, scalar2=BIG, op0=ALU.mult, op1=ALU.add), reads=["msk"], writes=["msk"])
            S.op("dve", lambda e: e.tensor_tensor(out=dest, in0=dest, in1=msk, op=ALU.add), reads=["dest", "msk"], writes=["dest"])
            S.op("dve", lambda e: e.tensor_copy(out=vals[:, :, 0:3], in_=tokc), reads=["tokc"], writes=["vals_c"])
            S.op("pool", lambda e: e.memset(xg, 0.0), writes=["xg0", "xg1", "xg2", "xg3"])

            wg_src = [wg_d[x].rearrange("(k p) f -> p k f", p=128) for x in range(n_exp)]
            wu_src = [wu_d[x].rearrange("(k p) f -> p k f", p=128) for x in range(n_exp)]
            wd_src = [wd_d[x].rearrange("(k p) c -> p k c", p=128) for x in range(n_exp)]

            def load_weights(x):
                for hf in range(2):
                    S.op("pool", lambda e, x=x, hf=hf: e.dma_start(out=wgb[hf], in_=wg_src[x][:, :, hf * 1024:(hf + 1) * 1024]),
                         writes=[f"wg{hf}"], dma=True)
                    S.op("pool", lambda e, x=x, hf=hf: e.dma_start(out=wub[hf], in_=wu_src[x][:, :, hf * 1024:(hf + 1) * 1024]),
                         writes=[f"wu{hf}"], dma=True)
                for hf in range(2):
                    bi = (2 * x + hf) % 3
                    S.op("pool", lambda e, x=x, hf=hf, bi=bi: e.dma_start(out=wdb[bi], in_=wd_src[x][:, hf * 8:(hf + 1) * 8, :]),
                         writes=[f"wd{bi}"], dma=True)

            CH = [(0, 128), (128, 128), (256, 128), (384, 128)]
            _DBG["g"] = (xg, h2_d, idxi)
            load_weights(0)
            for x in range(n_exp):
                pp = x % 2
                S.op("dve", lambda e, x=x: e.tensor_copy(out=vals[:, :, 3], in_=aff[:, :, x]), reads=AFF_R, writes=["vals_g"])
                S.op("dve", lambda e, x=x: e.tensor_tensor(out=gres, in0=aff[:, :, x], in1=vals[:, :, 3], op=ALU.subtract), reads=AFF_R + ["vals_g"], writes=["gres"])
                S.op("dve", lambda e: e.tensor_copy(out=vals[:, :, 4], in_=gres), reads=["gres"], writes=["vals_g"])
                S.op("dve", lambda e: e.tensor_tensor(out=gres, in0=gres, in1=vals[:, :, 4], op=ALU.subtract), reads=["gres", "vals_g"], writes=["gres"])
                S.op("dve", lambda e: e.tensor_copy(out=vals[:, :, 5], in_=gres), reads=["gres"], writes=["vals_g"])
                for j in range(NO):
                    sb_ = selb[j % 2]
                    S.op("dve", lambda e, j=j, x=x, sb_=sb_: e.tensor_scalar(out=sb_, in0=iota, scalar1=dest[:, j, x:x + 1], scalar2=None, op0=ALU.is_equal),
                         reads=["iota", "dest"], writes=[f"selb{j % 2}"])
                    for c, (s0, sn) in enumerate(CH):
                        IB = [5, 4, 6, 7]
                        S.op("pe", lambda e, j=j, c=c, s0=s0, sn=sn, sb_=sb_: e.matmul(
                            pb[IB[c]][0:sn, 0:6], lhsT=sb_[:, s0:s0 + sn], rhs=vals[:, j, :], start=(j == 0), stop=(j == NO - 1)),
                            reads=[f"selb{j % 2}", "vals_c", "vals_g"], writes=[f"pb{IB[c]}"])
                for c in range(4):
                    S.op("act", lambda e, c=c: e.copy(out=idxf[:, c, :], in_=pb[[5, 4, 6, 7][c]][:, 0:6]), reads=[f"pb{[5, 4, 6, 7][c]}"], writes=["idxf"])
                S.op("dve", lambda e: e.tensor_scalar(out=idxf[:, :, 0], in0=idxf[:, :, 0], scalar1=64.0, scalar2=None, op0=ALU.mult), reads=["idxf"], writes=["idxf"])
                S.op("dve", lambda e: e.tensor_tensor(out=idxf[:, :, 0], in0=idxf[:, :, 0], in1=idxf[:, :, 1], op=ALU.add), reads=["idxf"], writes=["idxf"])
                S.op("dve", lambda e: e.tensor_scalar(out=idxf[:, :, 2], in0=idxf[:, :, 2], scalar1=-BIG, scalar2=BIG, op0=ALU.mult, op1=ALU.add), reads=["idxf"], writes=["idxf"])
                S.op("dve", lambda e: e.tensor_tensor(out=idxf[:, :, 0], in0=idxf[:, :, 0], in1=idxf[:, :, 2], op=ALU.add), reads=["idxf"], writes=["idxf"])
                for c in range(4):
                    S.op("dve", lambda e, pp=pp, c=c: e.tensor_copy(out=idxi[pp][c], in_=idxf[:, c, 0:1]), reads=["idxf"], writes=[f"idxi{pp}"])
                S.op("dve", lambda e, pp=pp: e.tensor_tensor(out=gate[pp], in0=idxf[:, :, 3], in1=idxf[:, :, 4], op=ALU.add), reads=["idxf"], writes=[f"gate{pp}"])
                S.op("dve", lambda e, pp=pp: e.tensor_tensor(out=gate[pp], in0=gate[pp], in1=idxf[:, :, 5], op=ALU.add), reads=["idxf", f"gate{pp}"], writes=[f"gate{pp}"])
                for c, (s0, sn) in enumerate(CH):
                    S.op("pool", lambda e, c=c, sn=sn, pp=pp: e.indirect_dma_start(
                        out=xg[0:sn, c, :], out_offset=None, in_=h2_d[:, :],
                        in_offset=bass.IndirectOffsetOnAxis(ap=idxi[pp][c][0:sn, :], axis=0),
                        bounds_check=_bc(e, OWN - 1), oob_is_err=False),
                        reads=[f"idxi{pp}", "h2d"], writes=[f"xg{c}"], dma=True)
                pT = pbh[5].rearrange("p (k c) -> p k c", k=8)
                for c, (s0, sn) in enumerate(CH):
                    for kc in range(8):
                        S.op("pe", lambda e, c=c, kc=kc, sn=sn: e.transpose(out=pT[:, kc, 0:sn], in_=xg[0:sn, c, kc * 128:(kc + 1) * 128], identity=ident[0:sn, 0:sn]),
                             reads=[f"xg{c}", "ident"], writes=["pb5"])
                    eng = "act" if c % 2 == 0 else "dve"
                    if eng == "act":
                        S.op("act", lambda e, s0=s0, sn=sn, pp=pp: e.copy(out=xeT[pp][:, :, s0:s0 + sn], in_=pT[:, :, 0:sn]), reads=["pb5"], writes=[f"xeT{pp}_{c}"])
                    else:
                        S.op("dve", lambda e, s0=s0, sn=sn, pp=pp: e.tensor_copy(out=xeT[pp][:, :, s0:s0 + sn], in_=pT[:, :, 0:sn]), reads=["pb5"], writes=[f"xeT{pp}_{c}"])
                XE_R = [f"xeT{pp}_{c}" for c in range(4)]
                for fc in range(16):
                    hf = fc // 8
                    ba = (2 * fc) % 4
                    bu = (2 * fc + 1) % 4
                    for kc in range(8):
                        S.op("pe", lambda e, kc=kc, fc=fc, hf=hf, ba=ba, pp=pp: e.matmul(
                            pb[ba][:, 0:CAP], lhsT=wgb[hf][:, kc, (fc % 8) * 128:(fc % 8 + 1) * 128], rhs=xeT[pp][:, kc, :], start=(kc == 0), stop=(kc == 7)),
                            reads=[f"wg{hf}"] + XE_R, writes=[f"pb{ba}"])
                    for kc in range(8):
                        S.op("pe", lambda e, kc=kc, fc=fc, hf=hf, bu=bu, pp=pp: e.matmul(
                            pb[bu][:, 0:CAP], lhsT=wub[hf][:, kc, (fc % 8) * 128:(fc % 8 + 1) * 128], rhs=xeT[pp][:, kc, :], start=(kc == 0), stop=(kc == 7)),
                            reads=[f"wu{hf}"] + XE_R, writes=[f"pb{bu}"])
                    S.op("act", lambda e, fc=fc, ba=ba: e.activation(out=sa[fc % 2], in_=pb[ba][:, 0:CAP], func=AF.Silu), reads=[f"pb{ba}"], writes=[f"sa{fc % 2}"])
                    S.op("dve", lambda e, fc=fc, bu=bu: e.tensor_tensor(out=hTm[:, fc, :], in0=pb[bu][:, 0:CAP], in1=sa[fc % 2], op=ALU.mult),
                         reads=[f"pb{bu}", f"sa{fc % 2}"], writes=[f"hTm{fc}"])
                    if fc == 7 and x + 1 < n_exp:
                        S.op("pool", lambda e, x=x: e.dma_start(out=wgb[0], in_=wg_src[x + 1][:, :, 0:1024]), writes=["wg0"], dma=True)
                        S.op("pool", lambda e, x=x: e.dma_start(out=wub[0], in_=wu_src[x + 1][:, :, 0:1024]), writes=["wu0"], dma=True)
                if x + 1 < n_exp:
                    S.op("pool", lambda e, x=x: e.dma_start(out=wgb[1], in_=wg_src[x + 1][:, :, 1024:2048]), writes=["wg1"], dma=True)
                    S.op("pool", lambda e, x=x: e.dma_start(out=wub[1], in_=wu_src[x + 1][:, :, 1024:2048]), writes=["wu1"], dma=True)
                H_R = [f"hTm{fc}" for fc in range(16)]
                for c, (s0, sn) in enumerate(CH):
                    yb = (4 * x + c) % 2
                    for hf2 in range(2):
                        bank = 6 + hf2
                        for fc in range(16):
                            bi = (2 * x + fc // 8) % 3
                            S.op("pe", lambda e, fc=fc, bi=bi, s0=s0, sn=sn, hf2=hf2, bank=bank: e.matmul(
                                pb[bank][0:sn, :], lhsT=hTm[:, fc, s0:s0 + sn], rhs=wdb[bi][:, fc % 8, hf2 * 512:(hf2 + 1) * 512],
                                start=(fc == 0), stop=(fc == 15)),
                                reads=[f"hTm{fc}", f"wd{bi}"], writes=[f"pb{bank}"])
                        if hf2 == 0:
                            S.op("act", lambda e, sn=sn, yb=yb, c=c, pp=pp, bank=bank: e.activation(
                                out=yo[yb][0:sn, 0:512], in_=pb[bank][0:sn, :], func=AF.Copy, scale=gate[pp][0:sn, c:c + 1]),
                                reads=[f"pb{bank}", f"gate{pp}"], writes=[f"yo{yb}_0"])
                        else:
                            S.op("dve", lambda e, sn=sn, yb=yb, c=c, pp=pp, bank=bank: e.tensor_scalar(
                                out=yo[yb][0:sn, 512:1024], in0=pb[bank][0:sn, :], scalar1=gate[pp][0:sn, c:c + 1], scalar2=None, op0=ALU.mult),
                                reads=[f"pb{bank}", f"gate{pp}"], writes=[f"yo{yb}_1"])
                    prev = ["acc"] if x == 0 else [f"accw{x - 1}_{k}" for k in range(4)]
                    S.op("pool", lambda e, yb=yb, c=c, pp=pp: e.indirect_dma_start(
                        out=acc_d[:, :], out_offset=bass.IndirectOffsetOnAxis(ap=idxi[pp][c][:, :], axis=0),
                        in_=yo[yb][:, :], in_offset=None, bounds_check=_bc(e, OWN - 1), oob_is_err=False, compute_op=ALU.add),
                        reads=[f"yo{yb}_0", f"yo{yb}_1", f"idxi{pp}"] + prev, writes=[f"accw{x}_{c}"], dma=True)
                if x + 1 < n_exp:
                    for hf in range(2):
                        bi = (2 * (x + 1) + hf) % 3
                        S.op("pool", lambda e, x=x, hf=hf, bi=bi: e.dma_start(out=wdb[bi], in_=wd_src[x + 1][:, hf * 8:(hf + 1) * 8, :]),
                             writes=[f"wd{bi}"], dma=True)

        S.barrier()
        AR.off = mark_persist
        NFIN = 4
        fin = [AR.alloc([128, D], F32) for _ in range(NFIN)]
        fjunk = AR.alloc([128, D], F32)
        ACC_FINAL = ["acc"] if stop_after_phase1 else [f"accw{n_exp - 1}_{k}" for k in range(4)]
        def fin_load(jj):
            bb = jj % NFIN
            S.op("sp", lambda e: e.dma_start(out=fin[bb], in_=acc_d[jj * 128:(jj + 1) * 128, :]), reads=ACC_FINAL, writes=[f"fin{bb}"], dma=True)
        for jj in range(NFIN - 1):
            fin_load(jj)
        for j in range(NO):
            if j + NFIN - 1 < NO:
                fin_load(j + NFIN - 1)
            b = j % NFIN
            c4 = 40 + 4 * (j % 2)
            S.op("act", lambda e, b=b, c4=c4: e.activation(out=fjunk, in_=fin[b], func=AF.Square, scale=1.0 / 32, accum_out=small[:, c4:c4 + 1]),
                 reads=[f"fin{b}"], writes=["junkf", f"fss{j % 2}"])
            S.op("act", lambda e, c4=c4: e.activation(out=small[:, c4 + 1:c4 + 2], in_=small[:, c4:c4 + 1], func=AF.Sqrt, bias=EPS), reads=[f"fss{j % 2}"], writes=[f"frs{j % 2}"])
            S.op("dve", lambda e, c4=c4: e.reciprocal(out=small[:, c4 + 2:c4 + 3], in_=small[:, c4 + 1:c4 + 2]), reads=[f"frs{j % 2}"], writes=[f"frstd{j % 2}"])
            S.op("dve", lambda e, b=b, c4=c4: e.scalar_tensor_tensor(out=fin[b], in0=fin[b], scalar=small[:, c4 + 2:c4 + 3], in1=gfin, op0=ALU.mult, op1=ALU.mult),
                 reads=[f"fin{b}", f"frstd{j % 2}", "gfin"], writes=[f"fin{b}"])
            S.op("sp", lambda e, j=j, b=b: e.dma_start(out=out_d[j * 128:(j + 1) * 128, :], in_=fin[b]), reads=[f"fin{b}"], dma=True)
        S.barrier()
        sems["cc"] = es.enter_context(nc.semaphore("sem_cc"))
        S.emit(sems)
    return nc


def _weight_mask(delta):
    a = np.abs(delta)
    w = (a <= 64).astype(np.float32)
    w += ((a <= 256) & (delta % 4 == 0)).astype(np.float32)
    w += ((a <= 1024) & (delta % 16 == 0)).astype(np.float32)
    return w


def _consts():
    bf = ml_dtypes.bfloat16
    kk = np.arange(128)[:, None]
    qq = np.arange(128)[None, :]
    mA = np.concatenate([(kk >= qq), np.ones((128, 128), bool), (kk <= qq)], axis=1).astype(np.float32)
    blocks = []
    for o in range(23):
        d = 128 * (11 - o) + kk - qq
        blocks.append(_weight_mask(d))
    mB = np.concatenate(blocks, axis=1)
    tri = (kk < qq).astype(np.float32)
    tok = (np.arange(NO)[None, :] * 128 + np.arange(128)[:, None])
    tokc = np.stack([tok // 64, tok % 64, np.ones_like(tok)], axis=-1).astype(np.float32).reshape(128, NO * 3)
    return {
        "ident": np.eye(128).astype(bf),
        "identf": np.eye(128, dtype=np.float32),
        "tri": tri,
        "ones": np.ones((128, 128), np.float32),
        "maskA": mA.astype(bf),
        "maskB": mB.astype(bf),
        "iota": np.arange(CAP, dtype=np.float32).reshape(1, CAP),
        "tokc": np.ascontiguousarray(tokc),
    }


def _positions(c):
    half = c % 2
    i = np.arange(WIN)
    return i if half == 0 else (SEQ - 1 - i)


def _in_maps(x, g_mix, w_in, a_sink, g_out_a, g_out_b, w_out, g_ffn, w_router, w_gate, w_up, w_down, g_final, n_exp=NE):
    consts = _consts()
    inv_freq = (np.float32(500000.0) ** (-np.arange(0, 16, 2, dtype=np.float32) / np.float32(16))).astype(np.float32)
    sink = np.asarray(a_sink[0], np.float32)
    perm = [0, 2, 1, 3, 4, 6, 5, 7]
    shared = {
        "g_mix": np.ascontiguousarray(g_mix[0:1]), "g_ffn": np.ascontiguousarray(g_ffn[0:1]),
        "g_final": np.ascontiguousarray(np.asarray(g_final).reshape(1, D)),
        "g_out": np.ascontiguousarray(np.concatenate([g_out_a[0], g_out_b[0]])[None, :]),
        "w_in": np.ascontiguousarray(w_in[0]), "w_out": np.ascontiguousarray(w_out[0]),
        "w_router": np.ascontiguousarray(w_router[0]),
        "w_gate": np.ascontiguousarray(w_gate[0][:n_exp]), "w_up": np.ascontiguousarray(w_up[0][:n_exp]),
        "w_down": np.ascontiguousarray(w_down[0][:n_exp]),
        "sinkp": np.ascontiguousarray(sink[perm][None, :]),
    }
    shared.update(consts)
    maps = []
    for c in range(8):
        pos = _positions(c)
        ang = pos.astype(np.float32)[:, None] * inv_freq[None, :]
        m = dict(shared)
        m["xw"] = np.ascontiguousarray(x[c // 2][pos])
        m["cosw"] = np.cos(ang).astype(np.float32)
        m["sinw"] = np.sin(ang).astype(np.float32)
        m["par"] = np.array([[1.0, 0.0]] if c % 2 == 0 else [[0.0, 1.0]], np.float32)
        maps.append(m)
    return maps


_NC_CACHE = {}


def kernel(x, g_mix, w_in, a_sink, g_out_a, g_out_b, w_out, g_ffn, w_router, w_gate, w_up, w_down, g_final):
    args = [np.asarray(a) for a in (x, g_mix, w_in, a_sink, g_out_a, g_out_b, w_out, g_ffn, w_router, w_gate, w_up, w_down, g_final)]
    if "nc" not in _NC_CACHE:
        _NC_CACHE["nc"] = build()
    nc = _NC_CACHE["nc"]
    maps = _in_maps(*args)
    res = run_bass_kernel_spmd(nc, maps, core_ids=list(range(8)))
    out = np.empty((4, SEQ, D), np.float32)
    for c in range(8):
        pos = _positions(c)[:OWN]
        out[c // 2][pos] = res.results[c]["out"]
    return out
```

```python
import numpy as np
import ml_dtypes
from contextlib import ExitStack
import concourse.bass as bass
import concourse.mybir as mybir
from concourse.bass_utils import run_bass_kernel_spmd

F32 = mybir.dt.float32
I32 = mybir.dt.int32
BF16 = mybir.dt.bfloat16
ALU = mybir.AluOpType
AF = mybir.ActivationFunctionType
AX = mybir.AxisListType

D = 1024
SEQ = 4096
WIN = 3072
OWN = 2048
NT = 24
NO = 16
NE = 16
FF = 2048
CAP = 512
EPS = 1e-6
BIG = 1.0e6
ENGS = ["pe", "act", "dve", "pool", "sp"]


class Sched:
    def __init__(self, nc, n_dma=32, same_engine_wait=("act", "dve", "pool")):
        self.nc = nc
        self.ops = {e: [] for e in ENGS}
        self.cnt = {e: 0 for e in ENGS}
        self.waited = {e: {} for e in ENGS}
        self.last_w = {}
        self.readers = {}
        self.n_dma = n_dma
        self.dma_val = [0] * n_dma
        self.dma_rr = {"pool": 0, "sp": 0, "act": 0}
        self.same = set(same_engine_wait)
        self.all_tokens = {}

    def op(self, eng, fn, reads=(), writes=(), dma=False):
        writes = list(writes) + [r for r in reads if r.startswith("pb")]
        reads = [r for r in reads if not r.startswith("pb")]
        deps = set()
        for r in reads:
            if r in self.last_w:
                deps.add(self.last_w[r])
        for w in writes:
            if w in self.last_w:
                deps.add(self.last_w[w])
            for t in self.readers.get(w, ()):
                deps.add(t)
        if dma:
            half = self.n_dma // 2
            base = 0 if eng == "pool" else half
            k = base + self.dma_rr[eng]
            self.dma_rr[eng] = (self.dma_rr[eng] + 1) % half
            if self.dma_val[k] > 0:
                deps.add((("dma", k), self.dma_val[k]))
            self.dma_val[k] += 16
            token = (("dma", k), self.dma_val[k])
        else:
            self.cnt[eng] += 1
            token = (eng, self.cnt[eng])
        waits = []
        wd = self.waited[eng]
        mx = {}
        for key, val in deps:
            if mx.get(key, 0) < val:
                mx[key] = val
        for key, val in sorted(mx.items(), key=lambda t: str(t[0])):
            if key == eng and eng not in self.same:
                continue
            if wd.get(key, 0) < val:
                wd[key] = val
                waits.append((key, val))
        self.ops[eng].append((waits, fn, token))
        for r in reads:
            self.readers.setdefault(r, []).append(token)
        for w in writes:
            self.last_w[w] = token
            self.readers[w] = []
        self.all_tokens[token[0]] = max(self.all_tokens.get(token[0], 0), token[1])
        return token

    def barrier(self, engs=ENGS):
        toks = dict(self.all_tokens)
        for e in engs:
            waits = []
            wd = self.waited[e]
            for key, val in toks.items():
                if key == e:
                    continue
                if wd.get(key, 0) < val:
                    wd[key] = val
                    waits.append((key, val))
            if waits:
                self.ops[e].append((waits, None, None))

    def emit(self, semaphores):
        nc = self.nc

        def run(ename):
            def body(eng):
                for waits, fn, token in self.ops[ename]:
                    for key, val in waits:
                        eng.wait_ge(semaphores[key], val)
                    if fn is None:
                        continue
                    ins = fn(eng)
                    key = token[0]
                    if isinstance(key, tuple):
                        ins.then_inc(semaphores[key], 16)
                    else:
                        ins.then_inc(semaphores[key], 1)
            return body

        with nc.Block() as block:
            block.tensor(run("pe"))
            block.scalar(run("act"))
            block.vector(run("dve"))
            block.gpsimd(run("pool"))
            block.sync(run("sp"))


import os
SUB = int(os.environ.get('KSUB', '9'))
LOG = []
_DBG = {}
_BCREG = {}


def _bc(eng, val):
    key = (id(eng), val)
    if key not in _BCREG:
        _BCREG[key] = eng.to_reg(val)
    return _BCREG[key]


class Arena:
    def __init__(self, big, nbytes):
        self.big = big
        self.nbytes = nbytes
        self.off = 0

    def alloc(self, shape, dt):
        esz = 2 if dt == BF16 else 4
        n = int(np.prod(shape[1:]))
        nb = (n * esz + 63) // 64 * 64
        o = self.off
        self.off += nb
        LOG.append((o, tuple(shape), str(dt)))
        assert self.off <= self.nbytes, (self.off, self.nbytes)
        v = self.big[:, o // 4:(o + nb) // 4]
        if dt != F32:
            v = v.bitcast(dt)
        v = v[:, 0:n]
        if len(shape) == 3:
            v = v.rearrange("p (a b) -> p a b", a=shape[1])
        elif len(shape) == 4:
            v = v.rearrange("p (a b c) -> p a b c", a=shape[1], b=shape[2])
        return v


def build(n_exp=NE, debug=False, stop_after_phase1=False, level=9):
    nc = bass.Bass("TRN2", target_bir_lowering=False)
    _BCREG.clear()

    def din(name, shape, dt=F32):
        return nc.dram_tensor(name, list(shape), dt, kind="ExternalInput").ap()

    xw = din("xw", [WIN, D])
    cosw = din("cosw", [WIN, 8])
    sinw = din("sinw", [WIN, 8])
    gmix_d = din("g_mix", [1, D])
    gffn_d = din("g_ffn", [1, D])
    gfin_d = din("g_final", [1, D])
    gout_d = din("g_out", [1, D])
    win_d = din("w_in", [D, 2304])
    wout_d = din("w_out", [D, D])
    wr_d = din("w_router", [D, NE])
    wg_d = din("w_gate", [n_exp, D, FF])
    wu_d = din("w_up", [n_exp, D, FF])
    wd_d = din("w_down", [n_exp, FF, D])
    sink_d = din("sinkp", [1, 8])
    par_d = din("par", [1, 2])
    ident_d = din("ident", [128, 128], BF16)
    identf_d = din("identf", [128, 128])
    tri_d = din("tri", [128, 128])
    ones_d = din("ones", [128, 128])
    maskA_d = din("maskA", [128, 3 * 128], BF16)
    maskB_d = din("maskB", [128, 23 * 128], BF16)
    iota_d = din("iota", [1, CAP])
    tokc_d = din("tokc", [128, NO * 3])
    out_d = nc.dram_tensor("out", [OWN, D], F32, kind="ExternalOutput").ap()
    if debug:
        dbg_x1 = nc.dram_tensor("dbg_x1", [OWN, D], F32, kind="ExternalOutput").ap()
        dbg_aff = nc.dram_tensor("dbg_aff", [128, NO * NE], F32, kind="ExternalOutput").ap()
        dbg_thr = nc.dram_tensor("dbg_thr", [128, 2 * NE], F32, kind="ExternalOutput").ap()

    acc_d = nc.dram_tensor("acc", [OWN, D], F32).ap()
    h2_d = nc.dram_tensor("h2buf", [OWN, D], BF16).ap()
    cin_d = nc.dram_tensor("cin", [128, 2 * NO * NE], F32)
    cout_d = nc.dram_tensor("cout", [128, 2 * NO * NE], F32)

    S = Sched(nc)
    with ExitStack() as es:
        SB_BYTES = 196 * 1024
        big = es.enter_context(nc.sbuf_tensor("big", [128, SB_BYTES // 4], F32))
        AR = Arena(big, SB_BYTES)
        pbt = [es.enter_context(nc.psum_tensor(f"pb{i}", [128, 512], F32)) for i in range(8)]
        pb = [t[:, :] for t in pbt]
        pbh = [t[:, :].bitcast(BF16) for t in pbt]
        sems = {e: es.enter_context(nc.semaphore("sem_" + e)) for e in ENGS}
        for k in range(S.n_dma):
            sems[("dma", k)] = es.enter_context(nc.semaphore(f"dsem{k}"))

        ident = AR.alloc([128, 128], BF16)
        identf = AR.alloc([128, 128], F32)
        tri = AR.alloc([128, 128], F32)
        ones = AR.alloc([128, 128], F32)
        iota = AR.alloc([128, CAP], F32)
        tokc = AR.alloc([128, NO, 3], F32)
        par = AR.alloc([128, 2], F32)
        gfin = AR.alloc([128, D], F32)
        gffn = AR.alloc([128, D], F32)
        aff = AR.alloc([128, NO, NE], F32)
        small = AR.alloc([128, 64], F32)
        mark_persist = AR.off

        def ld(eng, dst, src, name, rd=()):
            S.op(eng, lambda e: e.dma_start(out=dst, in_=src), reads=rd, writes=[name], dma=True)

        ld("sp", ident, ident_d, "ident")
        ld("sp", identf, identf_d, "identf")
        ld("sp", tri, tri_d, "tri")
        ld("sp", ones, ones_d, "ones")
        ld("sp", iota, iota_d.partition_broadcast(128), "iota")
        ld("sp", tokc, tokc_d.rearrange("p (a b) -> p a b", a=NO), "tokc")
        ld("sp", par, par_d.partition_broadcast(128), "par")
        ld("sp", gfin, gfin_d.partition_broadcast(128), "gfin")
        ld("sp", gffn, gffn_d.partition_broadcast(128), "gffn")

        esink = AR.alloc([128, 8], F32)
        maskA = AR.alloc([128, 3 * 128], BF16)
        maskB = AR.alloc([128, 23 * 128], BF16)
        qaT = AR.alloc([128, 4, OWN], BF16)
        kaT = AR.alloc([128, 2, 17 * 128], BF16)
        qbT = AR.alloc([128, 4, OWN], BF16)
        kbT = AR.alloc([128, 4, WIN], BF16)
        vA = AR.alloc([128, 17, 2 * 65], BF16)
        vB = AR.alloc([128, NT, 8 * 65], BF16)
        xbuf = [AR.alloc([128, D], F32) for _ in range(2)]
        junk = AR.alloc([128, D], F32)
        hT = [AR.alloc([128, 8, 128], BF16) for _ in range(2)]
        mark_1a = AR.off
        gmix = AR.alloc([128, D], F32)
        cos_t = AR.alloc([128, NT, 8], F32)
        sin_t = AR.alloc([128, NT, 8], F32)
        win = AR.alloc([128, 8, 2304], BF16)
        xn = [AR.alloc([128, D], BF16) for _ in range(2)]
        qk_tm = [AR.alloc([128, 14 * 128], BF16) for _ in range(2)]
        tmpA = AR.alloc([128, 8 * 16], F32)
        tmpB = AR.alloc([128, 8 * 16], F32)
        print("phase1 sbuf bytes/partition:", AR.off)

        ld("sp", gmix, gmix_d.partition_broadcast(128), "gmix")
        ld("sp", cos_t, cosw.rearrange("(j p) d -> p j d", p=128), "cos")
        ld("sp", sin_t, sinw.rearrange("(j p) d -> p j d", p=128), "sin")
        ld("sp", esink, sink_d.partition_broadcast(128), "esink")
        ld("sp", maskA, maskA_d, "maskA")
        ld("sp", maskB, maskB_d, "maskB")
        win_src = win_d.rearrange("(k p) c -> p k c", p=128)
        for k0 in range(0, 8, 2):
            S.op("pool", lambda e, k0=k0: e.dma_start(out=win[:, k0:k0 + 2, :], in_=win_src[:, k0:k0 + 2, :]),
                 writes=[f"win{k0}"], dma=True)
        WIN_R = [f"win{k0}" for k0 in range(0, 8, 2)]
        S.op("act", lambda e: e.activation(out=esink, in_=esink, func=AF.Exp), reads=["esink"], writes=["esink"])
        vA4 = vA.rearrange("p j (h d) -> p j h d", h=2)
        vB4 = vB.rearrange("p j (h d) -> p j h d", h=8)
        S.op("pool", lambda e: e.memset(vA4[:, :, :, 64:65], 1.0), writes=["vA_ones"])
        S.op("pool", lambda e: e.memset(vB4[:, :, :, 64:65], 1.0), writes=["vB_ones"])

        psel = [0]

        def rope(j, pg, c0, H, dst, dups):
            if os.environ.get("KROPE", "1") == "0":
                return
            v = pg[:, c0:c0 + H * 64].rearrange("p (h d) -> p h d", h=H)
            x1 = v[:, :, 0:8]
            x2 = v[:, :, 8:16]
            cb = cos_t[:, j, :].unsqueeze(1).to_broadcast([128, H, 8])
            sb_ = sin_t[:, j, :].unsqueeze(1).to_broadcast([128, H, 8])
            t1c = tmpA[:, 0:H * 8].rearrange("p (h d) -> p h d", h=H)
            t2c = tmpA[:, 64:64 + H * 8].rearrange("p (h d) -> p h d", h=H)
            t1s = tmpB[:, 0:H * 8].rearrange("p (h d) -> p h d", h=H)
            t2s = tmpB[:, 64:64 + H * 8].rearrange("p (h d) -> p h d", h=H)
            pgn = dups["pg"]
            S.op("dve", lambda e: e.tensor_tensor(out=t1c, in0=x1, in1=cb, op=ALU.mult), reads=[pgn, "cos"], writes=["t1c"])
            S.op("dve", lambda e: e.tensor_tensor(out=t2c, in0=x2, in1=cb, op=ALU.mult), reads=[pgn, "cos"], writes=["t2c"])
            S.op("dve", lambda e: e.tensor_tensor(out=t1s, in0=x1, in1=sb_, op=ALU.mult), reads=[pgn, "sin"], writes=["t1s"])
            S.op("dve", lambda e: e.tensor_tensor(out=t2s, in0=x2, in1=sb_, op=ALU.mult), reads=[pgn, "sin"], writes=["t2s"])
            for dv, dn in dst:
                S.op("dve", lambda e, dv=dv: e.tensor_tensor(out=dv[:, :, 0:8], in0=t1c, in1=t2s, op=ALU.subtract),
                     reads=["t1c", "t2s"], writes=[dn + "_a"])
                S.op("dve", lambda e, dv=dv: e.tensor_tensor(out=dv[:, :, 8:16], in0=t2c, in1=t1s, op=ALU.add),
                     reads=["t2c", "t1s"], writes=[dn + "_b"])
                if os.environ.get("KROPE", "3") == "2":
                    continue
                S.op("act", lambda e, dv=dv: e.copy(out=dv[:, :, 16:64], in_=v[:, :, 16:64]), reads=[pgn], writes=[dn + "_c"])

        def proj_group(j, hTj, c0, ncols, bank):
            for kc in range(8):
                S.op("pe", lambda e, kc=kc: e.matmul(pb[bank][:, 0:ncols], lhsT=hTj[:, kc, :], rhs=win[:, kc, c0:c0 + ncols],
                                                     start=(kc == 0), stop=(kc == 7)),
                     reads=[f"hT{j % 2}"] + WIN_R, writes=[f"pb{bank}"])

        def tile_gen(j):
            b = j % 2
            own = j < NO
            xt = xbuf[b]
            S.op("sp", lambda e, j=j, xt=xt: e.dma_start(out=xt, in_=xw[j * 128:(j + 1) * 128, :]), writes=[f"x{b}"], dma=True)
            S.op("act", lambda e, xt=xt: e.activation(out=junk, in_=xt, func=AF.Square, scale=1.0 / 32, accum_out=small[:, 0:1]),
                 reads=[f"x{b}"], writes=["junk", "ss"])
            S.op("act", lambda e: e.activation(out=small[:, 1:2], in_=small[:, 0:1], func=AF.Sqrt, bias=EPS), reads=["ss"], writes=["rs"])
            S.op("dve", lambda e: e.reciprocal(out=small[:, 2:3], in_=small[:, 1:2]), reads=["rs"], writes=["rstd"])
            S.op("dve", lambda e, xt=xt, b=b: e.scalar_tensor_tensor(out=xn[b], in0=xt, scalar=small[:, 2:3], in1=gmix, op0=ALU.mult, op1=ALU.mult),
                 reads=[f"x{b}", "rstd", "gmix"], writes=[f"xn{b}"])
            pT = pbh[5].rearrange("p (k c) -> p k c", k=8)
            for kc in range(8):
                S.op("pe", lambda e, kc=kc, b=b: e.transpose(out=pT[:, kc, :], in_=xn[b][:, kc * 128:(kc + 1) * 128], identity=ident),
                     reads=[f"xn{b}", "ident"], writes=["pb5"])
            S.op("act", lambda e, b=b: e.copy(out=hT[b], in_=pT), reads=["pb5"], writes=[f"hT{b}"])
            yield
            qk = qk_tm[b]
            qkn = f"qk{b}"
            if own:
                bank = psel[0] % 3; psel[0] += 1
                proj_group(j, hT[b], 0, 512, bank)
                rope(j, pb[bank], 0, 8, [(qk[:, 0:512].rearrange("p (h d) -> p h d", h=8), qkn + "_qa")], {"pg": f"pb{bank}"})
            if j <= NO:
                bank = psel[0] % 3; psel[0] += 1
                proj_group(j, hT[b], 512, 256, bank)
                kd = qk[:, 512:768].rearrange("p (h t d) -> p h t d", h=2, t=2)
                rope(j, pb[bank], 0, 2, [(kd[:, :, 0, :], qkn + "_ka0"), (kd[:, :, 1, :], qkn + "_ka1")], {"pg": f"pb{bank}"})
                S.op("act", lambda e, j=j, bank=bank: e.copy(out=vA4[:, j, :, 0:64], in_=pb[bank][:, 128:256].rearrange("p (h d) -> p h d", h=2)),
                     reads=[f"pb{bank}"], writes=[f"vA{j}"])
            if own:
                bank = psel[0] % 3; psel[0] += 1
                proj_group(j, hT[b], 768, 512, bank)
                rope(j, pb[bank], 0, 8, [(qk[:, 768:1280].rearrange("p (h d) -> p h d", h=8), qkn + "_qb")], {"pg": f"pb{bank}"})
            bank = psel[0] % 3; psel[0] += 1
            proj_group(j, hT[b], 1280, 512, bank)
            rope(j, pb[bank], 0, 8, [(qk[:, 1280:1792].rearrange("p (h d) -> p h d", h=8), qkn + "_kb")], {"pg": f"pb{bank}"})
            bank = psel[0] % 3; psel[0] += 1
            proj_group(j, hT[b], 1792, 512, bank)
            S.op("dve", lambda e, j=j, bank=bank: e.tensor_copy(out=vB4[:, j, :, 0:64], in_=pb[bank].rearrange("p (h d) -> p h d", h=8)),
                 reads=[f"pb{bank}"], writes=[f"vB{j}"])
            yield
            qT3 = pbh[3].rearrange("p (k c) -> p k c", k=8)
            qT4 = pbh[4].rearrange("p (k c) -> p k c", k=8)

            def tr(dst_bank_view, slot, ft, rd, bankname, qk=qk):
                S.op("pe", lambda e: e.transpose(out=dst_bank_view[:, slot, :], in_=qk[:, ft * 128:(ft + 1) * 128], identity=ident),
                     reads=rd + ["ident"], writes=[bankname])
            rd_qa = [qkn + "_qa_a", qkn + "_qa_b", qkn + "_qa_c"]
            rd_ka = [qkn + s for s in ("_ka0_a", "_ka0_b", "_ka0_c", "_ka1_a", "_ka1_b", "_ka1_c")]
            rd_qb = [qkn + "_qb_a", qkn + "_qb_b", qkn + "_qb_c"]
            rd_kb = [qkn + "_kb_a", qkn + "_kb_b", qkn + "_kb_c"]
            if own:
                for t in range(4):
                    tr(qT3, t, t, rd_qa, "pb3")
            if j <= NO:
                for t in range(2):
                    tr(qT3, 4 + t, 4 + t, rd_ka, "pb3")
            if own:
                S.op("act", lambda e, j=j: e.copy(out=qaT[:, :, j * 128:(j + 1) * 128], in_=qT3[:, 0:4, :]), reads=["pb3"], writes=[f"qaT{j}"])
            if j <= NO:
                S.op("dve", lambda e, j=j: e.tensor_copy(out=kaT[:, :, j * 128:(j + 1) * 128], in_=qT3[:, 4:6, :]), reads=["pb3"], writes=[f"kaT{j}"])
            if own:
                for t in range(4):
                    tr(qT4, t, 6 + t, rd_qb, "pb4")
            for t in range(4):
                tr(qT4, 4 + t, 10 + t, rd_kb, "pb4")
            if own:
                S.op("act", lambda e, j=j: e.copy(out=qbT[:, :, j * 128:(j + 1) * 128], in_=qT4[:, 0:4, :]), reads=["pb4"], writes=[f"qbT{j}"])
            S.op("dve", lambda e, j=j: e.tensor_copy(out=kbT[:, :, j * 128:(j + 1) * 128], in_=qT4[:, 4:8, :]), reads=["pb4"], writes=[f"kbT{j}"])

        if level >= 1:
            gens = [tile_gen(j) for j in range(NT)]
            next(gens[0])
            for j in range(NT):
                if j + 1 < NT:
                    next(gens[j + 1])
                next(gens[j])
                if j >= 1:
                    next(gens[j - 1], None)
            next(gens[NT - 1], None)

        S.barrier()
        AR.off = mark_1a
        gout = AR.alloc([128, D], F32)
        wout = AR.alloc([128, 8, D], BF16)
        wr = AR.alloc([128, 8, NE], F32)
        NPB = 6
        Pb = [AR.alloc([128, 512], BF16) for _ in range(NPB)]
        o_all2 = AR.alloc([128, 2, 4 * D], BF16)
        mixed = [AR.alloc([128, D], BF16)] * 2
        x1t = [AR.alloc([128, D], F32)] * 2
        h2f = [AR.alloc([128, D], F32)] * 2
        h2b = [AR.alloc([128, D], BF16)] * 2
        h2T = AR.alloc([128, 4, 128], F32)
        print("phase1b sbuf bytes/partition:", AR.off)
        ld("sp", gout, gout_d.partition_broadcast(128), "gout")
        ld("sp", wr, wr_d.rearrange("(k p) c -> p k c", p=128), "wr")
        S.op("pool", lambda e: e.dma_start(out=wout, in_=wout_d.rearrange("(k p) c -> p k c", p=128)),
             writes=["wout"], dma=True)
        sbank = [0]
        pbuf = [0]
        OB = [3, 4, 6, 7]
        OBN = [f"pb{q}" for q in OB]
        HD = [0, 2, 1, 3]

        def o_view(g):
            return o_all2[:, g % 2, :].rearrange("p (q d) -> p q d", q=4)

        def attn_steps(g):
            steps = []
            o_all = o_view(g)
            gp = g % 2
            for kvh in range(2):
                for qi in range(4):
                    qb_ = 4 * g + qi
                    kbs = [kb for kb in (qb_ - 1, qb_, qb_ + 1) if 0 <= kb <= 16]
                    for kb in kbs:
                        st = {}

                        def s1(st=st, kb=kb, qb_=qb_, kvh=kvh):
                            pi = pbuf[0] % NPB; pbuf[0] += 1
                            st["pi"] = pi
                            P = Pb[pi]
                            for half in range(2):
                                sbk = sbank[0] % 3; sbank[0] += 1
                                r0 = half * 64
                                S.op("pe", lambda e, half=half, r0=r0, sbk=sbk: e.matmul(
                                    pb[sbk][:, 0:256].rearrange("p (a c) -> p a c", a=2),
                                    lhsT=kaT[r0:r0 + 64, kvh, kb * 128:(kb + 1) * 128],
                                    rhs=qaT[r0:r0 + 64, 2 * kvh:2 * kvh + 2, qb_ * 128:(qb_ + 1) * 128],
                                    start=True, stop=True),
                                    reads=[f"kaT{kb}", f"qaT{qb_}"], writes=[f"pb{sbk}"])
                                S.op("act", lambda e, sbk=sbk, half=half: e.activation(out=P[:, half * 256:(half + 1) * 256], in_=pb[sbk][:, 0:256], func=AF.Exp, scale=0.125),
                                     reads=[f"pb{sbk}"], writes=[f"P{pi}"])
                            if kb != qb_:
                                blk = 0 if kb < qb_ else 2
                                S.op("dve", lambda e: e.tensor_tensor(
                                    out=P.rearrange("p (h c) -> p h c", h=4), in0=P.rearrange("p (h c) -> p h c", h=4),
                                    in1=maskA[:, blk * 128:(blk + 1) * 128].unsqueeze(1).to_broadcast([128, 4, 128]), op=ALU.mult),
                                    reads=[f"P{pi}", "maskA"], writes=[f"P{pi}"])

                        def s2(st=st, kb=kb, kbs=kbs, kvh=kvh, qi=qi):
                            pi = st["pi"]
                            P = Pb[pi]
                            for hh in range(4):
                                S.op("pe", lambda e, hh=hh: e.matmul(
                                    pb[OB[hh]][:, 0:65], lhsT=P[:, hh * 128:(hh + 1) * 128], rhs=vA4[:, kb, kvh, :],
                                    start=(kb == kbs[0]), stop=(kb == kbs[-1])),
                                    reads=[f"P{pi}", f"vA{kb}", "vA_ones"], writes=[OBN[hh]])
                            if kb == kbs[-1]:
                                denA = small[:, 8:12]
                                for hh in range(4):
                                    S.op("dve", lambda e, hh=hh: e.tensor_tensor(out=denA[:, hh:hh + 1], in0=pb[OB[hh]][:, 64:65],
                                                                                 in1=esink[:, kvh * 4 + hh:kvh * 4 + hh + 1], op=ALU.add),
                                         reads=[OBN[hh], "esink"], writes=[f"den{hh}"])
                                    S.op("dve", lambda e, hh=hh: e.reciprocal(out=denA[:, hh:hh + 1], in_=denA[:, hh:hh + 1]), reads=[f"den{hh}"], writes=[f"den{hh}"])
                                    hd = kvh * 4 + HD[hh]
                                    S.op("dve", lambda e, hh=hh, hd=hd: e.tensor_scalar(
                                        out=o_all[:, qi, hd * 64:(hd + 1) * 64], in0=pb[OB[hh]][:, 0:64], scalar1=denA[:, hh:hh + 1], scalar2=None, op0=ALU.mult),
                                        reads=[OBN[hh], f"den{hh}"], writes=[f"oall{gp}_{qi}A{kvh}_{hh}"])
                        steps.append((s1, s2))
            for h in range(8):
                ft = h // 2
                r0 = (h % 2) * 64
                kb_list = []
                for kb in range(max(0, 4 * g - 8), min(NT, 4 * g + 12)):
                    qlo = max(4 * g, kb - 8)
                    qhi = min(4 * g + 3, kb + 8)
                    if qhi >= qlo:
                        kb_list.append((kb, qlo, qhi))
                for (kb, qlo, qhi) in kb_list:
                    st = {}
                    ncol = (qhi - qlo + 1) * 128

                    def s1(st=st, kb=kb, qlo=qlo, qhi=qhi, ncol=ncol, ft=ft, r0=r0):
                        sbk = sbank[0] % 3; sbank[0] += 1
                        pi = pbuf[0] % NPB; pbuf[0] += 1
                        st["pi"] = pi
                        P = Pb[pi]
                        S.op("pe", lambda e: e.matmul(
                            pb[sbk][:, 0:ncol], lhsT=kbT[r0:r0 + 64, ft, kb * 128:(kb + 1) * 128],
                            rhs=qbT[r0:r0 + 64, ft, qlo * 128:qlo * 128 + ncol], start=True, stop=True),
                            reads=[f"kbT{kb}"] + [f"qbT{q}" for q in range(qlo, qhi + 1)], writes=[f"pb{sbk}"])
                        S.op("act", lambda e: e.activation(out=P[:, 0:ncol], in_=pb[sbk][:, 0:ncol], func=AF.Exp, scale=0.125),
                             reads=[f"pb{sbk}"], writes=[f"P{pi}"])
                        m0 = (qlo - kb + 11) * 128
                        S.op("dve", lambda e: e.tensor_tensor(out=P[:, 0:ncol], in0=P[:, 0:ncol], in1=maskB[:, m0:m0 + ncol], op=ALU.mult),
                             reads=[f"P{pi}", "maskB"], writes=[f"P{pi}"])

                    def s2(st=st, kb=kb, qlo=qlo, qhi=qhi, h=h, last_kb=kb_list[-1][0]):
                        pi = st["pi"]
                        P = Pb[pi]
                        for qb_ in range(qlo, qhi + 1):
                            first = max(0, qb_ - 8)
                            last = min(NT - 1, qb_ + 8)
                            S.op("pe", lambda e, qb_=qb_, first=first, last=last: e.matmul(
                                pb[OB[qb_ - 4 * g]][:, 0:65], lhsT=P[:, (qb_ - qlo) * 128:(qb_ - qlo + 1) * 128], rhs=vB4[:, kb, h, :],
                                start=(kb == first), stop=(kb == last)),
                                reads=[f"P{pi}", f"vB{kb}", "vB_ones"], writes=[OBN[qb_ - 4 * g]])
                        if kb == last_kb:
                            denB = small[:, 12:16]
                            for qi in range(4):
                                S.op("dve", lambda e, qi=qi: e.reciprocal(out=denB[:, qi:qi + 1], in_=pb[OB[qi]][:, 64:65]), reads=[OBN[qi]], writes=[f"denB{qi}"])
                                S.op("dve", lambda e, qi=qi: e.tensor_scalar(
                                    out=o_all[:, qi, 512 + h * 64:512 + (h + 1) * 64], in0=pb[OB[qi]][:, 0:64], scalar1=denB[:, qi:qi + 1], scalar2=None, op0=ALU.mult),
                                    reads=[OBN[qi], f"denB{qi}"], writes=[f"oallB{gp}_{h}_{qi}"])
                    steps.append((s1, s2))
            return steps

        def finish_tile(g, qi):
            j = 4 * g + qi
            b = j % 2
            gp = g % 2
            o_all = o_view(g)
            oa_r = [f"oall{gp}_{qi}A{k_}_{h_}" for k_ in range(2) for h_ in range(4)]
            ob_r = [f"oallB{gp}_{h}_{qi}" for h in range(8)]
            for gi, rd in ((0, oa_r), (1, ob_r)):
                S.op("act", lambda e, gi=gi: e.activation(out=junk[:, 0:512], in_=o_all[:, qi, gi * 512:(gi + 1) * 512], func=AF.Square,
                                                          scale=float(1.0 / np.sqrt(512.0)), accum_out=small[:, 16 + gi:17 + gi]),
                     reads=rd, writes=["junk", f"gss{gi}"])
                S.op("act", lambda e, gi=gi: e.activation(out=small[:, 18 + gi:19 + gi], in_=small[:, 16 + gi:17 + gi], func=AF.Sqrt, bias=EPS),
                     reads=[f"gss{gi}"], writes=[f"grs{gi}"])
                S.op("dve", lambda e, gi=gi: e.reciprocal(out=small[:, 20 + gi:21 + gi], in_=small[:, 18 + gi:19 + gi]), reads=[f"grs{gi}"], writes=[f"grstd{gi}"])
                S.op("dve", lambda e, gi=gi: e.scalar_tensor_tensor(
                    out=mixed[0][:, gi * 512:(gi + 1) * 512], in0=o_all[:, qi, gi * 512:(gi + 1) * 512], scalar=small[:, 20 + gi:21 + gi],
                    in1=gout[:, gi * 512:(gi + 1) * 512], op0=ALU.mult, op1=ALU.mult),
                    reads=rd + [f"grstd{gi}", "gout"], writes=[f"mixed0_{gi}"])
            yield
            pT = pbh[5].rearrange("p (k c) -> p k c", k=8)
            for kc in range(8):
                S.op("pe", lambda e, kc=kc: e.transpose(out=pT[:, kc, :], in_=mixed[0][:, kc * 128:(kc + 1) * 128], identity=ident),
                     reads=["mixed0_0", "mixed0_1", "ident"], writes=["pb5"])
            S.op("act", lambda e: e.copy(out=hT[b], in_=pT), reads=["pb5"], writes=[f"hT{b}"])
            xt = xbuf[b]
            S.op("sp", lambda e: e.dma_start(out=xt, in_=xw[j * 128:(j + 1) * 128, :]), writes=[f"x{b}"], dma=True)
            yield
            for hf in range(2):
                for kc in range(8):
                    S.op("pe", lambda e, kc=kc, hf=hf: e.matmul(
                        pb[5], lhsT=hT[b][:, kc, :], rhs=wout[:, kc, hf * 512:(hf + 1) * 512], start=(kc == 0), stop=(kc == 7)),
                        reads=[f"hT{b}", "wout"], writes=["pb5"])
                S.op("dve", lambda e, hf=hf: e.tensor_tensor(
                    out=x1t[0][:, hf * 512:(hf + 1) * 512], in0=pb[5], in1=xt[:, hf * 512:(hf + 1) * 512], op=ALU.add),
                    reads=["pb5", f"x{b}"], writes=[f"x1t0_{hf}"])
                yield
            x1r = ["x1t0_0", "x1t0_1"]
            S.op("sp", lambda e: e.dma_start(out=acc_d[j * 128:(j + 1) * 128, :], in_=x1t[0]), reads=x1r, writes=["acc"], dma=True)
            if debug:
                S.op("sp", lambda e: e.dma_start(out=dbg_x1[j * 128:(j + 1) * 128, :], in_=x1t[0]), reads=x1r, dma=True)
            S.op("act", lambda e: e.activation(out=junk, in_=x1t[0], func=AF.Square, scale=1.0 / 32, accum_out=small[:, 24:25]),
                 reads=x1r, writes=["junk", "ss2"])
            S.op("act", lambda e: e.activation(out=small[:, 25:26], in_=small[:, 24:25], func=AF.Sqrt, bias=EPS), reads=["ss2"], writes=["rs2"])
            S.op("dve", lambda e: e.reciprocal(out=small[:, 26:27], in_=small[:, 25:26]), reads=["rs2"], writes=["rstd2"])
            S.op("dve", lambda e: e.scalar_tensor_tensor(out=h2f[0], in0=x1t[0], scalar=small[:, 26:27], in1=gffn, op0=ALU.mult, op1=ALU.mult),
                 reads=x1r + ["rstd2", "gffn"], writes=["h2f0"])
            S.op("act", lambda e: e.copy(out=h2b[0], in_=h2f[0]), reads=["h2f0"], writes=["h2b0"])
            S.op("sp", lambda e: e.dma_start(out=h2_d[j * 128:(j + 1) * 128, :], in_=h2b[0]), reads=["h2b0"], writes=["h2d"], dma=True)
            yield
            lg = small[:, 44:60]
            for part in range(2):
                for k4 in range(4):
                    kc = part * 4 + k4
                    S.op("pe", lambda e, kc=kc, k4=k4: e.transpose(
                        out=pb[5][:, k4 * 128:(k4 + 1) * 128], in_=h2f[0][:, kc * 128:(kc + 1) * 128], identity=identf),
                        reads=["h2f0", "identf"], writes=["pb5"])
                eng = "act" if part == 0 else "dve"
                if eng == "act":
                    S.op("act", lambda e: e.copy(out=h2T, in_=pb[5].rearrange("p (k c) -> p k c", k=4)), reads=["pb5"], writes=["h2T"])
                else:
                    S.op("dve", lambda e: e.tensor_copy(out=h2T, in_=pb[5].rearrange("p (k c) -> p k c", k=4)), reads=["pb5"], writes=["h2T"])
                yield
                for k4 in range(4):
                    kc = part * 4 + k4
                    S.op("pe", lambda e, kc=kc, k4=k4: e.matmul(pb[5][:, 0:NE], lhsT=h2T[:, k4, :], rhs=wr[:, kc, :], start=(k4 == 0), stop=(k4 == 3)),
                         reads=["h2T", "wr"], writes=["pb5"])
                if part == 0:
                    S.op("dve", lambda e: e.tensor_copy(out=lg, in_=pb[5][:, 0:NE]), reads=["pb5"], writes=["lg"])
                else:
                    S.op("dve", lambda e: e.tensor_tensor(out=lg, in0=pb[5][:, 0:NE], in1=lg, op=ALU.add), reads=["pb5", "lg"], writes=["lg"])
                yield
            S.op("dve", lambda e: e.reduce_max(out=small[:, 28:29], in_=lg, axis=AX.X), reads=["lg"], writes=["lmax"])
            S.op("dve", lambda e: e.tensor_scalar(out=small[:, 29:30], in0=small[:, 28:29], scalar1=-1.0, scalar2=None, op0=ALU.mult),
                 reads=["lmax"], writes=["nlmax"])
            S.op("act", lambda e: e.activation(out=aff[:, j, :], in_=lg, func=AF.Exp, bias=small[:, 29:30], scale=1.0,
                                               accum_out=small[:, 30:31]),
                 reads=["lg", "nlmax"], writes=[f"aff{j}", "lsum"])
            S.op("dve", lambda e: e.reciprocal(out=small[:, 31:32], in_=small[:, 30:31]), reads=["lsum"], writes=["rlsum"])
            S.op("dve", lambda e: e.tensor_scalar(out=aff[:, j, :], in0=aff[:, j, :], scalar1=small[:, 31:32], scalar2=None, op0=ALU.mult),
                 reads=[f"aff{j}", "rlsum"], writes=[f"aff{j}"])
            yield

        def chain(gens):
            for gn in gens:
                yield from gn

        LOOK = 3
        pending = None
        for g in range(4 if level >= 2 else 0):
            steps = attn_steps(g)
            n = len(steps)
            every = 3
            for i in range(n + LOOK):
                if i < n:
                    steps[i][0]()
                if i >= LOOK:
                    steps[i - LOOK][1]()
                if pending is not None and i % every == every - 1:
                    if next(pending, "done") == "done":
                        pending = None
            if pending is not None:
                for _ in pending:
                    pass
            pending = chain([finish_tile(g, qi) for qi in range(4)])
        if pending is not None:
            for _ in pending:
                pass

        AFF_R = [f"aff{j}" for j in range(NO)]
        if debug and level == 0:
            aff_in = din("aff_in", [128, NO * NE])
            S.op("sp", lambda e: e.dma_start(out=aff.rearrange("p a b -> p (a b)"), in_=aff_in), writes=AFF_R, dma=True)
        if debug and level >= 2:
            S.op("sp", lambda e: e.dma_start(out=dbg_aff, in_=aff.rearrange("p a b -> p (a b)")), reads=AFF_R, dma=True)

        if not stop_after_phase1:
            S.barrier()
            AR.off = mark_persist
            afull = AR.alloc([128, 2, NO * NE], F32)
            cmpb = AR.alloc([128, 2 * NO, NE], F32)
            lo = AR.alloc([128, NE], F32)
            hi = AR.alloc([128, NE], F32)
            mid = AR.alloc([128, NE], F32)
            cntp = AR.alloc([128, NE], F32)
            sel = AR.alloc([128, NE], F32)
            tmpn = AR.alloc([128, NE], F32)
            msk = AR.alloc([128, NO, NE], F32)
            dest = AR.alloc([128, NO, NE], F32)
            offs = AR.alloc([128, NO, NE], F32)
            vals = AR.alloc([128, NO, 6], BF16)
            gres = AR.alloc([128, NO], F32)
            selb = [AR.alloc([128, CAP], BF16) for _ in range(2)]
            idxf = AR.alloc([128, 4, 6], F32)
            idxi = [[AR.alloc([128, 1], I32) for _c in range(4)] for _ in range(2)]
            gate = [AR.alloc([128, 4], F32) for _ in range(2)]
            xg = AR.alloc([128, 4, D], BF16)
            xeT = [AR.alloc([128, 8, CAP], BF16) for _ in range(2)]
            hTm = AR.alloc([128, 16, CAP], BF16)
            sa = [AR.alloc([128, CAP], F32) for _ in range(2)]
            yo = [AR.alloc([128, D], F32) for _ in range(2)]
            wgb = [AR.alloc([128, 8, 1024], BF16) for _ in range(2)]
            wub = [AR.alloc([128, 8, 1024], BF16) for _ in range(2)]
            wdb = [AR.alloc([128, 8, D], BF16) for _ in range(3)]
            print("phase2 sbuf bytes/partition:", AR.off)

            aff2 = aff.rearrange("p a b -> p (a b)")
            S.op("dve", lambda e: e.tensor_scalar(out=afull[:, 0, :], in0=aff2, scalar1=par[:, 0:1], scalar2=None, op0=ALU.mult),
                 reads=AFF_R + ["par"], writes=["afull0"])
            S.op("dve", lambda e: e.tensor_scalar(out=afull[:, 1, :], in0=aff2, scalar1=par[:, 1:2], scalar2=None, op0=ALU.mult),
                 reads=AFF_R + ["par"], writes=["afull1"])
            S.op("pool", lambda e: e.dma_start(out=cin_d[:, :], in_=afull.rearrange("p a b -> p (a b)")), reads=["afull0", "afull1"], writes=["cin"], dma=True)

            def cc(e):
                return e.collective_compute("AllReduce", ALU.add, replica_groups=[[0, 1], [2, 3], [4, 5], [6, 7]],
                                            ins=[cin_d.ap().opt()], outs=[cout_d.ap().opt()])
            S.cnt["cc"] = 0
            S.ops["pool"].append(([(k, v) for k, v in [S.last_w["cin"]]], None, None))
            S.waited["pool"][S.last_w["cin"][0]] = max(S.waited["pool"].get(S.last_w["cin"][0], 0), S.last_w["cin"][1])
            S.ops["pool"].append(([], cc, ("cc", 1)))
            S.last_w["cout"] = ("cc", 1)
            S.readers["cout"] = []
            S.all_tokens["cc"] = 1
            S.op("pool", lambda e: e.dma_start(out=afull.rearrange("p a b -> p (a b)"), in_=cout_d[:, :]), reads=["cout"], writes=["afull0", "afull1"], dma=True)
            AF_R = ["afull0", "afull1"]
            S.op("dve", lambda e: e.memset(lo, 0.0), writes=["lo"])
            S.op("dve", lambda e: e.memset(hi, 1.0), writes=["hi"])
            af3 = afull.rearrange("p a (j e) -> p (a j) e", e=NE)
            for it in range(26):
                step = float(2.0 ** -(it + 1))
                S.op("dve", lambda e, step=step: e.tensor_scalar(out=mid, in0=lo, scalar1=step, scalar2=None, op0=ALU.add), reads=["lo"], writes=["mid"])
                S.op("dve", lambda e: e.tensor_tensor(out=cmpb, in0=af3, in1=mid.unsqueeze(1).to_broadcast([128, 2 * NO, NE]), op=ALU.is_ge),
                     reads=AF_R + ["mid"], writes=["cmpb"])
                S.op("dve", lambda e: e.tensor_reduce(out=cntp, in_=cmpb.rearrange("p s e -> p e s"), axis=AX.X, op=ALU.add), reads=["cmpb"], writes=["cntp"])
                S.op("pe", lambda e: e.matmul(pb[5][:, 0:NE], lhsT=ones, rhs=cntp, start=True, stop=True), reads=["ones", "cntp"], writes=["pb5"])
                S.op("dve", lambda e, step=step: e.tensor_scalar(out=sel, in0=pb[5][:, 0:NE], scalar1=511.5, scalar2=step, op0=ALU.is_ge, op1=ALU.mult),
                     reads=["pb5"], writes=["sel"])
                S.op("dve", lambda e: e.tensor_tensor(out=lo, in0=lo, in1=sel, op=ALU.add), reads=["lo", "sel"], writes=["lo"])
            S.op("dve", lambda e: e.tensor_copy(out=hi, in_=lo), reads=["lo"], writes=["hi"])
            if debug:
                S.op("sp", lambda e: e.dma_start(out=dbg_thr[:, 0:NE], in_=lo), reads=["lo"], dma=True)
                S.op("sp", lambda e: e.dma_start(out=dbg_thr[:, NE:2 * NE], in_=hi), reads=["hi"], dma=True)
            S.op("dve", lambda e: e.tensor_tensor(out=msk, in0=aff, in1=lo.unsqueeze(1).to_broadcast([128, NO, NE]), op=ALU.is_ge),
                 reads=AFF_R + ["lo"], writes=["msk"])
            mk2 = msk.rearrange("p a b -> p (a b)")
            S.op("pe", lambda e: e.matmul(pb[3][:, 0:NO * NE], lhsT=tri, rhs=mk2, start=True, stop=True), reads=["tri", "msk"], writes=["pb3"])
            S.op("pe", lambda e: e.matmul(pb[4][:, 0:NO * NE], lhsT=ones, rhs=mk2, start=True, stop=True), reads=["ones", "msk"], writes=["pb4"])
            tot = pb[4][:, 0:NO * NE].rearrange("p (a b) -> p a b", a=NO)
            S.op("dve", lambda e: e.memset(offs[:, 0, :], 0.0), writes=["offs"])
            for j in range(1, NO):
                S.op("dve", lambda e, j=j: e.tensor_tensor(out=offs[:, j, :], in0=offs[:, j - 1, :], in1=tot[:, j - 1, :], op=ALU.add),
                     reads=["offs", "pb4"], writes=["offs"])
            S.op("dve", lambda e: e.tensor_tensor(out=dest, in0=offs, in1=pb[3][:, 0:NO * NE].rearrange("p (a b) -> p a b", a=NO), op=ALU.add),
                 reads=["offs", "pb3"], writes=["dest"])
            S.op("dve", lambda e: e.tensor_scalar(out=msk, in0=msk, scalar1=-BIG, scalar2=BIG, op0=ALU.mult, op1=ALU.add), reads=["msk"], writes=["msk"])
            S.op("dve", lambda e: e.tensor_tensor(out=dest, in0=dest, in1=msk, op=ALU.add), reads=["dest", "msk"], writes=["dest"])
            S.op("dve", lambda e: e.tensor_copy(out=vals[:, :, 0:3], in_=tokc), reads=["tokc"], writes=["vals_c"])
            S.op("pool", lambda e: e.memset(xg, 0.0), writes=["xg0", "xg1", "xg2", "xg3"])

            wg_src = [wg_d[x].rearrange("(k p) f -> p k f", p=128) for x in range(n_exp)]
            wu_src = [wu_d[x].rearrange("(k p) f -> p k f", p=128) for x in range(n_exp)]
            wd_src = [wd_d[x].rearrange("(k p) c -> p k c", p=128) for x in range(n_exp)]

            def load_weights(x):
                for hf in range(2):
                    S.op("pool", lambda e, x=x, hf=hf: e.dma_start(out=wgb[hf], in_=wg_src[x][:, :, hf * 1024:(hf + 1) * 1024]),
                         writes=[f"wg{hf}"], dma=True)
                    S.op("pool", lambda e, x=x, hf=hf: e.dma_start(out=wub[hf], in_=wu_src[x][:, :, hf * 1024:(hf + 1) * 1024]),
                         writes=[f"wu{hf}"], dma=True)
                for hf in range(2):
                    bi = (2 * x + hf) % 3
                    S.op("pool", lambda e, x=x, hf=hf, bi=bi: e.dma_start(out=wdb[bi], in_=wd_src[x][:, hf * 8:(hf + 1) * 8, :]),
                         writes=[f"wd{bi}"], dma=True)

            CH = [(0, 128), (128, 128), (256, 128), (384, 128)]
            _DBG["g"] = (xg, h2_d, idxi)
            load_weights(0)
            for x in range(n_exp):
                pp = x % 2
                S.op("dve", lambda e, x=x: e.tensor_copy(out=vals[:, :, 3], in_=aff[:, :, x]), reads=AFF_R, writes=["vals_g"])
                S.op("dve", lambda e, x=x: e.tensor_tensor(out=gres, in0=aff[:, :, x], in1=vals[:, :, 3], op=ALU.subtract), reads=AFF_R + ["vals_g"], writes=["gres"])
                S.op("dve", lambda e: e.tensor_copy(out=vals[:, :, 4], in_=gres), reads=["gres"], writes=["vals_g"])
                S.op("dve", lambda e: e.tensor_tensor(out=gres, in0=gres, in1=vals[:, :, 4], op=ALU.subtract), reads=["gres", "vals_g"], writes=["gres"])
                S.op("dve", lambda e: e.tensor_copy(out=vals[:, :, 5], in_=gres), reads=["gres"], writes=["vals_g"])
                for j in range(NO):
                    sb_ = selb[j % 2]
                    S.op("dve", lambda e, j=j, x=x, sb_=sb_: e.tensor_scalar(out=sb_, in0=iota, scalar1=dest[:, j, x:x + 1], scalar2=None, op0=ALU.is_equal),
                         reads=["iota", "dest"], writes=[f"selb{j % 2}"])
                    for c, (s0, sn) in enumerate(CH):
                        IB = [5, 4, 6, 7]
                        S.op("pe", lambda e, j=j, c=c, s0=s0, sn=sn, sb_=sb_: e.matmul(
                            pb[IB[c]][0:sn, 0:6], lhsT=sb_[:, s0:s0 + sn], rhs=vals[:, j, :], start=(j == 0), stop=(j == NO - 1)),
                            reads=[f"selb{j % 2}", "vals_c", "vals_g"], writes=[f"pb{IB[c]}"])
                for c in range(4):
                    S.op("act", lambda e, c=c: e.copy(out=idxf[:, c, :], in_=pb[[5, 4, 6, 7][c]][:, 0:6]), reads=[f"pb{[5, 4, 6, 7][c]}"], writes=["idxf"])
                S.op("dve", lambda e: e.tensor_scalar(out=idxf[:, :, 0], in0=idxf[:, :, 0], scalar1=64.0, scalar2=None, op0=ALU.mult), reads=["idxf"], writes=["idxf"])
                S.op("dve", lambda e: e.tensor_tensor(out=idxf[:, :, 0], in0=idxf[:, :, 0], in1=idxf[:, :, 1], op=ALU.add), reads=["idxf"], writes=["idxf"])
                S.op("dve", lambda e: e.tensor_scalar(out=idxf[:, :, 2], in0=idxf[:, :, 2], scalar1=-BIG, scalar2=BIG, op0=ALU.mult, op1=ALU.add), reads=["idxf"], writes=["idxf"])
                S.op("dve", lambda e: e.tensor_tensor(out=idxf[:, :, 0], in0=idxf[:, :, 0], in1=idxf[:, :, 2], op=ALU.add), reads=["idxf"], writes=["idxf"])
                for c in range(4):
                    S.op("dve", lambda e, pp=pp, c=c: e.tensor_copy(out=idxi[pp][c], in_=idxf[:, c, 0:1]), reads=["idxf"], writes=[f"idxi{pp}"])
                S.op("dve", lambda e, pp=pp: e.tensor_tensor(out=gate[pp], in0=idxf[:, :, 3], in1=idxf[:, :, 4], op=ALU.add), reads=["idxf"], writes=[f"gate{pp}"])
                S.op("dve", lambda e, pp=pp: e.tensor_tensor(out=gate[pp], in0=gate[pp], in1=idxf[:, :, 5], op=ALU.add), reads=["idxf", f"gate{pp}"], writes=[f"gate{pp}"])
                for c, (s0, sn) in enumerate(CH):
                    S.op("pool", lambda e, c=c, sn=sn, pp=pp: e.indirect_dma_start(
                        out=xg[0:sn, c, :], out_offset=None, in_=h2_d[:, :],
                        in_offset=bass.IndirectOffsetOnAxis(ap=idxi[pp][c][0:sn, :], axis=0),
                        bounds_check=_bc(e, OWN - 1), oob_is_err=False),
                        reads=[f"idxi{pp}", "h2d"], writes=[f"xg{c}"], dma=True)
                pT = pbh[5].rearrange("p (k c) -> p k c", k=8)
                for c, (s0, sn) in enumerate(CH):
                    for kc in range(8):
                        S.op("pe", lambda e, c=c, kc=kc, sn=sn: e.transpose(out=pT[:, kc, 0:sn], in_=xg[0:sn, c, kc * 128:(kc + 1) * 128], identity=ident[0:sn, 0:sn]),
                             reads=[f"xg{c}", "ident"], writes=["pb5"])
                    eng = "act" if c % 2 == 0 else "dve"
                    if eng == "act":
                        S.op("act", lambda e, s0=s0, sn=sn, pp=pp: e.copy(out=xeT[pp][:, :, s0:s0 + sn], in_=pT[:, :, 0:sn]), reads=["pb5"], writes=[f"xeT{pp}_{c}"])
                    else:
                        S.op("dve", lambda e, s0=s0, sn=sn, pp=pp: e.tensor_copy(out=xeT[pp][:, :, s0:s0 + sn], in_=pT[:, :, 0:sn]), reads=["pb5"], writes=[f"xeT{pp}_{c}"])
                XE_R = [f"xeT{pp}_{c}" for c in range(4)]
                for fc in range(16):
                    hf = fc // 8
                    ba = (2 * fc) % 4
                    bu = (2 * fc + 1) % 4
                    for kc in range(8):
                        S.op("pe", lambda e, kc=kc, fc=fc, hf=hf, ba=ba, pp=pp: e.matmul(
                            pb[ba][:, 0:CAP], lhsT=wgb[hf][:, kc, (fc % 8) * 128:(fc % 8 + 1) * 128], rhs=xeT[pp][:, kc, :], start=(kc == 0), stop=(kc == 7)),
                            reads=[f"wg{hf}"] + XE_R, writes=[f"pb{ba}"])
                    for kc in range(8):
                        S.op("pe", lambda e, kc=kc, fc=fc, hf=hf, bu=bu, pp=pp: e.matmul(
                            pb[bu][:, 0:CAP], lhsT=wub[hf][:, kc, (fc % 8) * 128:(fc % 8 + 1) * 128], rhs=xeT[pp][:, kc, :], start=(kc == 0), stop=(kc == 7)),
                            reads=[f"wu{hf}"] + XE_R, writes=[f"pb{bu}"])
                    S.op("act", lambda e, fc=fc, ba=ba: e.activation(out=sa[fc % 2], in_=pb[ba][:, 0:CAP], func=AF.Silu), reads=[f"pb{ba}"], writes=[f"sa{fc % 2}"])
                    S.op("dve", lambda e, fc=fc, bu=bu: e.tensor_tensor(out=hTm[:, fc, :], in0=pb[bu][:, 0:CAP], in1=sa[fc % 2], op=ALU.mult),
                         reads=[f"pb{bu}", f"sa{fc % 2}"], writes=[f"hTm{fc}"])
                    if fc == 7 and x + 1 < n_exp:
                        S.op("pool", lambda e, x=x: e.dma_start(out=wgb[0], in_=wg_src[x + 1][:, :, 0:1024]), writes=["wg0"], dma=True)
                        S.op("pool", lambda e, x=x: e.dma_start(out=wub[0], in_=wu_src[x + 1][:, :, 0:1024]), writes=["wu0"], dma=True)
                if x + 1 < n_exp:
                    S.op("pool", lambda e, x=x: e.dma_start(out=wgb[1], in_=wg_src[x + 1][:, :, 1024:2048]), writes=["wg1"], dma=True)
                    S.op("pool", lambda e, x=x: e.dma_start(out=wub[1], in_=wu_src[x + 1][:, :, 1024:2048]), writes=["wu1"], dma=True)
                H_R = [f"hTm{fc}" for fc in range(16)]
                for c, (s0, sn) in enumerate(CH):
                    yb = (4 * x + c) % 2
                    for hf2 in range(2):
                        bank = 6 + hf2
                        for fc in range(16):
                            bi = (2 * x + fc // 8) % 3
                            S.op("pe", lambda e, fc=fc, bi=bi, s0=s0, sn=sn, hf2=hf2, bank=bank: e.matmul(
                                pb[bank][0:sn, :], lhsT=hTm[:, fc, s0:s0 + sn], rhs=wdb[bi][:, fc % 8, hf2 * 512:(hf2 + 1) * 512],
                                start=(fc == 0), stop=(fc == 15)),
                                reads=[f"hTm{fc}", f"wd{bi}"], writes=[f"pb{bank}"])
                        if hf2 == 0:
                            S.op("act", lambda e, sn=sn, yb=yb, c=c, pp=pp, bank=bank: e.activation(
                                out=yo[yb][0:sn, 0:512], in_=pb[bank][0:sn, :], func=AF.Copy, scale=gate[pp][0:sn, c:c + 1]),
                                reads=[f"pb{bank}", f"gate{pp}"], writes=[f"yo{yb}_0"])
                        else:
                            S.op("dve", lambda e, sn=sn, yb=yb, c=c, pp=pp, bank=bank: e.tensor_scalar(
                                out=yo[yb][0:sn, 512:1024], in0=pb[bank][0:sn, :], scalar1=gate[pp][0:sn, c:c + 1], scalar2=None, op0=ALU.mult),
                                reads=[f"pb{bank}", f"gate{pp}"], writes=[f"yo{yb}_1"])
                    prev = ["acc"] if x == 0 else [f"accw{x - 1}_{k}" for k in range(4)]
                    S.op("pool", lambda e, yb=yb, c=c, pp=pp: e.indirect_dma_start(
                        out=acc_d[:, :], out_offset=bass.IndirectOffsetOnAxis(ap=idxi[pp][c][:, :], axis=0),
                        in_=yo[yb][:, :], in_offset=None, bounds_check=_bc(e, OWN - 1), oob_is_err=False, compute_op=ALU.add),
                        reads=[f"yo{yb}_0", f"yo{yb}_1", f"idxi{pp}"] + prev, writes=[f"accw{x}_{c}"], dma=True)
                if x + 1 < n_exp:
                    for hf in range(2):
                        bi = (2 * (x + 1) + hf) % 3
                        S.op("pool", lambda e, x=x, hf=hf, bi=bi: e.dma_start(out=wdb[bi], in_=wd_src[x + 1][:, hf * 8:(hf + 1) * 8, :]),
                             writes=[f"wd{bi}"], dma=True)

        S.barrier()
        AR.off = mark_persist
        NFIN = 4
        fin = [AR.alloc([128, D], F32) for _ in range(NFIN)]
        fjunk = AR.alloc([128, D], F32)
        ACC_FINAL = ["acc"] if stop_after_phase1 else [f"accw{n_exp - 1}_{k}" for k in range(4)]
        def fin_load(jj):
            bb = jj % NFIN
            S.op("sp", lambda e: e.dma_start(out=fin[bb], in_=acc_d[jj * 128:(jj + 1) * 128, :]), reads=ACC_FINAL, writes=[f"fin{bb}"], dma=True)
        for jj in range(NFIN - 1):
            fin_load(jj)
        for j in range(NO):
            if j + NFIN - 1 < NO:
                fin_load(j + NFIN - 1)
            b = j % NFIN
            c4 = 40 + 4 * (j % 2)
            S.op("act", lambda e, b=b, c4=c4: e.activation(out=fjunk, in_=fin[b], func=AF.Square, scale=1.0 / 32, accum_out=small[:, c4:c4 + 1]),
                 reads=[f"fin{b}"], writes=["junkf", f"fss{j % 2}"])
            S.op("act", lambda e, c4=c4: e.activation(out=small[:, c4 + 1:c4 + 2], in_=small[:, c4:c4 + 1], func=AF.Sqrt, bias=EPS), reads=[f"fss{j % 2}"], writes=[f"frs{j % 2}"])
            S.op("dve", lambda e, c4=c4: e.reciprocal(out=small[:, c4 + 2:c4 + 3], in_=small[:, c4 + 1:c4 + 2]), reads=[f"frs{j % 2}"], writes=[f"frstd{j % 2}"])
            S.op("dve", lambda e, b=b, c4=c4: e.scalar_tensor_tensor(out=fin[b], in0=fin[b], scalar=small[:, c4 + 2:c4 + 3], in1=gfin, op0=ALU.mult, op1=ALU.mult),
                 reads=[f"fin{b}", f"frstd{j % 2}", "gfin"], writes=[f"fin{b}"])
            S.op("sp", lambda e, j=j, b=b: e.dma_start(out=out_d[j * 128:(j + 1) * 128, :], in_=fin[b]), reads=[f"fin{b}"], dma=True)
        S.barrier()
        sems["cc"] = es.enter_context(nc.semaphore("sem_cc"))
        S.emit(sems)
    return nc


def _weight_mask(delta):
    a = np.abs(delta)
    w = (a <= 64).astype(np.float32)
    w += ((a <= 256) & (delta % 4 == 0)).astype(np.float32)
    w += ((a <= 1024) & (delta % 16 == 0)).astype(np.float32)
    return w


def _consts():
    bf = ml_dtypes.bfloat16
    kk = np.arange(128)[:, None]
    qq = np.arange(128)[None, :]
    mA = np.concatenate([(kk >= qq), np.ones((128, 128), bool), (kk <= qq)], axis=1).astype(np.float32)
    blocks = []
    for o in range(23):
        d = 128 * (11 - o) + kk - qq
        blocks.append(_weight_mask(d))
    mB = np.concatenate(blocks, axis=1)
    tri = (kk < qq).astype(np.float32)
    tok = (np.arange(NO)[None, :] * 128 + np.arange(128)[:, None])
    tokc = np.stack([tok // 64, tok % 64, np.ones_like(tok)], axis=-1).astype(np.float32).reshape(128, NO * 3)
    return {
        "ident": np.eye(128).astype(bf),
        "identf": np.eye(128, dtype=np.float32),
        "tri": tri,
        "ones": np.ones((128, 128), np.float32),
        "maskA": mA.astype(bf),
        "maskB": mB.astype(bf),
        "iota": np.arange(CAP, dtype=np.float32).reshape(1, CAP),
        "tokc": np.ascontiguousarray(tokc),
    }


def _positions(c):
    half = c % 2
    i = np.arange(WIN)
    return i if half == 0 else (SEQ - 1 - i)


def _in_maps(x, g_mix, w_in, a_sink, g_out_a, g_out_b, w_out, g_ffn, w_router, w_gate, w_up, w_down, g_final, n_exp=NE):
    consts = _consts()
    inv_freq = (np.float32(500000.0) ** (-np.arange(0, 16, 2, dtype=np.float32) / np.float32(16))).astype(np.float32)
    sink = np.asarray(a_sink[0], np.float32)
    perm = [0, 2, 1, 3, 4, 6, 5, 7]
    shared = {
        "g_mix": np.ascontiguousarray(g_mix[0:1]), "g_ffn": np.ascontiguousarray(g_ffn[0:1]),
        "g_final": np.ascontiguousarray(np.asarray(g_final).reshape(1, D)),
        "g_out": np.ascontiguousarray(np.concatenate([g_out_a[0], g_out_b[0]])[None, :]),
        "w_in": np.ascontiguousarray(w_in[0]), "w_out": np.ascontiguousarray(w_out[0]),
        "w_router": np.ascontiguousarray(w_router[0]),
        "w_gate": np.ascontiguousarray(w_gate[0][:n_exp]), "w_up": np.ascontiguousarray(w_up[0][:n_exp]),
        "w_down": np.ascontiguousarray(w_down[0][:n_exp]),
        "sinkp": np.ascontiguousarray(sink[perm][None, :]),
    }
    shared.update(consts)
    maps = []
    for c in range(8):
        pos = _positions(c)
        ang = pos.astype(np.float32)[:, None] * inv_freq[None, :]
        m = dict(shared)
        m["xw"] = np.ascontiguousarray(x[c // 2][pos])
        m["cosw"] = np.cos(ang).astype(np.float32)
        m["sinw"] = np.sin(ang).astype(np.float32)
        m["par"] = np.array([[1.0, 0.0]] if c % 2 == 0 else [[0.0, 1.0]], np.float32)
        maps.append(m)
    return maps


_NC_CACHE = {}


def kernel(x, g_mix, w_in, a_sink, g_out_a, g_out_b, w_out, g_ffn, w_router, w_gate, w_up, w_down, g_final):
    args = [np.asarray(a) for a in (x, g_mix, w_in, a_sink, g_out_a, g_out_b, w_out, g_ffn, w_router, w_gate, w_up, w_down, g_final)]
    if "nc" not in _NC_CACHE:
        _NC_CACHE["nc"] = build()
    nc = _NC_CACHE["nc"]
    maps = _in_maps(*args)
    res = run_bass_kernel_spmd(nc, maps, core_ids=list(range(8)))
    out = np.empty((4, SEQ, D), np.float32)
    for c in range(8):
        pos = _positions(c)[:OWN]
        out[c // 2][pos] = res.results[c]["out"]
    return out
```
